# Optimizing a Trainium2 kernel written in Bass

```python
import math
import jax, jax.numpy as jnp
from jax import lax
import numpy as np


D_MODEL = 2048
BATCH = 2
SEQ = 8192
DEPTH = 4
DEC_BATCH = 1
DEC_SEQ = 8192
PAST_LEN = 128

N_MIXERS = 2
EPS = 1e-6
MLA_HEADS = 16
Q_LORA = 512
KV_LORA = 256
D_NOPE = 128
D_ROPE = 64
D_V = 128
ROPE_BASE = 10000.0
Q_BLOCK = 128
MLA_IN = Q_LORA + KV_LORA + D_ROPE
ML_HEADS = 8
ML_DK = 128
ML_DV = 256
ML_CHUNK = 128
GATE_CAP = 15.0
ML_HK = ML_HEADS * ML_DK
ML_HV = ML_HEADS * ML_DV
ML_IN = 2 * ML_HK + 2 * ML_HV + 4 * ML_HEADS
PEER_HEADS = 8
PEER_DQ = 256
PEER_DH = PEER_DQ // 2
N_KEYS = 128
N_EXPERTS = N_KEYS * N_KEYS
PEER_TOPK = 16
PEER_BLOCK = 128

N_MLA_LAYERS = (DEPTH + N_MIXERS - 1) // N_MIXERS
N_ML_LAYERS = DEPTH // N_MIXERS

kernel_name = 'hybrid_mla_mlstm_peer_encoder'


def _rmsnorm(x, g):
    x32 = x.astype(jnp.float32)
    y = x32 * lax.rsqrt(jnp.mean(x32 * x32, axis=-1, keepdims=True) + EPS)
    return (y * g.astype(jnp.float32)).astype(x.dtype)


def _rope_tables(S):
    inv_freq = ROPE_BASE ** (-jnp.arange(0, D_ROPE, 2, dtype=jnp.float32) / D_ROPE)
    ang = jnp.arange(S, dtype=jnp.float32)[:, None] * inv_freq[None, :]
    return jnp.cos(ang), jnp.sin(ang)


def _rope(x, cos, sin):
    x1, x2 = jnp.split(x.astype(jnp.float32), 2, axis=-1)
    return jnp.concatenate([x1 * cos - x2 * sin, x2 * cos + x1 * sin], axis=-1).astype(x.dtype)


def _mla(x, w_in, q_norm, w_q_up, kv_norm, w_kv_up, w_out):
    B, S, _ = x.shape
    z = x @ w_in
    cq, ckv, kr = jnp.split(z, [Q_LORA, Q_LORA + KV_LORA], axis=-1)
    q = (_rmsnorm(cq, q_norm) @ w_q_up).reshape(B, S, MLA_HEADS, D_NOPE + D_ROPE)
    q_nope, q_rope = jnp.split(q, [D_NOPE], axis=-1)
    kv = (_rmsnorm(ckv, kv_norm) @ w_kv_up).reshape(B, S, MLA_HEADS, D_NOPE + D_V)
    k_nope, v = jnp.split(kv, [D_NOPE], axis=-1)
    cos, sin = _rope_tables(S)
    q_rope = _rope(q_rope, cos[:, None, :], sin[:, None, :])
    k_rope = _rope(kr, cos, sin)
    nq = S // Q_BLOCK
    qn = q_nope.reshape(B, nq, Q_BLOCK, MLA_HEADS, D_NOPE).transpose(1, 0, 2, 3, 4)
    qr = q_rope.reshape(B, nq, Q_BLOCK, MLA_HEADS, D_ROPE).transpose(1, 0, 2, 3, 4)
    scale = (D_NOPE + D_ROPE) ** -0.5

    def attend(args):
        qn_b, qr_b = args
        s = (jnp.einsum('bqhd,bkhd->bhqk', qn_b, k_nope)
             + jnp.einsum('bqhr,bkr->bhqk', qr_b, k_rope)).astype(jnp.float32) * scale
        p = jax.nn.softmax(s, axis=-1).astype(v.dtype)
        return jnp.einsum('bhqk,bkhd->bqhd', p, v)

    o = lax.map(attend, (qn, qr))
    o = o.transpose(1, 0, 2, 3, 4).reshape(B, S, MLA_HEADS * D_V)
    return o @ w_out


def _mlstm_dir(q, k, v, i_pre, f_pre):
    B, H, S, _ = q.shape
    L = ML_CHUNK
    nc = S // L
    qc = q.reshape(B, H, nc, L, ML_DK)
    kc = k.reshape(B, H, nc, L, ML_DK)
    vc = v.reshape(B, H, nc, L, ML_DV)
    log_f = jax.nn.log_sigmoid(f_pre).reshape(B, H, nc, L)
    log_i = i_pre.reshape(B, H, nc, L)
    b = jnp.cumsum(log_f, axis=-1)
    b_last = b[..., -1]
    w_end = b_last[..., None] - b + log_i
    m_loc = jnp.max(w_end, axis=-1)

    def step(carry, xs):
        C, n, m = carry
        k_j, v_j, w_j, ml_j, bl_j = xs
        m_new = jnp.maximum(bl_j + m, ml_j)
        dec = jnp.exp(bl_j + m - m_new)
        wk = jnp.exp(w_j - m_new[..., None])[..., None] * k_j
        C_new = dec[..., None, None] * C + jnp.einsum('bhld,bhle->bhde', wk, v_j)
        n_new = dec[..., None] * n + jnp.sum(wk, axis=2)
        return (C_new, n_new, m_new), (C, n, m)

    init = (jnp.zeros((B, H, ML_DK, ML_DV), jnp.float32),
            jnp.zeros((B, H, ML_DK), jnp.float32),
            jnp.zeros((B, H), jnp.float32))
    xs = (jnp.moveaxis(kc, 2, 0), jnp.moveaxis(vc, 2, 0), jnp.moveaxis(w_end, 2, 0),
          jnp.moveaxis(m_loc, 2, 0), jnp.moveaxis(b_last, 2, 0))
    _, (C_prev, n_prev, m_prev) = lax.scan(step, init, xs)
    C_prev = jnp.moveaxis(C_prev, 0, 2)
    n_prev = jnp.moveaxis(n_prev, 0, 2)
    m_prev = jnp.moveaxis(m_prev, 0, 2)
    lower = jnp.tril(jnp.ones((L, L), dtype=bool))
    d = jnp.where(lower, b[..., :, None] - b[..., None, :] + log_i[..., None, :], -jnp.inf)
    inter = b + m_prev[..., None]
    m_c = jnp.maximum(inter, jnp.max(d, axis=-1))
    s = jnp.einsum('bhctd,bhcsd->bhcts', qc, kc) * jnp.exp(d - m_c[..., None])
    a_inter = jnp.exp(inter - m_c)
    num = (jnp.einsum('bhcts,bhcse->bhcte', s, vc)
           + a_inter[..., None] * jnp.einsum('bhctd,bhcde->bhcte', qc, C_prev))
    den = jnp.sum(s, axis=-1) + a_inter * jnp.einsum('bhctd,bhcd->bhct', qc, n_prev)
    h = num / jnp.maximum(jnp.abs(den), jnp.exp(-m_c))[..., None]
    return h.reshape(B, H, S, ML_DV)


def _mlstm(x, w_in, b_gates, head_norm, w_out):
    B, S, _ = x.shape
    z = x @ w_in
    q, k, v, o, g = jnp.split(z, [ML_HK, 2 * ML_HK, 2 * ML_HK + ML_HV, 2 * ML_HK + 2 * ML_HV], axis=-1)

    def heads(t, dh):
        return t.reshape(B, S, ML_HEADS, dh).transpose(0, 2, 1, 3).astype(jnp.float32)

    q = heads(q, ML_DK) * (ML_DK ** -0.5)
    k = heads(k, ML_DK)
    v = heads(v, ML_DV)
    g = g.astype(jnp.float32).reshape(B, S, 4, ML_HEADS) + b_gates.astype(jnp.float32)
    g = GATE_CAP * jnp.tanh(g / GATE_CAP)
    g = g.transpose(2, 0, 3, 1)
    h_f = _mlstm_dir(q, k, v, g[0], g[1])
    flip = lambda t: jnp.flip(t, axis=2)
    h_b = flip(_mlstm_dir(flip(q), flip(k), flip(v), flip(g[2]), flip(g[3])))
    hs = _rmsnorm(h_f + h_b, head_norm[:, None, :])
    hs = hs.transpose(0, 2, 1, 3).reshape(B, S, ML_HV).astype(x.dtype)
    return (jax.nn.sigmoid(o) * hs) @ w_out


def _peer(x, w_query, sub_keys, u_tab, v_tab):
    B, S, D = x.shape
    xt = x.reshape((B * S) // PEER_BLOCK, PEER_BLOCK, D)

    def block(xb):
        tb = xb.shape[0]
        q = (xb @ w_query).reshape(tb, PEER_HEADS, 2, PEER_DH)
        s = jnp.einsum('thpc,hpnc->thpn', q, sub_keys).astype(jnp.float32)
        s_top, i_top = lax.top_k(s, PEER_TOPK)
        comb = s_top[:, :, 0, :, None] + s_top[:, :, 1, None, :]
        c_top, c_idx = lax.top_k(comb.reshape(tb, PEER_HEADS, PEER_TOPK * PEER_TOPK), PEER_TOPK)
        i1 = jnp.take_along_axis(i_top[:, :, 0], c_idx // PEER_TOPK, axis=-1)
        i2 = jnp.take_along_axis(i_top[:, :, 1], c_idx % PEER_TOPK, axis=-1)
        expert = i1 * N_KEYS + i2
        gate = jax.nn.softmax(c_top, axis=-1)
        u = jnp.take(u_tab, expert, axis=0)
        a = jax.nn.gelu(jnp.einsum('thkd,td->thk', u, xb))
        w = (gate * a.astype(jnp.float32)).astype(xb.dtype)
        vv = jnp.take(v_tab, expert, axis=0)
        return jnp.einsum('thk,thkd->td', w, vv)

    return lax.map(block, xt).reshape(B, S, D)


def setup_inputs(seed: int = 0) -> dict:
    key = jax.random.key(seed)
    ks = jax.random.split(key, 22)
    nrm = lambda k, shape, scale: jax.random.normal(k, shape, jnp.float32) * scale
    NA, NM = N_MLA_LAYERS, N_ML_LAYERS
    f_base = jnp.linspace(3.0, 6.0, ML_HEADS, dtype=jnp.float32)
    zh = jnp.zeros((ML_HEADS,), jnp.float32)
    gate_base = jnp.stack([zh, f_base, zh, f_base])
    return {
        'x_prompt': nrm(ks[0], (BATCH, SEQ, D_MODEL), 1.0),
        'x_sample': nrm(ks[1], (DEC_BATCH, DEC_SEQ, D_MODEL), 1.0),
        'norm_mix': 1.0 + nrm(ks[2], (DEPTH, D_MODEL), 0.02),
        'norm_ffn': 1.0 + nrm(ks[3], (DEPTH, D_MODEL), 0.02),
        'norm_final': 1.0 + nrm(ks[4], (D_MODEL,), 0.02),
        'mla_w_in': nrm(ks[5], (NA, D_MODEL, MLA_IN), D_MODEL ** -0.5),
        'mla_q_norm': 1.0 + nrm(ks[6], (NA, Q_LORA), 0.02),
        'mla_w_q_up': nrm(ks[7], (NA, Q_LORA, MLA_HEADS * (D_NOPE + D_ROPE)), Q_LORA ** -0.5),
        'mla_kv_norm': 1.0 + nrm(ks[8], (NA, KV_LORA), 0.02),
        'mla_w_kv_up': nrm(ks[9], (NA, KV_LORA, MLA_HEADS * (D_NOPE + D_V)), KV_LORA ** -0.5),
        'mla_w_out': nrm(ks[10], (NA, MLA_HEADS * D_V, D_MODEL), (MLA_HEADS * D_V) ** -0.5),
        'ml_w_in': nrm(ks[11], (NM, D_MODEL, ML_IN), D_MODEL ** -0.5),
        'ml_b_gates': gate_base[None] + nrm(ks[12], (NM, 4, ML_HEADS), 0.1),
        'ml_head_norm': 1.0 + nrm(ks[13], (NM, ML_HEADS, ML_DV), 0.02),
        'ml_w_out': nrm(ks[14], (NM, ML_HV, D_MODEL), ML_HV ** -0.5),
        'peer_w_query': nrm(ks[15], (DEPTH, D_MODEL, PEER_HEADS * PEER_DQ), D_MODEL ** -0.5),
        'peer_sub_keys': nrm(ks[16], (DEPTH, PEER_HEADS, 2, N_KEYS, PEER_DH), PEER_DH ** -0.5),
        'peer_u': nrm(ks[17], (DEPTH, N_EXPERTS, D_MODEL), D_MODEL ** -0.5),
        'peer_v': nrm(ks[18], (DEPTH, N_EXPERTS, D_MODEL), D_MODEL ** -0.5),
    }


def reference(x_prompt, x_sample, norm_mix, norm_ffn, norm_final,
              mla_w_in, mla_q_norm, mla_w_q_up, mla_kv_norm, mla_w_kv_up, mla_w_out,
              ml_w_in, ml_b_gates, ml_head_norm, ml_w_out,
              peer_w_query, peer_sub_keys, peer_u, peer_v):
    def trunk(x):
        for i in range(DEPTH):
            j = i // N_MIXERS
            h = _rmsnorm(x, norm_mix[i])
            if i % N_MIXERS == 0:
                x = x + _mla(h, mla_w_in[j], mla_q_norm[j], mla_w_q_up[j],
                             mla_kv_norm[j], mla_w_kv_up[j], mla_w_out[j])
            else:
                x = x + _mlstm(h, ml_w_in[j], ml_b_gates[j], ml_head_norm[j], ml_w_out[j])
            x = x + _peer(_rmsnorm(x, norm_ffn[i]), peer_w_query[i], peer_sub_keys[i],
                          peer_u[i], peer_v[i])
        return _rmsnorm(x, norm_final)

    y_prompt = trunk(x_prompt)
    y_sample = trunk(x_sample)
    return (y_prompt, y_sample)
```

```python
import contextlib
import numpy as np
import concourse.bass as bass
import concourse.mybir as mybir
from concourse.bass_utils import run_bass_kernel_spmd

F32 = mybir.dt.float32; BF16 = mybir.dt.bfloat16; I32 = mybir.dt.int32; U32 = mybir.dt.uint32
AF = mybir.ActivationFunctionType
ALU = mybir.AluOpType
AX = mybir.AxisListType
D = 2048
EPS = 1e-6
NEG = -1.0e30


class T:
    def __init__(self, h, name=""):
        self.h = h; self.name = name; self.w = None; self.r = []
    def __getitem__(self, idx):
        return self.h[idx]


class S:
    NDMA = 16
    def __init__(self, nc):
        self.nc = nc
        self.eng = {'pe': nc.tensor, 'act': nc.scalar, 'dve': nc.vector, 'pool': nc.gpsimd, 'sp': nc.sync}
        self.sem = {k: nc.alloc_semaphore("s_" + k) for k in self.eng}
        self.cnt = {k: 0 for k in self.eng}
        self.seen = {k: {} for k in self.eng}
        self.dsem = [nc.alloc_semaphore("d%d" % i) for i in range(self.NDMA)]
        self.dval = [0] * self.NDMA
        self.dnext = 0
        self.ninst = 0
    def _wait(self, eng, deps):
        need = {}
        for tok in deps:
            if tok is None: continue
            k, v = tok
            if v > need.get(k, 0): need[k] = v
        for k, v in need.items():
            if self.seen[eng].get(k, 0) >= v: continue
            sem = self.sem[k] if isinstance(k, str) else self.dsem[k]
            self.eng[eng].wait_ge(sem, v)
            self.seen[eng][k] = v
    def _deps(self, reads, writes):
        deps = []
        for t in reads: deps.append(t.w)
        for t in writes:
            deps.append(t.w); deps.extend(t.r)
        return deps
    def _commit(self, tok, reads, writes):
        for t in reads:
            t.r.append(tok)
            if len(t.r) > 64: t.r = t.r[-48:]
        for t in writes:
            t.w = tok; t.r = []
    def op(self, eng, fn, reads=(), writes=(), extra=()):
        self._wait(eng, self._deps(reads, writes) + list(extra))
        inst = fn(self.eng[eng])
        self.cnt[eng] += 1; self.ninst += 1
        inst.then_inc(self.sem[eng], 1)
        tok = (eng, self.cnt[eng])
        self._commit(tok, reads, writes)
        return tok
    def dma(self, q, fn, reads=(), writes=(), extra=()):
        j = self.dnext; self.dnext = (self.dnext + 1) % self.NDMA
        deps = self._deps(reads, writes) + list(extra)
        if self.dval[j] > 0: deps.append((j, self.dval[j]))
        self._wait(q, deps)
        inst = fn(self.eng[q])
        self.dval[j] += 16; self.ninst += 1
        inst.then_inc(self.dsem[j], 16)
        tok = (j, self.dval[j])
        self._commit(tok, reads, writes)
        return tok
    def barrier(self):
        allt = [(k, self.cnt[k]) for k in self.eng if self.cnt[k] > 0]
        allt += [(j, self.dval[j]) for j in range(self.NDMA) if self.dval[j] > 0]
        for e in self.eng:
            self._wait(e, allt)


class Ctx:
    pass


_UNIQ = [0]


def sb(es, nc, name, shape, dt):
    _UNIQ[0] += 1
    name = "%s_%d" % (name, _UNIQ[0])
    return T(es.enter_context(nc.sbuf_tensor(name, shape, dt)), name)


def psb(es, nc, name, shape, dt):
    _UNIQ[0] += 1
    name = "%s_%d" % (name, _UNIQ[0])
    return T(es.enter_context(nc.psum_tensor(name, shape, dt)), name)


def load_cast(s, dst, dst_ap, src_ap, q='pool'):
    return s.dma(q, lambda e: e.dma_start(out=dst_ap, in_=src_ap), writes=[dst])


def norm_tile(c, s, src_ap, xt, gB, xn, small, junk):
    ss, rs = small
    s.dma('sp', lambda e: e.dma_start(out=xt[:], in_=src_ap), writes=[xt])
    s.op('act', lambda e: e.activation(out=junk[:], in_=xt[:], func=AF.Square, accum_out=ss[:, 0:1]),
         reads=[xt], writes=[junk, ss])
    s.op('dve', lambda e: e.tensor_scalar(out=rs[:, 0:1], in0=ss[:, 0:1], scalar1=1.0 / D, scalar2=EPS,
                                          op0=ALU.mult, op1=ALU.add), reads=[ss], writes=[rs])
    s.op('act', lambda e: e.activation(out=rs[:, 1:2], in_=rs[:, 0:1], func=AF.Sqrt), reads=[rs], writes=[rs])
    s.op('dve', lambda e: e.reciprocal(out=rs[:, 2:3], in_=rs[:, 1:2]), reads=[rs], writes=[rs])
    s.op('dve', lambda e: e.scalar_tensor_tensor(out=xn[:], in0=xt[:], scalar=rs[:, 2:3], in1=gB[:],
                                                 op0=ALU.mult, op1=ALU.mult), reads=[xt, rs, gB], writes=[xn])


def transpose_tile(c, s, xn, xnT, col0, pst, nchunk=16):
    for g in range(nchunk // 4):
        p = pst[g % len(pst)]
        for i in range(4):
            ch = g * 4 + i
            s.op('pe', lambda e: e.transpose(out=p[:, i * 128:(i + 1) * 128], in_=xn[:, ch * 128:(ch + 1) * 128],
                                             identity=c.ident_bf[:]), reads=[xn, c.ident_bf], writes=[p])
        eng = 'act' if g % 2 == 0 else 'dve'
        if eng == 'act':
            s.op('act', lambda e: e.copy(out=xnT[:, g * 4:g * 4 + 4, col0:col0 + 128],
                                         in_=p[:, 0:512].rearrange("p (a b) -> p a b", a=4)), reads=[p], writes=[xnT])
        else:
            s.op('dve', lambda e: e.tensor_copy(out=xnT[:, g * 4:g * 4 + 4, col0:col0 + 128],
                                                in_=p[:, 0:512].rearrange("p (a b) -> p a b", a=4)), reads=[p], writes=[xnT])


def load_gain(c, s, gB, vec_ap):
    s.dma('sp', lambda e: e.dma_start(out=gB[:], in_=vec_ap.partition_broadcast(128)), writes=[gB])


def final_norm(c, s, src, dst):
    nc = c.nc
    with contextlib.ExitStack() as es:
        gB = sb(es, nc, "fn_gB", [128, D], F32)
        xts = [sb(es, nc, "fn_xt%d" % i, [128, D], F32) for i in range(2)]
        xos = [sb(es, nc, "fn_xo%d" % i, [128, D], F32) for i in range(2)]
        junk = sb(es, nc, "fn_junk", [128, D], BF16)
        smalls = [(sb(es, nc, "fn_ss%d" % i, [128, 1], F32), sb(es, nc, "fn_rs%d" % i, [128, 4], F32)) for i in range(2)]
        load_gain(c, s, gB, c.w['norm_final'])
        for t in range(c.NT):
            xt = xts[t % 2]; xo = xos[t % 2]
            norm_tile(c, s, src[t * 128:(t + 1) * 128, :], xt, gB, xo, smalls[t % 2], junk)
            s.dma('sp', lambda e: e.dma_start(out=dst[t * 128:(t + 1) * 128, :], in_=xo[:]), reads=[xo])
        s.barrier()


def peer_layer(c, s, li, src, dst):
    nc = c.nc
    NH, KK = 8, 16
    with contextlib.ExitStack() as es:
        gB = sb(es, nc, "pr_gB", [128, D], F32)
        wq = sb(es, nc, "pr_wq", [128, 16, D], BF16)
        KT = sb(es, nc, "pr_KT", [128, 16, 128], BF16)
        kst = sb(es, nc, "pr_kst", [128, 128], F32)
        xts = [sb(es, nc, "pr_xt%d" % i, [128, D], F32) for i in range(2)]
        xn = sb(es, nc, "pr_xn", [128, D], BF16)
        junk = sb(es, nc, "pr_junk", [128, D], BF16)
        small = (sb(es, nc, "pr_ss", [128, 1], F32), sb(es, nc, "pr_rs", [128, 4], F32))
        xnT = sb(es, nc, "pr_xnT", [128, 16, 128], BF16)
        qT = sb(es, nc, "pr_qT", [128, 16, 128], BF16)
        NUG = 4
        ug = [sb(es, nc, "pr_ug%d" % i, [128, D], BF16) for i in range(NUG)]
        vg = [sb(es, nc, "pr_vg%d" % i, [128, D], BF16) for i in range(NUG)]
        wexp = sb(es, nc, "pr_wexp", [128, 128 * 128], BF16)
        s16 = sb(es, nc, "pr_s16", [128, 16, 16], F32)
        i16 = sb(es, nc, "pr_i16", [128, 16, 16], U32)
        i16f = sb(es, nc, "pr_i16f", [128, 16, 16], F32)
        srep = sb(es, nc, "pr_srep", [128, 128], F32)
        comb = sb(es, nc, "pr_comb", [128, 8, 256], F32)
        comb2 = sb(es, nc, "pr_comb2", [128, 256], F32)
        ct = sb(es, nc, "pr_ct", [128, 8, 16], F32)
        ci = sb(es, nc, "pr_ci", [128, 8, 16], U32)
        k1 = sb(es, nc, "pr_k1", [128, 128], U32)
        k2 = sb(es, nc, "pr_k2", [128, 128], U32)
        k1f = sb(es, nc, "pr_k1f", [128, 128], F32)
        k2f = sb(es, nc, "pr_k2f", [128, 128], F32)
        oh = sb(es, nc, "pr_oh", [128, 128, 16], F32)
        i1f = sb(es, nc, "pr_i1f", [128, 128], F32)
        i2f = sb(es, nc, "pr_i2f", [128, 128], F32)
        ef = sb(es, nc, "pr_ef", [128, 128], F32)
        eu = sb(es, nc, "pr_eu", [128, 128], I32)
        evTs = [sb(es, nc, "pr_evT%d" % i, [128, 128], I32) for i in range(2)]
        gate = sb(es, nc, "pr_gate", [128, 8, 16], F32)
        gsum = sb(es, nc, "pr_gsum", [128, 8], F32)
        av = sb(es, nc, "pr_av", [128, 128], F32)
        g1 = sb(es, nc, "pr_g1", [128, 128], F32)
        g2 = sb(es, nc, "pr_g2", [128, 128], F32)
        wv = sb(es, nc, "pr_wv", [128, 128], F32)
        wT = sb(es, nc, "pr_wT", [128, 128], BF16)
        iota16 = sb(es, nc, "pr_iota16", [128, 16], F32)
        ps_s = [psb(es, nc, "pr_ps%d" % i, [128, 512], F32) for i in range(2)]
        ps_t = [psb(es, nc, "pr_pt%d" % i, [128, 1024], BF16) for i in range(2)]
        ps_o = [psb(es, nc, "pr_po%d" % i, [128, 512], F32) for i in range(4)]

        load_gain(c, s, gB, c.w['norm_ffn'][li])
        wsrc = c.w['peer_w_query'][li].rearrange("(c p) n -> p c n", p=128)
        for c0 in range(0, 16, 2):
            load_cast(s, wq, wq[:, c0:c0 + 2, :], wsrc[:, c0:c0 + 2, :])
        for hp in range(16):
            s.dma('sp', lambda e: e.dma_start(out=kst[:], in_=c.w['peer_sub_keys'][li, hp // 2, hp % 2]), writes=[kst])
            p = ps_s[hp % 2]
            s.op('pe', lambda e: e.transpose(out=p[:, 0:128], in_=kst[:], identity=c.ident_f[:]),
                 reads=[kst, c.ident_f], writes=[p])
            s.op('dve', lambda e: e.tensor_copy(out=KT[:, hp, :], in_=p[:, 0:128]), reads=[p], writes=[KT])
        s.op('pool', lambda e: e.iota(iota16[:], pattern=[[1, 16]], base=0, channel_multiplier=0,
                                      allow_small_or_imprecise_dtypes=True), writes=[iota16])
        s.op('dve', lambda e: e.memset(wexp[:], 0.0), writes=[wexp])
        utab = c.tab_u
        vtab = c.tab_v

        def stage_A(t):
            xt = xts[t % 2]; evT = evTs[t % 2]
            rows = slice(t * 128, (t + 1) * 128)
            norm_tile(c, s, src[rows, :], xt, gB, xn, small, junk)
            transpose_tile(c, s, xn, xnT, 0, ps_t)
            for hp in range(16):
                p = ps_s[hp % 2]
                for kc in range(16):
                    s.op('pe', lambda e: e.matmul(p[:, 0:128], lhsT=wq[:, kc, hp * 128:(hp + 1) * 128], rhs=xnT[:, kc, :],
                                                  start=(kc == 0), stop=(kc == 15)), reads=[wq, xnT], writes=[p])
                if hp % 2 == 0:
                    s.op('act', lambda e: e.copy(out=qT[:, hp, :], in_=p[:, 0:128]), reads=[p], writes=[qT])
                else:
                    s.op('dve', lambda e: e.tensor_copy(out=qT[:, hp, :], in_=p[:, 0:128]), reads=[p], writes=[qT])
            for hp in range(16):
                p = ps_s[hp % 2]
                s.op('pe', lambda e: e.matmul(p[:, 0:128], lhsT=qT[:, hp, :], rhs=KT[:, hp, :], start=True, stop=True),
                     reads=[qT, KT], writes=[p])
                s.op('dve', lambda e: e.tensor_copy(out=srep[:], in_=p[:, 0:128]), reads=[p], writes=[srep])
                s.op('dve', lambda e: e.max(out=s16[:, hp, 0:8], in_=srep[:]), reads=[srep], writes=[s16])
                s.op('dve', lambda e: e.max_index(out=i16[:, hp, 0:8], in_max=s16[:, hp, 0:8], in_values=srep[:]),
                     reads=[srep, s16], writes=[i16])
                s.op('dve', lambda e: e.match_replace(out=srep[:], in_to_replace=s16[:, hp, 0:8], in_values=srep[:],
                                                      imm_value=NEG), reads=[s16], writes=[srep])
                s.op('dve', lambda e: e.max(out=s16[:, hp, 8:16], in_=srep[:]), reads=[srep], writes=[s16])
                s.op('dve', lambda e: e.max_index(out=i16[:, hp, 8:16], in_max=s16[:, hp, 8:16], in_values=srep[:]),
                     reads=[srep, s16], writes=[i16])
            s.op('dve', lambda e: e.tensor_copy(out=i16f[:], in_=i16[:]), reads=[i16], writes=[i16f])
            for h in range(NH):
                s.op('dve', lambda e: e.tensor_tensor(
                    out=comb[:, h, :].rearrange("p (a b) -> p a b", a=16),
                    in0=s16[:, 2 * h, :].unsqueeze(2).to_broadcast([128, 16, 16]),
                    in1=s16[:, 2 * h + 1, :].unsqueeze(1).to_broadcast([128, 16, 16]), op=ALU.add),
                    reads=[s16], writes=[comb])
                s.op('dve', lambda e: e.max(out=ct[:, h, 0:8], in_=comb[:, h, :]), reads=[comb], writes=[ct])
                s.op('dve', lambda e: e.max_index(out=ci[:, h, 0:8], in_max=ct[:, h, 0:8], in_values=comb[:, h, :]),
                     reads=[comb, ct], writes=[ci])
                s.op('dve', lambda e: e.match_replace(out=comb2[:], in_to_replace=ct[:, h, 0:8], in_values=comb[:, h, :],
                                                      imm_value=NEG), reads=[ct, comb], writes=[comb2])
                s.op('dve', lambda e: e.max(out=ct[:, h, 8:16], in_=comb2[:]), reads=[comb2], writes=[ct])
                s.op('dve', lambda e: e.max_index(out=ci[:, h, 8:16], in_max=ct[:, h, 8:16], in_values=comb2[:]),
                     reads=[comb2, ct], writes=[ci])
            s.op('dve', lambda e: e.tensor_tensor(out=gate[:], in0=ct[:], in1=ct[:, :, 0:1].to_broadcast([128, 8, 16]),
                                                  op=ALU.subtract), reads=[ct], writes=[gate])
            s.op('act', lambda e: e.activation(out=gate[:], in_=gate[:], func=AF.Exp), reads=[gate], writes=[gate])
            s.op('dve', lambda e: e.tensor_reduce(out=gsum[:], in_=gate[:], axis=AX.X, op=ALU.add), reads=[gate], writes=[gsum])
            s.op('dve', lambda e: e.reciprocal(out=gsum[:], in_=gsum[:]), reads=[gsum], writes=[gsum])
            s.op('dve', lambda e: e.tensor_tensor(out=gate[:], in0=gate[:], in1=gsum[:].unsqueeze(2).to_broadcast([128, 8, 16]),
                                                  op=ALU.mult), reads=[gate, gsum], writes=[gate])
            cif = ci[:].rearrange("p h k -> p (h k)")
            s.op('dve', lambda e: e.tensor_single_scalar(out=k1[:], in_=cif, scalar=4, op=ALU.logical_shift_right),
                 reads=[ci], writes=[k1])
            s.op('dve', lambda e: e.tensor_single_scalar(out=k2[:], in_=cif, scalar=15, op=ALU.bitwise_and),
                 reads=[ci], writes=[k2])
            s.op('dve', lambda e: e.tensor_copy(out=k1f[:], in_=k1[:]), reads=[k1], writes=[k1f])
            s.op('dve', lambda e: e.tensor_copy(out=k2f[:], in_=k2[:]), reads=[k2], writes=[k2f])
            for (kf, pi, of) in ((k1f, 0, i1f), (k2f, 1, i2f)):
                s.op('dve', lambda e: e.tensor_tensor(out=oh[:], in0=kf[:].unsqueeze(2).to_broadcast([128, 128, 16]),
                                                      in1=iota16[:].unsqueeze(1).to_broadcast([128, 128, 16]),
                                                      op=ALU.is_equal), reads=[kf, iota16], writes=[oh])
                i16v = i16f[:].rearrange("p (h q) k -> p h q k", q=2)[:, :, pi, :]
                s.op('dve', lambda e: e.tensor_tensor(out=oh[:].rearrange("p (h k) j -> p h k j", h=8),
                                                      in0=oh[:].rearrange("p (h k) j -> p h k j", h=8),
                                                      in1=i16v.unsqueeze(2).to_broadcast([128, 8, 16, 16]),
                                                      op=ALU.mult), reads=[oh, i16f], writes=[oh])
                s.op('dve', lambda e: e.tensor_reduce(out=of[:], in_=oh[:], axis=AX.X, op=ALU.add), reads=[oh], writes=[of])
            s.op('dve', lambda e: e.scalar_tensor_tensor(out=ef[:], in0=i1f[:], scalar=128.0, in1=i2f[:],
                                                         op0=ALU.mult, op1=ALU.add), reads=[i1f, i2f], writes=[ef])
            if li > 0:
                s.op('dve', lambda e: e.tensor_scalar(out=ef[:], in0=ef[:], scalar1=float(li * 16384), scalar2=None, op0=ALU.add),
                     reads=[ef], writes=[ef])
            s.op('dve', lambda e: e.tensor_copy(out=eu[:], in_=ef[:]), reads=[ef], writes=[eu])
            p = ps_s[0]
            s.op('pe', lambda e: e.transpose(out=p[:, 0:128], in_=ef[:], identity=c.ident_f[:]), reads=[ef, c.ident_f], writes=[p])
            s.op('dve', lambda e: e.tensor_copy(out=evT[:], in_=p[:, 0:128]), reads=[p], writes=[evT])

        def stage_U(t):
            for k in range(128):
                g = ug[k % NUG]
                s.dma('pool', lambda e: e.indirect_dma_start(out=g[:], out_offset=None, in_=utab,
                                                             in_offset=bass.IndirectOffsetOnAxis(ap=eu[:, k:k + 1], axis=0)),
                      reads=[eu], writes=[g])
                s.op('dve', lambda e: e.scalar_tensor_tensor(out=junk[:], in0=g[:], scalar=1.0, in1=xn[:], op0=ALU.mult,
                                                             op1=ALU.mult, accum_out=av[:, k:k + 1]),
                     reads=[g, xn], writes=[junk, av])
            s.op('dve', lambda e: e.tensor_tensor(out=g1[:], in0=av[:], in1=av[:], op=ALU.mult), reads=[av], writes=[g1])
            s.op('dve', lambda e: e.tensor_scalar(out=g1[:], in0=g1[:], scalar1=0.044715 * 0.7978845608028654,
                                                  scalar2=0.7978845608028654, op0=ALU.mult, op1=ALU.add), reads=[g1], writes=[g1])
            s.op('dve', lambda e: e.tensor_tensor(out=g1[:], in0=g1[:], in1=av[:], op=ALU.mult), reads=[g1, av], writes=[g1])
            s.op('act', lambda e: e.activation(out=g2[:], in_=g1[:], func=AF.Tanh), reads=[g1], writes=[g2])
            s.op('dve', lambda e: e.tensor_scalar(out=g2[:], in0=g2[:], scalar1=1.0, scalar2=0.5, op0=ALU.add, op1=ALU.mult),
                 reads=[g2], writes=[g2])
            s.op('dve', lambda e: e.tensor_tensor(out=g2[:], in0=g2[:], in1=av[:], op=ALU.mult), reads=[g2, av], writes=[g2])
            s.op('dve', lambda e: e.tensor_tensor(out=wv[:], in0=g2[:], in1=gate[:].rearrange("p h k -> p (h k)"), op=ALU.mult),
                 reads=[g2, gate], writes=[wv])
            p = ps_s[1]
            s.op('pe', lambda e: e.transpose(out=p[:, 0:128], in_=wv[:], identity=c.ident_f[:]), reads=[wv, c.ident_f], writes=[p])
            s.op('dve', lambda e: e.tensor_copy(out=wexp[:, 0:128 * 128:129], in_=p[:, 0:128]), reads=[p], writes=[wexp])

        def stage_V(t):
            xt = xts[t % 2]; evT = evTs[t % 2]
            rows = slice(t * 128, (t + 1) * 128)
            for tk in range(128):
                g = vg[tk % NUG]
                s.dma('pool', lambda e: e.indirect_dma_start(out=g[:], out_offset=None, in_=vtab,
                                                             in_offset=bass.IndirectOffsetOnAxis(ap=evT[:, tk:tk + 1], axis=0)),
                      reads=[evT], writes=[g])
                for nb in range(4):
                    s.op('pe', lambda e: e.matmul(ps_o[nb][:], lhsT=wexp[:, tk * 128:(tk + 1) * 128],
                                                  rhs=g[:, nb * 512:(nb + 1) * 512], start=(tk == 0), stop=(tk == 127)),
                         reads=[wexp, g], writes=[ps_o[nb]])
            for nb in range(4):
                s.op('dve', lambda e: e.tensor_tensor(out=xt[:, nb * 512:(nb + 1) * 512], in0=ps_o[nb][:],
                                                      in1=xt[:, nb * 512:(nb + 1) * 512], op=ALU.add),
                     reads=[ps_o[nb], xt], writes=[xt])
            s.dma('sp', lambda e: e.dma_start(out=dst[rows, :], in_=xt[:]), reads=[xt])

        stage_A(0); stage_U(0)
        for t in range(c.NT):
            if t + 1 < c.NT:
                stage_A(t + 1)
            stage_V(t)
            if t + 1 < c.NT:
                stage_U(t + 1)
        s.barrier()


def scr(c, name, shape, dt):
    if name not in c.scr:
        kind = "ExternalOutput" if (getattr(c, 'dbg', False) and name in ('hf_scr', 'hn_scr', 'g_scr', 'b_scr', 'ao_scr')) else "Internal"
        c.scr[name] = c.nc.dram_tensor(name, shape, dt, kind=kind).ap()
    return c.scr[name]


def evac(s, i, out_ap, in_ap, reads, writes):
    if i % 2 == 0:
        s.op('act', lambda e: e.copy(out=out_ap, in_=in_ap), reads=reads, writes=writes)
    else:
        s.op('dve', lambda e: e.tensor_copy(out=out_ap, in_=in_ap), reads=reads, writes=writes)


def mla_layer(c, s, li, j, src, dst):
    import os
    nc = c.nc; Sq = c.S; NB = Sq // 512; NT = c.NT
    scale = 192.0 ** -0.5
    qn_scr = scr(c, "qn_scr", [16, 128, Sq], BF16)
    qr_scr = scr(c, "qr_scr", [16, 64, Sq], BF16)
    ao_scr = scr(c, "ao_scr", [16, 128, Sq], BF16)
    ckv_scr = scr(c, "ckv_scr", [2, 128, Sq], BF16)
    kr_scr = scr(c, "kr_scr", [64, Sq], BF16)
    with contextlib.ExitStack() as es:
        gB = sb(es, nc, "m1_gB", [128, D], F32)
        w_in = sb(es, nc, "m1_win", [128, 16, 832], BF16)
        w_krsw = sb(es, nc, "m1_wkrsw", [128, 16, 64], BF16)
        wqu = sb(es, nc, "m1_wqu", [128, 4, 3072], BF16)
        wqsw = sb(es, nc, "m1_wqsw", [128, 4, 16, 64], BF16)
        gq = sb(es, nc, "m1_gq", [128, 4], F32)
        gkv = sb(es, nc, "m1_gkv", [128, 2], F32)
        xt = sb(es, nc, "m1_xt", [128, D], F32)
        xn = sb(es, nc, "m1_xn", [128, D], BF16)
        junk = sb(es, nc, "m1_junk", [128, D], BF16)
        small = (sb(es, nc, "m1_ss", [128, 1], F32), sb(es, nc, "m1_rs", [128, 4], F32))
        xnT = sb(es, nc, "m1_xnT", [128, 16, 512], BF16)
        raw = sb(es, nc, "m1_raw", [128, 6, 512], F32)
        sq = sb(es, nc, "m1_sq", [128, 6, 512], BF16)
        tq = sb(es, nc, "m1_tq", [128, 512], F32)
        tkv = sb(es, nc, "m1_tkv", [128, 512], F32)
        cqn = sb(es, nc, "m1_cqn", [128, 4, 512], BF16)
        ckvn = sb(es, nc, "m1_ckvn", [128, 2, 512], BF16)
        krn = sb(es, nc, "m1_krn", [64, 512], BF16)
        CC = sb(es, nc, "m1_CC", [64, 512], F32); SS = sb(es, nc, "m1_SS", [64, 512], F32)
        CCq = sb(es, nc, "m1_CCq", [64, 512], F32); SSq = sb(es, nc, "m1_SSq", [64, 512], F32)
        t1 = sb(es, nc, "m1_t1", [64, 512], F32); t2 = sb(es, nc, "m1_t2", [64, 512], F32)
        qst = [sb(es, nc, "m1_qst%d" % i, [128, 512], BF16) for i in range(2)]
        qrst = [sb(es, nc, "m1_qrst%d" % i, [64, 512], BF16) for i in range(2)]
        ps_t = [psb(es, nc, "m1_pt%d" % i, [128, 1024], BF16) for i in range(2)]
        ps_m = [psb(es, nc, "m1_pm%d" % i, [128, 512], F32) for i in range(4)]
        ps_r = [psb(es, nc, "m1_pr%d" % i, [128, 512], F32) for i in range(2)]

        load_gain(c, s, gB, c.w['norm_mix'][li])
        wsrc = c.w['mla_w_in'][j].rearrange("(c p) n -> p c n", p=128)
        for c0 in range(0, 16, 4):
            load_cast(s, w_in, w_in[:, c0:c0 + 4, :], wsrc[:, c0:c0 + 4, :])
        load_cast(s, w_krsw, w_krsw[:, :, 0:32], wsrc[:, :, 800:832])
        load_cast(s, w_krsw, w_krsw[:, :, 32:64], wsrc[:, :, 768:800])
        qsrc = c.w['mla_w_q_up'][j].rearrange("(c p) n -> p c n", p=128)
        q5 = c.w['mla_w_q_up'][j].rearrange("(c p) (h f) -> p c h f", p=128, f=192)
        for cc in range(4):
            for h0 in (0, 1536):
                load_cast(s, wqu, wqu[:, cc, h0:h0 + 1536], qsrc[:, cc, h0:h0 + 1536])
            load_cast(s, wqsw, wqsw[:, cc, :, 0:32], q5[:, cc, :, 160:192])
            load_cast(s, wqsw, wqsw[:, cc, :, 32:64], q5[:, cc, :, 128:160])
        s.dma('sp', lambda e: e.dma_start(out=gq[:], in_=c.w['mla_q_norm'][j].rearrange("(c p) -> p c", p=128),
                                          allow_slow_non_contiguous=True), writes=[gq])
        s.dma('sp', lambda e: e.dma_start(out=gkv[:], in_=c.w['mla_kv_norm'][j].rearrange("(c p) -> p c", p=128),
                                          allow_slow_non_contiguous=True), writes=[gkv])
        STG = os.environ.get('MLA_STG', 'Z')
        if STG == 'A':
            s.barrier(); return
        for b in range(NB):
            cols = slice(b * 512, (b + 1) * 512)
            for jt in range(4):
                rows = slice(b * 512 + jt * 128, b * 512 + (jt + 1) * 128)
                norm_tile(c, s, src[rows, :], xt, gB, xn, small, junk)
                transpose_tile(c, s, xn, xnT, jt * 128, ps_t)
            s.dma('sp', lambda e: e.dma_start(out=CC[:], in_=c.w['rope_c'][:, cols]), writes=[CC])
            s.dma('sp', lambda e: e.dma_start(out=SS[:], in_=c.w['rope_s'][:, cols]), writes=[SS])
            s.op('dve', lambda e: e.tensor_scalar(out=CCq[:], in0=CC[:], scalar1=scale, scalar2=None, op0=ALU.mult),
                 reads=[CC], writes=[CCq])
            s.op('dve', lambda e: e.tensor_scalar(out=SSq[:], in0=SS[:], scalar1=scale, scalar2=None, op0=ALU.mult),
                 reads=[SS], writes=[SSq])
            if STG == 'B':
                s.barrier(); return
            for fb in range(6):
                p = ps_m[fb % 4]
                for kc in range(16):
                    s.op('pe', lambda e: e.matmul(p[:], lhsT=w_in[:, kc, fb * 128:(fb + 1) * 128], rhs=xnT[:, kc, :],
                                                  start=(kc == 0), stop=(kc == 15)), reads=[w_in, xnT], writes=[p])
                s.op('dve', lambda e: e.tensor_copy(out=raw[:, fb, :], in_=p[:]), reads=[p], writes=[raw])
                s.op('act', lambda e: e.activation(out=sq[:, fb, :], in_=raw[:, fb, :], func=AF.Square), reads=[raw], writes=[sq])
            if STG == 'C':
                s.barrier(); return
            for (wt, lo, pr) in ((w_in, 768, ps_r[0]), (w_krsw, 0, ps_r[1])):
                for kc in range(16):
                    s.op('pe', lambda e: e.matmul(pr[0:64, :], lhsT=wt[:, kc, lo:lo + 64], rhs=xnT[:, kc, :],
                                                  start=(kc == 0), stop=(kc == 15)), reads=[wt, xnT], writes=[pr])
            s.op('dve', lambda e: e.tensor_tensor(out=t1[:], in0=ps_r[0][0:64, :], in1=CC[:], op=ALU.mult),
                 reads=[ps_r[0], CC], writes=[t1])
            s.op('dve', lambda e: e.tensor_tensor(out=t2[:], in0=ps_r[1][0:64, :], in1=SS[:], op=ALU.mult),
                 reads=[ps_r[1], SS], writes=[t2])
            s.op('dve', lambda e: e.tensor_tensor(out=krn[:], in0=t1[:], in1=t2[:], op=ALU.add), reads=[t1, t2], writes=[krn])
            s.dma('sp', lambda e: e.dma_start(out=kr_scr[:, cols], in_=krn[:]), reads=[krn])
            if STG == 'D':
                s.barrier(); return
            for (f0, nf, tt, n) in ((0, 4, tq, 512.0), (4, 2, tkv, 256.0)):
                p = ps_m[0 if f0 == 0 else 1]
                for i in range(nf):
                    s.op('pe', lambda e: e.matmul(p[:], lhsT=c.ones_bf[:], rhs=sq[:, f0 + i, :], start=(i == 0), stop=(i == nf - 1)),
                         reads=[c.ones_bf, sq], writes=[p])
                s.op('dve', lambda e: e.tensor_scalar(out=tt[:], in0=p[:], scalar1=1.0 / n, scalar2=EPS, op0=ALU.mult, op1=ALU.add),
                     reads=[p], writes=[tt])
                s.op('act', lambda e: e.activation(out=tt[:], in_=tt[:], func=AF.Ln), reads=[tt], writes=[tt])
                s.op('act', lambda e: e.activation(out=tt[:], in_=tt[:], func=AF.Exp, scale=-0.5), reads=[tt], writes=[tt])
            for fb in range(4):
                s.op('dve', lambda e: e.scalar_tensor_tensor(out=cqn[:, fb, :], in0=raw[:, fb, :], scalar=gq[:, fb:fb + 1], in1=tq[:],
                                                             op0=ALU.mult, op1=ALU.mult), reads=[raw, gq, tq], writes=[cqn])
            for fb in range(2):
                s.op('dve', lambda e: e.scalar_tensor_tensor(out=ckvn[:, fb, :], in0=raw[:, 4 + fb, :], scalar=gkv[:, fb:fb + 1],
                                                             in1=tkv[:], op0=ALU.mult, op1=ALU.mult), reads=[raw, gkv, tkv], writes=[ckvn])
            s.dma('sp', lambda e: e.dma_start(out=ckv_scr.rearrange("c p s -> p c s")[:, :, cols], in_=ckvn[:]), reads=[ckvn])
            if STG == 'E':
                s.barrier(); return
            for h in range(16):
                p = ps_m[h % 4]
                for kc in range(4):
                    s.op('pe', lambda e: e.matmul(p[:], lhsT=wqu[:, kc, h * 192:h * 192 + 128], rhs=cqn[:, kc, :],
                                                  start=(kc == 0), stop=(kc == 3)), reads=[wqu, cqn], writes=[p])
                qs = qst[h % 2]
                s.op('act', lambda e: e.mul(out=qs[:], in_=p[:], mul=scale), reads=[p], writes=[qs])
                s.dma('sp', lambda e: e.dma_start(out=qn_scr[h, :, cols], in_=qs[:]), reads=[qs])
                for kc in range(4):
                    s.op('pe', lambda e: e.matmul(ps_r[0][0:64, :], lhsT=wqu[:, kc, h * 192 + 128:h * 192 + 192], rhs=cqn[:, kc, :],
                                                  start=(kc == 0), stop=(kc == 3)), reads=[wqu, cqn], writes=[ps_r[0]])
                for kc in range(4):
                    s.op('pe', lambda e: e.matmul(ps_r[1][0:64, :], lhsT=wqsw[:, kc, h, :], rhs=cqn[:, kc, :],
                                                  start=(kc == 0), stop=(kc == 3)), reads=[wqsw, cqn], writes=[ps_r[1]])
                s.op('dve', lambda e: e.tensor_tensor(out=t1[:], in0=ps_r[0][0:64, :], in1=CCq[:], op=ALU.mult),
                     reads=[ps_r[0], CCq], writes=[t1])
                s.op('dve', lambda e: e.tensor_tensor(out=t2[:], in0=ps_r[1][0:64, :], in1=SSq[:], op=ALU.mult),
                     reads=[ps_r[1], SSq], writes=[t2])
                qr = qrst[h % 2]
                s.op('dve', lambda e: e.tensor_tensor(out=qr[:], in0=t1[:], in1=t2[:], op=ALU.add), reads=[t1, t2], writes=[qr])
                s.dma('sp', lambda e: e.dma_start(out=qr_scr[h, :, cols], in_=qr[:]), reads=[qr])
        s.barrier()
    import os
    if os.environ.get('MLA_STOP') == '1': return
    with contextlib.ExitStack() as es:
        wkv = sb(es, nc, "m2_wkv", [128, 2, 4096], BF16)
        ckvT = sb(es, nc, "m2_ckvT", [128, 2, Sq], BF16)
        krT = sb(es, nc, "m2_krT", [64, Sq], BF16)
        knT = [sb(es, nc, "m2_knT%d" % i, [128, Sq], BF16) for i in range(2)]
        Vh = [sb(es, nc, "m2_Vh%d" % i, [128, NT, 128], BF16) for i in range(2)]
        qn = [sb(es, nc, "m2_qn%d" % i, [128, 512], BF16) for i in range(2)]
        qr = [sb(es, nc, "m2_qr%d" % i, [64, 512], BF16) for i in range(2)]
        pT = [sb(es, nc, "m2_pT%d" % i, [128, 512], BF16) for i in range(3)]
        rden = sb(es, nc, "m2_rden", [128, 512], F32)
        ao = [sb(es, nc, "m2_ao%d" % i, [128, 512], BF16) for i in range(2)]
        ps_s = [psb(es, nc, "m2_ps%d" % i, [128, 512], F32) for i in range(3)]
        ps_o = psb(es, nc, "m2_po", [128, 512], F32)
        ps_d = psb(es, nc, "m2_pd", [128, 512], F32)
        ps_b = [psb(es, nc, "m2_pb%d" % i, [128, 512], F32) for i in range(2)]
        ksrc = c.w['mla_w_kv_up'][j].rearrange("(c p) n -> p c n", p=128)
        for cc in range(2):
            for h0 in (0, 2048):
                load_cast(s, wkv, wkv[:, cc, h0:h0 + 2048], ksrc[:, cc, h0:h0 + 2048])
            s.dma('sp', lambda e: e.dma_start(out=ckvT[:, cc, :], in_=ckv_scr[cc]), writes=[ckvT])
        s.dma('sp', lambda e: e.dma_start(out=krT[:], in_=kr_scr), writes=[krT])
        ev = 0
        for h in range(16):
            kb = knT[h % 2]; vb = Vh[h % 2]
            for b in range(NB):
                cols = slice(b * 512, (b + 1) * 512)
                p = ps_b[b % 2]
                for cc in range(2):
                    s.op('pe', lambda e: e.matmul(p[:], lhsT=wkv[:, cc, h * 256:h * 256 + 128], rhs=ckvT[:, cc, cols],
                                                  start=(cc == 0), stop=(cc == 1)), reads=[wkv, ckvT], writes=[p])
                ev += 1
                evac(s, ev, kb[:, cols], p[:], [p], [kb])
            for g in range(NT // 4):
                p = ps_b[g % 2]
                for i in range(4):
                    tl = g * 4 + i
                    for cc in range(2):
                        s.op('pe', lambda e: e.matmul(p[:, i * 128:(i + 1) * 128], lhsT=ckvT[:, cc, tl * 128:(tl + 1) * 128],
                                                      rhs=wkv[:, cc, h * 256 + 128:h * 256 + 256], start=(cc == 0), stop=(cc == 1)),
                             reads=[wkv, ckvT], writes=[p])
                ev += 1
                evac(s, ev, vb[:, g * 4:(g + 1) * 4, :], p[:].rearrange("p (a b) -> p a b", a=4), [p], [vb])
            for qb in range(NB):
                qcols = slice(qb * 512, (qb + 1) * 512)
                qnb = qn[qb % 2]; qrb = qr[qb % 2]
                s.dma('sp', lambda e: e.dma_start(out=qnb[:], in_=qn_scr[h, :, qcols]), writes=[qnb])
                s.dma('sp', lambda e: e.dma_start(out=qrb[:], in_=qr_scr[h, :, qcols]), writes=[qrb])

                def scores(kt):
                    p = ps_s[kt % 3]
                    s.op('pe', lambda e: e.matmul(p[:], lhsT=kb[:, kt * 128:(kt + 1) * 128], rhs=qnb[:], start=True, stop=False),
                         reads=[kb, qnb], writes=[p])
                    s.op('pe', lambda e: e.matmul(p[:], lhsT=krT[:, kt * 128:(kt + 1) * 128], rhs=qrb[:], start=False, stop=True),
                         reads=[krT, qrb], writes=[p])
                scores(0)
                for kt in range(NT):
                    if kt + 1 < NT:
                        scores(kt + 1)
                    p = ps_s[kt % 3]; pt = pT[kt % 3]
                    s.op('act', lambda e: e.activation(out=pt[:], in_=p[:], func=AF.Exp), reads=[p], writes=[pt])
                    s.op('pe', lambda e: e.matmul(ps_o[:], lhsT=vb[:, kt, :], rhs=pt[:], start=(kt == 0), stop=(kt == NT - 1)),
                         reads=[vb, pt], writes=[ps_o])
                    s.op('pe', lambda e: e.matmul(ps_d[:], lhsT=c.ones_bf[:], rhs=pt[:], start=(kt == 0), stop=(kt == NT - 1)),
                         reads=[c.ones_bf, pt], writes=[ps_d])
                s.op('dve', lambda e: e.reciprocal(out=rden[:], in_=ps_d[:]), reads=[ps_d], writes=[rden])
                aob = ao[qb % 2]
                s.op('dve', lambda e: e.tensor_tensor(out=aob[:], in0=ps_o[:], in1=rden[:], op=ALU.mult),
                     reads=[ps_o, rden], writes=[aob])
                s.dma('sp', lambda e: e.dma_start(out=ao_scr[h, :, qcols], in_=aob[:]), reads=[aob])
        s.barrier()
    if os.environ.get('MLA_STOP') == '2': return
    out_proj(c, s, "m3", ao_scr, c.w['mla_w_out'][j], src, dst)


def out_proj(c, s, pfx, a_scr, w_ap, src, dst):
    nc = c.nc; Sq = c.S; NB = Sq // 512
    with contextlib.ExitStack() as es:
        wo = sb(es, nc, pfx + "_wo", [128, 16, D], BF16)
        aoT = [sb(es, nc, pfx + "_aoT%d" % i, [128, 16, 512], BF16) for i in range(2)]
        xts = [sb(es, nc, pfx + "_xt%d" % i, [128, D], F32) for i in range(2)]
        xos = [sb(es, nc, pfx + "_xo%d" % i, [128, D], F32) for i in range(2)]
        ps = [psb(es, nc, pfx + "_ps%d" % i, [128, 512], F32) for i in range(8)]
        wsrc = w_ap.rearrange("(h p) n -> p h n", p=128)
        for c0 in range(0, 16, 2):
            load_cast(s, wo, wo[:, c0:c0 + 2, :], wsrc[:, c0:c0 + 2, :])
        k = 0
        for b in range(NB):
            cols = slice(b * 512, (b + 1) * 512)
            a = aoT[b % 2]
            s.dma('sp', lambda e: e.dma_start(out=a[:], in_=a_scr.rearrange("h p s -> p h s")[:, :, cols]), writes=[a])
            for jt in range(4):
                rows = slice(b * 512 + jt * 128, b * 512 + (jt + 1) * 128)
                xt = xts[k % 2]; xo = xos[k % 2]
                s.dma('sp', lambda e: e.dma_start(out=xt[:], in_=src[rows, :]), writes=[xt])
                for nb in range(4):
                    p = ps[(k % 2) * 4 + nb]
                    for h in range(16):
                        s.op('pe', lambda e: e.matmul(p[:], lhsT=a[:, h, jt * 128:(jt + 1) * 128], rhs=wo[:, h, nb * 512:(nb + 1) * 512],
                                                      start=(h == 0), stop=(h == 15)), reads=[a, wo], writes=[p])
                    s.op('dve', lambda e: e.tensor_tensor(out=xo[:, nb * 512:(nb + 1) * 512], in0=p[:], in1=xt[:, nb * 512:(nb + 1) * 512],
                                                          op=ALU.add), reads=[p, xt], writes=[xo])
                s.dma('sp', lambda e: e.dma_start(out=dst[rows, :], in_=xo[:]), reads=[xo])
                k += 1
        s.barrier()


def mlstm_layer(c, s, li, j, src, dst):
    nc = c.nc; Sq = c.S; NT = c.NT; NC = NT
    hf_scr = scr(c, "hf_scr", [Sq, D], F32)
    hn_scr = scr(c, "hn_scr", [Sq, D], BF16)
    a_scr = scr(c, "ao_scr", [16, 128, Sq], BF16)
    win = c.w['ml_w_in'][j].rearrange("(c p) n -> p c n", p=128)
    with contextlib.ExitStack() as esl:
        tokv = sb(esl, nc, "l_tokv", [128, NC, 48], F32)
        bc = sb(esl, nc, "l_bc", [128, 2, 8, 2, NC], F32)
        maskF = sb(esl, nc, "l_maskF", [128, 128], F32)
        maskB = sb(esl, nc, "l_maskB", [128, 128], F32)
        SEG = min(Sq, 1024); NSEG = Sq // SEG; CPS = SEG // 128
        g_scr = scr(c, "g_scr", [2, 8, Sq], F32)
        b_scr = scr(c, "b_scr", [2, 8, Sq], F32)
        with contextlib.ExitStack() as es:
            gB = sb(es, nc, "l0_gB", [128, D], F32)
            wg = sb(es, nc, "l0_wg", [128, 16, 32], BF16)
            xt = sb(es, nc, "l0_xt", [128, D], F32); xn = sb(es, nc, "l0_xn", [128, D], BF16)
            junk = sb(es, nc, "l0_junk", [128, D], BF16)
            small = (sb(es, nc, "l0_ss", [128, 1], F32), sb(es, nc, "l0_rs", [128, 4], F32))
            xnT = sb(es, nc, "l0_xnT", [128, 16, 128], BF16)
            bg = sb(es, nc, "l0_bg", [8, 4], F32)
            gp = [sb(es, nc, "l0_gp%d" % i, [8, SEG], F32) for i in range(4)]
            lf = sb(es, nc, "l0_lf", [8, SEG], F32); pf = sb(es, nc, "l0_pf", [8, SEG], F32)
            bb = sb(es, nc, "l0_bb", [8, SEG], F32); gg = sb(es, nc, "l0_gg", [8, SEG], F32)
            tmp = sb(es, nc, "l0_tmp", [8, SEG], F32)
            cmask = sb(es, nc, "l0_cmask", [8, SEG], F32)
            vec = [[sb(es, nc, "l0_vec%d%d" % (d, v), [8, SEG], F32) for v in range(3)] for d in range(2)]
            blA = [sb(es, nc, "l0_bl%d" % d, [8, NC], F32) for d in range(2)]
            gmaxA = [sb(es, nc, "l0_gmax%d" % d, [8, NC], F32) for d in range(2)]
            MtA = [sb(es, nc, "l0_Mt%d" % d, [8, NC], F32) for d in range(2)]
            bmA = [sb(es, nc, "l0_bm%d" % d, [8, NC], F32) for d in range(2)]
            ml_ = sb(es, nc, "l0_ml", [8, NC], F32); mst = sb(es, nc, "l0_mst", [8, NC + 1], F32)
            t8 = sb(es, nc, "l0_t8", [8, NC], F32)
            dca = [[sb(es, nc, "l0_dca%d%d" % (d, v), [8, NC], F32) for v in range(2)] for d in range(2)]
            sel = sb(es, nc, "l0_sel", [8, 8, 128], F32)
            io = sb(es, nc, "l0_io", [128, 128], F32); ip = sb(es, nc, "l0_ip", [128, 128], F32)
            ps_t = [psb(es, nc, "l0_pt%d" % i, [128, 1024], BF16) for i in range(2)]
            ps_g = [psb(es, nc, "l0_pg%d" % i, [128, 512], F32) for i in range(4)]
            load_gain(c, s, gB, c.w['norm_mix'][li])
            load_cast(s, wg, wg[:], win[:, :, 6144:6176])
            s.dma('sp', lambda e: e.dma_start(out=bg[:], in_=c.w['ml_b_gates'][j].rearrange("t h -> h t"),
                                              allow_slow_non_contiguous=True), writes=[bg])
            s.op('dve', lambda e: e.tensor_scalar(out=bg[:], in0=bg[:], scalar1=1.0 / 15.0, scalar2=None, op0=ALU.mult),
                 reads=[bg], writes=[bg])
            s.op('pool', lambda e: e.iota(io[:], pattern=[[1, 128]], base=0, channel_multiplier=0,
                                          allow_small_or_imprecise_dtypes=True), writes=[io])
            s.op('pool', lambda e: e.iota(ip[:], pattern=[[0, 128]], base=0, channel_multiplier=1,
                                          allow_small_or_imprecise_dtypes=True), writes=[ip])
            s.op('dve', lambda e: e.tensor_tensor(out=maskF[:], in0=io[:], in1=ip[:], op=ALU.is_ge), reads=[io, ip], writes=[maskF])
            s.op('dve', lambda e: e.tensor_tensor(out=maskB[:], in0=io[:], in1=ip[:], op=ALU.is_le), reads=[io, ip], writes=[maskB])
            s.op('dve', lambda e: e.tensor_copy(out=sel[:], in_=c.ident_f[0:8, 0:8].unsqueeze(2).to_broadcast([8, 8, 128])),
                 reads=[c.ident_f], writes=[sel])
            s.op('dve', lambda e: e.memset(cmask[:], 1.0), writes=[cmask])
            s.op('dve', lambda e: e.memset(cmask[:, 0:SEG:128], 0.0), writes=[cmask])
            v3 = lambda tl: tl[:].rearrange("p (a b) -> p a b", b=128)
            bcast = lambda ap: ap.unsqueeze(2).to_broadcast([8, CPS, 128])
            for sg in range(NSEG):
                ch = slice(sg * CPS, (sg + 1) * CPS)
                scols = slice(sg * SEG, (sg + 1) * SEG)
                for tl in range(CPS):
                    t = sg * CPS + tl
                    rows = slice(t * 128, (t + 1) * 128)
                    lrows = slice(tl * 128, (tl + 1) * 128)
                    norm_tile(c, s, src[rows, :], xt, gB, xn, small, junk)
                    transpose_tile(c, s, xn, xnT, 0, ps_t)
                    for ty in range(4):
                        p = ps_g[ty]
                        for kc in range(16):
                            s.op('pe', lambda e: e.matmul(p[0:8, 0:128], lhsT=wg[:, kc, ty * 8:(ty + 1) * 8], rhs=xnT[:, kc, :],
                                                          start=(kc == 0), stop=(kc == 15)), reads=[wg, xnT], writes=[p])
                        s.op('act', lambda e: e.activation(out=gp[ty][:, lrows], in_=p[0:8, 0:128], func=AF.Tanh,
                                                           bias=bg[:, ty:ty + 1], scale=1.0 / 15.0), reads=[p, bg], writes=[gp[ty]])
                for d in range(2):
                    lig = gp[2 * d]; fpg = gp[2 * d + 1]; bl = blA[d]; gmax = gmaxA[d]
                    s.op('dve', lambda e: e.tensor_scalar(out=lig[:], in0=lig[:], scalar1=15.0, scalar2=None, op0=ALU.mult),
                         reads=[lig], writes=[lig])
                    s.op('act', lambda e: e.activation(out=lf[:], in_=fpg[:], func=AF.Sigmoid, scale=15.0), reads=[fpg], writes=[lf])
                    s.op('act', lambda e: e.activation(out=lf[:], in_=lf[:], func=AF.Ln), reads=[lf], writes=[lf])
                    s.op('dve', lambda e: e.tensor_tensor_scan(out=pf[:], data0=cmask[:], data1=lf[:], initial=0.0,
                                                               op0=ALU.mult, op1=ALU.add), reads=[cmask, lf], writes=[pf])
                    s.op('dve', lambda e: e.tensor_copy(out=bl[:, ch], in_=pf[:, 127:SEG:128]), reads=[pf], writes=[bl])
                    if d == 0:
                        s.op('dve', lambda e: e.tensor_copy(out=bb[:], in_=pf[:]), reads=[pf], writes=[bb])
                    else:
                        s.op('dve', lambda e: e.tensor_tensor(out=bb[:], in0=lf[:], in1=pf[:], op=ALU.subtract), reads=[lf, pf], writes=[bb])
                        s.op('dve', lambda e: e.tensor_tensor(out=v3(bb), in0=v3(bb), in1=bcast(bl[:, ch]), op=ALU.add),
                             reads=[bb, bl], writes=[bb])
                    s.op('dve', lambda e: e.tensor_tensor(out=gg[:], in0=lig[:], in1=bb[:], op=ALU.subtract), reads=[lig, bb], writes=[gg])
                    s.op('dve', lambda e: e.tensor_reduce(out=gmax[:, ch], in_=v3(gg), axis=AX.X, op=ALU.max), reads=[gg], writes=[gmax])
                    s.dma('sp', lambda e: e.dma_start(out=g_scr[d, :, scols], in_=gg[:]), reads=[gg])
                    s.dma('sp', lambda e: e.dma_start(out=b_scr[d, :, scols], in_=bb[:]), reads=[bb])
            for d in range(2):
                bl = blA[d]; gmax = gmaxA[d]; Mt = MtA[d]
                s.op('dve', lambda e: e.tensor_tensor(out=ml_[:], in0=bl[:], in1=gmax[:], op=ALU.add), reads=[bl, gmax], writes=[ml_])
                s.op('dve', lambda e: e.memset(mst[:], 0.0), writes=[mst])
                order = range(NC) if d == 0 else range(NC - 1, -1, -1)
                for jc in order:
                    pi, ni = (jc, jc + 1) if d == 0 else (jc + 1, jc)
                    s.op('dve', lambda e: e.scalar_tensor_tensor(out=mst[:, ni:ni + 1], in0=bl[:, jc:jc + 1], scalar=mst[:, pi:pi + 1],
                                                                 in1=ml_[:, jc:jc + 1], op0=ALU.add, op1=ALU.max),
                         reads=[bl, mst, ml_], writes=[mst])
                mp = mst[:, 0:NC] if d == 0 else mst[:, 1:NC + 1]
                mn = mst[:, 1:NC + 1] if d == 0 else mst[:, 0:NC]
                s.op('dve', lambda e: e.tensor_tensor(out=Mt[:], in0=mp, in1=gmax[:], op=ALU.max), reads=[mst, gmax], writes=[Mt])
                s.op('dve', lambda e: e.tensor_tensor(out=bmA[d][:], in0=bl[:], in1=mn, op=ALU.subtract), reads=[bl, mst], writes=[bmA[d]])
                s.op('dve', lambda e: e.tensor_tensor(out=t8[:], in0=bmA[d][:], in1=mp, op=ALU.add), reads=[bmA[d], mst], writes=[t8])
                s.op('act', lambda e: e.activation(out=dca[d][0][:], in_=t8[:], func=AF.Exp), reads=[t8], writes=[dca[d][0]])
                s.op('dve', lambda e: e.tensor_tensor(out=t8[:], in0=mp, in1=Mt[:], op=ALU.subtract), reads=[Mt, mst], writes=[t8])
                s.op('act', lambda e: e.activation(out=dca[d][1][:], in_=t8[:], func=AF.Exp), reads=[t8], writes=[dca[d][1]])
                for h in range(8):
                    for v in range(2):
                        p = ps_g[(h * 2 + v) % 4]
                        s.op('pe', lambda e: e.matmul(p[:, 0:NC], lhsT=sel[:, h, :], rhs=dca[d][v][:], start=True, stop=True),
                             reads=[sel, dca[d][v]], writes=[p])
                        s.op('dve', lambda e: e.tensor_copy(out=bc[:, d, h, v, :], in_=p[:, 0:NC]), reads=[p], writes=[bc])
            for sg in range(NSEG):
                ch = slice(sg * CPS, (sg + 1) * CPS)
                scols = slice(sg * SEG, (sg + 1) * SEG)
                for d in range(2):
                    Mt = MtA[d]
                    s.dma('sp', lambda e: e.dma_start(out=gg[:], in_=g_scr[d, :, scols]), writes=[gg])
                    s.dma('sp', lambda e: e.dma_start(out=bb[:], in_=b_scr[d, :, scols]), writes=[bb])
                    s.op('dve', lambda e: e.tensor_tensor(out=v3(tmp), in0=v3(gg), in1=bcast(Mt[:, ch]), op=ALU.subtract),
                         reads=[gg, Mt], writes=[tmp])
                    s.op('act', lambda e: e.activation(out=vec[d][0][:], in_=tmp[:], func=AF.Exp), reads=[tmp], writes=[vec[d][0]])
                    s.op('dve', lambda e: e.tensor_tensor(out=v3(tmp), in0=v3(gg), in1=bcast(bmA[d][:, ch]), op=ALU.add),
                         reads=[gg, bmA[d]], writes=[tmp])
                    s.op('act', lambda e: e.activation(out=vec[d][1][:], in_=tmp[:], func=AF.Exp), reads=[tmp], writes=[vec[d][1]])
                    s.op('dve', lambda e: e.tensor_tensor(out=v3(tmp), in0=v3(bb), in1=bcast(Mt[:, ch]), op=ALU.add),
                         reads=[bb, Mt], writes=[tmp])
                    s.op('act', lambda e: e.activation(out=vec[d][2][:], in_=tmp[:], func=AF.Exp, scale=-1.0), reads=[tmp], writes=[vec[d][2]])
                for tl in range(CPS):
                    jc = sg * CPS + tl
                    p = ps_g[jc % 4]
                    for d in range(2):
                        for v in range(3):
                            i = d * 3 + v
                            s.op('pe', lambda e: e.transpose(out=p[:, i * 8:(i + 1) * 8], in_=vec[d][v][:, tl * 128:(tl + 1) * 128],
                                                             identity=c.ident_f[0:8, 0:8]), reads=[vec[d][v], c.ident_f], writes=[p])
                    s.op('dve', lambda e: e.tensor_copy(out=tokv[:, jc, :], in_=p[:, 0:48]), reads=[p], writes=[tokv])
            if getattr(c, 'dbg', False):
                d1 = nc.dram_tensor("dbg_tokv", [128, NC, 48], F32, kind="ExternalOutput").ap()
                d2 = nc.dram_tensor("dbg_bc", [128, 2, 8, 2, NC], F32, kind="ExternalOutput").ap()
                s.dma('sp', lambda e: e.dma_start(out=d1, in_=tokv[:]), reads=[tokv])
                s.dma('sp', lambda e: e.dma_start(out=d2, in_=bc[:]), reads=[bc])
            s.barrier()
        for d in range(2):
            for hg in range(2):
                with contextlib.ExitStack() as es:
                    gB = sb(es, nc, "l1_gB", [128, D], F32)
                    wq = sb(es, nc, "l1_wq", [128, 16, 512], BF16)
                    wk = sb(es, nc, "l1_wk", [128, 16, 512], BF16)
                    wv = sb(es, nc, "l1_wv", [128, 16, 1024], BF16)
                    xt = sb(es, nc, "l1_xt", [128, D], F32); xn = sb(es, nc, "l1_xn", [128, D], BF16)
                    junk = sb(es, nc, "l1_junk", [128, D], BF16)
                    small = (sb(es, nc, "l1_ss", [128, 1], F32), sb(es, nc, "l1_rs", [128, 4], F32))
                    xnT = sb(es, nc, "l1_xnT", [128, 16, 128], BF16)
                    qT = sb(es, nc, "l1_qT", [128, 4, 128], BF16); kT = sb(es, nc, "l1_kT", [128, 4, 128], BF16)
                    kw = sb(es, nc, "l1_kw", [128, 4, 128], BF16)
                    vaug = sb(es, nc, "l1_vaug", [128, 4, 258], BF16)
                    Cst = sb(es, nc, "l1_C", [128, 4, 258], F32)
                    Ct = sb(es, nc, "l1_Ct", [128, 4, 258], BF16)
                    hacc = sb(es, nc, "l1_hacc", [128, 1024], F32)
                    hft = sb(es, nc, "l1_hft", [128, 1024], F32)
                    hn = sb(es, nc, "l1_hn", [128, 1024], BF16)
                    gH = sb(es, nc, "l1_gH", [128, 1024], F32)
                    PT = [sb(es, nc, "l1_PT%d" % i, [128, 128], BF16) for i in range(2)]
                    dd = sb(es, nc, "l1_dd", [128, 4], F32)
                    hs2 = (sb(es, nc, "l1_hss", [128, 1], F32), sb(es, nc, "l1_hrs", [128, 4], F32))
                    ps_t = [psb(es, nc, "l1_pt%d" % i, [128, 1024], BF16) for i in range(2)]
                    ps_a = [psb(es, nc, "l1_pa%d" % i, [128, 512], F32) for i in range(2)]
                    ps_s = psb(es, nc, "l1_pss", [128, 512], F32)
                    ps_n = [psb(es, nc, "l1_pn%d" % i, [128, 512], F32) for i in range(2)]
                    ps_c = psb(es, nc, "l1_pc", [128, 512], F32)
                    load_gain(c, s, gB, c.w['norm_mix'][li])
                    s.dma('sp', lambda e: e.dma_start(out=gH[:], in_=c.w['ml_head_norm'][j].rearrange("h d -> (h d)")[hg * 1024:(hg + 1) * 1024]
                                                      .partition_broadcast(128)), writes=[gH])
                    for c0 in range(0, 16, 4):
                        load_cast(s, wq, wq[:, c0:c0 + 4, :], win[:, c0:c0 + 4, hg * 512:(hg + 1) * 512])
                        load_cast(s, wk, wk[:, c0:c0 + 4, :], win[:, c0:c0 + 4, 1024 + hg * 512:1024 + (hg + 1) * 512])
                        load_cast(s, wv, wv[:, c0:c0 + 4, :], win[:, c0:c0 + 4, 2048 + hg * 1024:2048 + (hg + 1) * 1024])
                    s.op('dve', lambda e: e.memset(vaug[:], 1.0), writes=[vaug])
                    s.op('dve', lambda e: e.memset(Cst[:], 0.0), writes=[Cst])
                    s.op('dve', lambda e: e.memset(Ct[:], 0.0), writes=[Ct])
                    mask = maskF if d == 0 else maskB
                    order = list(range(NC)) if d == 0 else list(range(NC - 1, -1, -1))
                    for oi, jc in enumerate(order):
                        rows = slice(jc * 128, (jc + 1) * 128)
                        hcols = slice(hg * 1024, (hg + 1) * 1024)
                        norm_tile(c, s, src[rows, :], xt, gB, xn, small, junk)
                        transpose_tile(c, s, xn, xnT, 0, ps_t)
                        if d == 1:
                            s.dma('sp', lambda e: e.dma_start(out=hft[:], in_=hf_scr[rows, hcols]), writes=[hft])
                        for (wt, outt, sc, p) in ((wq, qT, 128.0 ** -0.5, ps_a[0]), (wk, kT, 1.0, ps_a[1])):
                            for h in range(4):
                                for kc in range(16):
                                    s.op('pe', lambda e: e.matmul(p[:, h * 128:(h + 1) * 128], lhsT=wt[:, kc, h * 128:(h + 1) * 128],
                                                                  rhs=xnT[:, kc, :], start=(kc == 0), stop=(kc == 15)),
                                         reads=[wt, xnT], writes=[p])
                            s.op('act', lambda e: e.mul(out=outt[:].rearrange("p a b -> p (a b)"), in_=p[:], mul=sc), reads=[p], writes=[outt])
                        p = ps_a[0]
                        for kc in range(16):
                            s.op('pe', lambda e: e.matmul(p[:], lhsT=xnT[:, kc, :], rhs=wk[:, kc, :], start=(kc == 0), stop=(kc == 15)),
                                 reads=[wk, xnT], writes=[p])
                        for h in range(4):
                            col = (d * 3 + 1) * 8 + hg * 4 + h
                            s.op('dve', lambda e: e.tensor_scalar(out=kw[:, h, :], in0=p[:, h * 128:(h + 1) * 128],
                                                                  scalar1=tokv[:, jc, col:col + 1], scalar2=None, op0=ALU.mult),
                                 reads=[p, tokv], writes=[kw])
                        for nb in range(2):
                            p = ps_a[1] if nb == 0 else ps_a[0]
                            for kc in range(16):
                                s.op('pe', lambda e: e.matmul(p[:], lhsT=xnT[:, kc, :], rhs=wv[:, kc, nb * 512:(nb + 1) * 512],
                                                              start=(kc == 0), stop=(kc == 15)), reads=[wv, xnT], writes=[p])
                            s.op('act', lambda e: e.copy(out=vaug[:, 2 * nb:2 * nb + 2, 0:256], in_=p[:].rearrange("p (a b) -> p a b", a=2)),
                                 reads=[p], writes=[vaug])
                        for h in range(4):
                            hh = hg * 4 + h
                            cew = (d * 3 + 0) * 8 + hh; ccl = (d * 3 + 2) * 8 + hh
                            s.op('pe', lambda e: e.matmul(ps_s[:, 0:128], lhsT=kT[:, h, :], rhs=qT[:, h, :], start=True, stop=True),
                                 reads=[kT, qT], writes=[ps_s])
                            pt = PT[h % 2]
                            s.op('dve', lambda e: e.scalar_tensor_tensor(out=pt[:], in0=ps_s[:, 0:128], scalar=tokv[:, jc, cew:cew + 1],
                                                                         in1=mask[:], op0=ALU.mult, op1=ALU.mult),
                                 reads=[ps_s, tokv, mask], writes=[pt])
                            pn = ps_n[h % 2]
                            s.op('pe', lambda e: e.matmul(pn[:, 0:258], lhsT=pt[:], rhs=vaug[:, h, :], start=True, stop=False),
                                 reads=[pt, vaug], writes=[pn])
                            s.op('pe', lambda e: e.matmul(pn[:, 0:258], lhsT=qT[:, h, :], rhs=Ct[:, h, :], start=False, stop=True),
                                 reads=[qT, Ct], writes=[pn])
                            s.op('dve', lambda e: e.tensor_scalar(out=dd[:, 0:1], in0=pn[:, 256:257], scalar1=-1.0, scalar2=None, op0=ALU.mult),
                                 reads=[pn], writes=[dd])
                            s.op('dve', lambda e: e.tensor_tensor(out=dd[:, 3:4], in0=dd[:, 0:1], in1=pn[:, 256:257], op=ALU.max),
                                 reads=[dd, pn], writes=[dd])
                            s.op('dve', lambda e: e.tensor_tensor(out=dd[:, 1:2], in0=dd[:, 3:4], in1=tokv[:, jc, ccl:ccl + 1], op=ALU.max),
                                 reads=[dd, tokv], writes=[dd])
                            s.op('dve', lambda e: e.reciprocal(out=dd[:, 2:3], in_=dd[:, 1:2]), reads=[dd], writes=[dd])
                            if d == 0:
                                s.op('dve', lambda e: e.tensor_scalar(out=hacc[:, h * 256:(h + 1) * 256], in0=pn[:, 0:256], scalar1=dd[:, 2:3],
                                                                      scalar2=None, op0=ALU.mult), reads=[pn, dd], writes=[hacc])
                            else:
                                s.op('dve', lambda e: e.scalar_tensor_tensor(out=hacc[:, h * 256:(h + 1) * 256], in0=pn[:, 0:256], scalar=dd[:, 2:3],
                                                                             in1=hft[:, h * 256:(h + 1) * 256], op0=ALU.mult, op1=ALU.add),
                                     reads=[pn, dd, hft], writes=[hacc])
                            s.op('pe', lambda e: e.matmul(ps_c[:, 0:258], lhsT=kw[:, h, :], rhs=vaug[:, h, :], start=True, stop=True),
                                 reads=[kw, vaug], writes=[ps_c])
                            s.op('dve', lambda e: e.scalar_tensor_tensor(out=Cst[:, h, :], in0=Cst[:, h, :], scalar=bc[:, d, hh, 0, jc:jc + 1],
                                                                         in1=ps_c[:, 0:258], op0=ALU.mult, op1=ALU.add),
                                 reads=[Cst, bc, ps_c], writes=[Cst])
                            if oi + 1 < NC:
                                jn = order[oi + 1]
                                s.op('dve', lambda e: e.tensor_scalar(out=Ct[:, h, :], in0=Cst[:, h, :], scalar1=bc[:, d, hh, 1, jn:jn + 1],
                                                                      scalar2=None, op0=ALU.mult), reads=[Cst, bc], writes=[Ct])
                        if d == 0:
                            s.dma('sp', lambda e: e.dma_start(out=hf_scr[rows, hcols], in_=hacc[:]), reads=[hacc])
                        else:
                            for h in range(4):
                                hsl = slice(h * 256, (h + 1) * 256)
                                ss, rs = hs2
                                s.op('act', lambda e: e.activation(out=junk[:, 0:256], in_=hacc[:, hsl], func=AF.Square, accum_out=ss[:, 0:1]),
                                     reads=[hacc], writes=[junk, ss])
                                s.op('dve', lambda e: e.tensor_scalar(out=rs[:, 0:1], in0=ss[:, 0:1], scalar1=1.0 / 256, scalar2=EPS,
                                                                      op0=ALU.mult, op1=ALU.add), reads=[ss], writes=[rs])
                                s.op('act', lambda e: e.activation(out=rs[:, 1:2], in_=rs[:, 0:1], func=AF.Sqrt), reads=[rs], writes=[rs])
                                s.op('dve', lambda e: e.reciprocal(out=rs[:, 2:3], in_=rs[:, 1:2]), reads=[rs], writes=[rs])
                                s.op('dve', lambda e: e.scalar_tensor_tensor(out=hn[:, hsl], in0=hacc[:, hsl], scalar=rs[:, 2:3], in1=gH[:, hsl],
                                                                             op0=ALU.mult, op1=ALU.mult), reads=[hacc, rs, gH], writes=[hn])
                            s.dma('sp', lambda e: e.dma_start(out=hn_scr[rows, hcols], in_=hn[:]), reads=[hn])
                    s.barrier()
        with contextlib.ExitStack() as es:
            gB = sb(es, nc, "l2_gB", [128, D], F32)
            wo = sb(es, nc, "l2_wo", [128, 16, D], BF16)
            xt = sb(es, nc, "l2_xt", [128, D], F32); xn = sb(es, nc, "l2_xn", [128, D], BF16)
            junk = sb(es, nc, "l2_junk", [128, D], BF16)
            small = (sb(es, nc, "l2_ss", [128, 1], F32), sb(es, nc, "l2_rs", [128, 4], F32))
            xnT = sb(es, nc, "l2_xnT", [128, 16, 128], BF16)
            og = sb(es, nc, "l2_og", [128, D], F32)
            hn = sb(es, nc, "l2_hn", [128, D], BF16)
            yv = sb(es, nc, "l2_yv", [128, D], BF16)
            yT = sb(es, nc, "l2_yT", [128, 16, 128], BF16)
            ps_t = [psb(es, nc, "l2_pt%d" % i, [128, 1024], BF16) for i in range(2)]
            ps_a = [psb(es, nc, "l2_pa%d" % i, [128, 512], F32) for i in range(4)]
            load_gain(c, s, gB, c.w['norm_mix'][li])
            for c0 in range(0, 16, 2):
                load_cast(s, wo, wo[:, c0:c0 + 2, :], win[:, c0:c0 + 2, 4096:6144])
            for t in range(NT):
                rows = slice(t * 128, (t + 1) * 128)
                norm_tile(c, s, src[rows, :], xt, gB, xn, small, junk)
                transpose_tile(c, s, xn, xnT, 0, ps_t)
                s.dma('sp', lambda e: e.dma_start(out=hn[:], in_=hn_scr[rows, :]), writes=[hn])
                for nb in range(4):
                    p = ps_a[nb]
                    for kc in range(16):
                        s.op('pe', lambda e: e.matmul(p[:], lhsT=xnT[:, kc, :], rhs=wo[:, kc, nb * 512:(nb + 1) * 512],
                                                      start=(kc == 0), stop=(kc == 15)), reads=[wo, xnT], writes=[p])
                    s.op('dve', lambda e: e.tensor_copy(out=og[:, nb * 512:(nb + 1) * 512], in_=p[:]), reads=[p], writes=[og])
                s.op('act', lambda e: e.activation(out=og[:], in_=og[:], func=AF.Sigmoid), reads=[og], writes=[og])
                s.op('dve', lambda e: e.tensor_tensor(out=yv[:], in0=og[:], in1=hn[:], op=ALU.mult), reads=[og, hn], writes=[yv])
                transpose_tile(c, s, yv, yT, 0, ps_t)
                s.dma('sp', lambda e: e.dma_start(out=a_scr.rearrange("h p s -> p h s")[:, :, rows], in_=yT[:]), reads=[yT])
            s.barrier()
    out_proj(c, s, "l3", a_scr, c.w['ml_w_out'][j], src, dst)


def build(Sq, layers, final=True, test=False, dbg=False):
    nc = bass.Bass("TRN2", target_bir_lowering=False)
    c = Ctx(); c.nc = nc; c.S = Sq; c.NT = Sq // 128; c.dbg = dbg
    shapes = dict(
        norm_mix=[4, D], norm_ffn=[4, D], norm_final=[D],
        mla_w_in=[2, D, 832], mla_q_norm=[2, 512], mla_w_q_up=[2, 512, 3072], mla_kv_norm=[2, 256],
        mla_w_kv_up=[2, 256, 4096], mla_w_out=[2, D, D],
        ml_w_in=[2, D, 6176], ml_b_gates=[2, 4, 8], ml_head_norm=[2, 8, 256], ml_w_out=[2, D, D],
        peer_w_query=[4, D, D], peer_sub_keys=[4, 8, 2, 128, 128], peer_u=[4, 16384, D], peer_v=[4, 16384, D],
        rope_c=[64, Sq], rope_s=[64, Sq])
    kinds = set(k for (k, _, _) in layers)
    if test:
        need = {'norm_final'}
        if 'peer' in kinds: need |= {'norm_ffn', 'peer_w_query', 'peer_sub_keys', 'peer_u', 'peer_v'}
        if 'mla' in kinds: need |= {'norm_mix', 'rope_c', 'rope_s'} | {k for k in shapes if k.startswith('mla_')}
        if 'mlstm' in kinds: need |= {'norm_mix'} | {k for k in shapes if k.startswith('ml_')}
        shapes = {k: ([1] + v[1:] if (k not in ('norm_final', 'rope_c', 'rope_s')) else v) for k, v in shapes.items() if k in need}
    c.w = {k: nc.dram_tensor(k, v, F32, kind="ExternalInput").ap() for k, v in shapes.items()}
    x = nc.dram_tensor("x", [Sq, D], F32, kind="ExternalInput").ap()
    y = nc.dram_tensor("y", [Sq, D], F32, kind="ExternalOutput").ap()
    res = nc.dram_tensor("res", [Sq, D], F32, kind="Internal").ap()
    c.x = x; c.y = y; c.res = res; c.scr = {}
    with contextlib.ExitStack() as es:
        s = S(nc)
        c.ident_bf = sb(es, nc, "ident_bf", [128, 128], BF16)
        c.ident_f = sb(es, nc, "ident_f", [128, 128], F32)
        c.ones_bf = sb(es, nc, "ones_bf", [128, 128], BF16)
        with contextlib.ExitStack() as es2:
            io = sb(es2, nc, "io_a", [128, 128], F32)
            ip = sb(es2, nc, "io_b", [128, 128], F32)
            s.op('pool', lambda e: e.iota(io[:], pattern=[[1, 128]], base=0, channel_multiplier=0,
                                          allow_small_or_imprecise_dtypes=True), writes=[io])
            s.op('pool', lambda e: e.iota(ip[:], pattern=[[0, 128]], base=0, channel_multiplier=1,
                                          allow_small_or_imprecise_dtypes=True), writes=[ip])
            s.op('dve', lambda e: e.tensor_tensor(out=c.ident_f[:], in0=io[:], in1=ip[:], op=ALU.is_equal),
                 reads=[io, ip], writes=[c.ident_f])
            s.op('dve', lambda e: e.tensor_copy(out=c.ident_bf[:], in_=c.ident_f[:]), reads=[c.ident_f], writes=[c.ident_bf])
            s.op('dve', lambda e: e.memset(c.ones_bf[:], 1.0), writes=[c.ones_bf])
            s.barrier()
        if 'peer' in kinds:
            nl = c.w['peer_u'].shape[0]
            c.tab_u = nc.dram_tensor("tab_u_bf", [nl * 16384, D], BF16, kind="Internal").ap()
            c.tab_v = nc.dram_tensor("tab_v_bf", [nl * 16384, D], BF16, kind="Internal").ap()
            for (srcn, dstt) in (('peer_u', c.tab_u), ('peer_v', c.tab_v)):
                fl = c.w[srcn].rearrange("l e d -> (l e) d")
                for r0 in range(0, nl * 16384, 8192):
                    s.dma('pool', lambda e: e.dma_start(out=dstt[r0:r0 + 8192, :], in_=fl[r0:r0 + 8192, :]))
            s.barrier()
        cur = x
        for (kind, li, j) in layers:
            if kind == 'peer':
                peer_layer(c, s, li, cur, res)
            elif kind == 'mla':
                mla_layer(c, s, li, j, cur, res)
            elif kind == 'mlstm':
                mlstm_layer(c, s, li, j, cur, res)
            cur = res
        if final:
            final_norm(c, s, cur, y)
        s.barrier()
        c.ninst = s.ninst
    return nc, c


def rope_tables(Sq):
    inv = 10000.0 ** (-np.arange(0, 64, 2, dtype=np.float32) / 64.0)
    ang = np.arange(Sq, dtype=np.float32)[:, None] * inv[None, :]
    cos = np.cos(ang).astype(np.float32).T
    sin = np.sin(ang).astype(np.float32).T
    return (np.ascontiguousarray(np.concatenate([cos, cos], 0)),
            np.ascontiguousarray(np.concatenate([-sin, sin], 0)))


FULL_LAYERS = [('mla', 0, 0), ('peer', 0, 0), ('mlstm', 1, 0), ('peer', 1, 0),
               ('mla', 2, 1), ('peer', 2, 0), ('mlstm', 3, 1), ('peer', 3, 0)]


def kernel(**inputs):
    Sq = 8192
    nc, c = build(Sq, FULL_LAYERS)
    rc, rs = rope_tables(Sq)
    wts = {k: np.ascontiguousarray(np.asarray(v, dtype=np.float32)) for k, v in inputs.items()
           if k not in ('x_prompt', 'x_sample')}
    wts['rope_c'] = rc; wts['rope_s'] = rs
    xs = [np.asarray(inputs['x_prompt'][0]), np.asarray(inputs['x_prompt'][1]), np.asarray(inputs['x_sample'][0])]
    in_maps = []
    for i in range(3):
        m = dict(wts); m['x'] = np.ascontiguousarray(xs[i], dtype=np.float32)
        in_maps.append(m)
    r = run_bass_kernel_spmd(nc, in_maps, core_ids=[0, 1, 2])
    ys = [np.asarray(r.results[i]['y'], dtype=np.float32) for i in range(3)]
    return (np.stack([ys[0], ys[1]], 0), ys[2][None])
```

```python
import contextlib
import numpy as np
import concourse.bass as bass
import concourse.mybir as mybir
from concourse.bass_utils import run_bass_kernel_spmd

F32 = mybir.dt.float32; BF16 = mybir.dt.bfloat16; I32 = mybir.dt.int32; U32 = mybir.dt.uint32
AF = mybir.ActivationFunctionType
ALU = mybir.AluOpType
AX = mybir.AxisListType
D = 2048
EPS = 1e-6
NEG = -1.0e30


class T:
    def __init__(self, h, name=""):
        self.h = h; self.name = name; self.w = None; self.r = []
    def __getitem__(self, idx):
        return self.h[idx]


class S:
    NDMA = 16
    def __init__(self, nc):
        self.nc = nc
        self.eng = {'pe': nc.tensor, 'act': nc.scalar, 'dve': nc.vector, 'pool': nc.gpsimd, 'sp': nc.sync}
        self.sem = {k: nc.alloc_semaphore("s_" + k) for k in self.eng}
        self.cnt = {k: 0 for k in self.eng}
        self.seen = {k: {} for k in self.eng}
        self.dsem = [nc.alloc_semaphore("d%d" % i) for i in range(self.NDMA)]
        self.dval = [0] * self.NDMA
        self.dnext = 0
        self.ninst = 0
    def _wait(self, eng, deps):
        need = {}
        for tok in deps:
            if tok is None: continue
            k, v = tok
            if v > need.get(k, 0): need[k] = v
        for k, v in need.items():
            if self.seen[eng].get(k, 0) >= v: continue
            sem = self.sem[k] if isinstance(k, str) else self.dsem[k]
            self.eng[eng].wait_ge(sem, v)
            self.seen[eng][k] = v
    def _deps(self, reads, writes):
        deps = []
        for t in reads: deps.append(t.w)
        for t in writes:
            deps.append(t.w); deps.extend(t.r)
        return deps
    def _commit(self, tok, reads, writes):
        for t in reads:
            t.r.append(tok)
            if len(t.r) > 64: t.r = t.r[-48:]
        for t in writes:
            t.w = tok; t.r = []
    def op(self, eng, fn, reads=(), writes=(), extra=()):
        self._wait(eng, self._deps(reads, writes) + list(extra))
        inst = fn(self.eng[eng])
        self.cnt[eng] += 1; self.ninst += 1
        inst.then_inc(self.sem[eng], 1)
        tok = (eng, self.cnt[eng])
        self._commit(tok, reads, writes)
        return tok
    def dma(self, q, fn, reads=(), writes=(), extra=()):
        j = self.dnext; self.dnext = (self.dnext + 1) % self.NDMA
        deps = self._deps(reads, writes) + list(extra)
        if self.dval[j] > 0: deps.append((j, self.dval[j]))
        self._wait(q, deps)
        inst = fn(self.eng[q])
        self.dval[j] += 16; self.ninst += 1
        inst.then_inc(self.dsem[j], 16)
        tok = (j, self.dval[j])
        self._commit(tok, reads, writes)
        return tok
    def barrier(self):
        allt = [(k, self.cnt[k]) for k in self.eng if self.cnt[k] > 0]
        allt += [(j, self.dval[j]) for j in range(self.NDMA) if self.dval[j] > 0]
        for e in self.eng:
            self._wait(e, allt)


class Ctx:
    pass


_UNIQ = [0]


def sb(es, nc, name, shape, dt):
    _UNIQ[0] += 1
    name = "%s_%d" % (name, _UNIQ[0])
    return T(es.enter_context(nc.sbuf_tensor(name, shape, dt)), name)


def psb(es, nc, name, shape, dt):
    _UNIQ[0] += 1
    name = "%s_%d" % (name, _UNIQ[0])
    return T(es.enter_context(nc.psum_tensor(name, shape, dt)), name)


def load_cast(s, dst, dst_ap, src_ap, q='pool'):
    return s.dma(q, lambda e: e.dma_start(out=dst_ap, in_=src_ap), writes=[dst])


def norm_tile(c, s, src_ap, xt, gB, xn, small, junk):
    ss, rs = small
    s.dma('sp', lambda e: e.dma_start(out=xt[:], in_=src_ap), writes=[xt])
    s.op('act', lambda e: e.activation(out=junk[:], in_=xt[:], func=AF.Square, accum_out=ss[:, 0:1]),
         reads=[xt], writes=[junk, ss])
    s.op('dve', lambda e: e.tensor_scalar(out=rs[:, 0:1], in0=ss[:, 0:1], scalar1=1.0 / D, scalar2=EPS,
                                          op0=ALU.mult, op1=ALU.add), reads=[ss], writes=[rs])
    s.op('act', lambda e: e.activation(out=rs[:, 1:2], in_=rs[:, 0:1], func=AF.Sqrt), reads=[rs], writes=[rs])
    s.op('dve', lambda e: e.reciprocal(out=rs[:, 2:3], in_=rs[:, 1:2]), reads=[rs], writes=[rs])
    s.op('dve', lambda e: e.scalar_tensor_tensor(out=xn[:], in0=xt[:], scalar=rs[:, 2:3], in1=gB[:],
                                                 op0=ALU.mult, op1=ALU.mult), reads=[xt, rs, gB], writes=[xn])


def transpose_tile(c, s, xn, xnT, col0, pst, nchunk=16):
    for g in range(nchunk // 4):
        p = pst[g % len(pst)]
        for i in range(4):
            ch = g * 4 + i
            s.op('pe', lambda e: e.transpose(out=p[:, i * 128:(i + 1) * 128], in_=xn[:, ch * 128:(ch + 1) * 128],
                                             identity=c.ident_bf[:]), reads=[xn, c.ident_bf], writes=[p])
        eng = 'act' if g % 2 == 0 else 'dve'
        if eng == 'act':
            s.op('act', lambda e: e.copy(out=xnT[:, g * 4:g * 4 + 4, col0:col0 + 128],
                                         in_=p[:, 0:512].rearrange("p (a b) -> p a b", a=4)), reads=[p], writes=[xnT])
        else:
            s.op('dve', lambda e: e.tensor_copy(out=xnT[:, g * 4:g * 4 + 4, col0:col0 + 128],
                                                in_=p[:, 0:512].rearrange("p (a b) -> p a b", a=4)), reads=[p], writes=[xnT])


def load_gain(c, s, gB, vec_ap):
    s.dma('sp', lambda e: e.dma_start(out=gB[:], in_=vec_ap.partition_broadcast(128)), writes=[gB])


def final_norm(c, s, src, dst):
    nc = c.nc
    with contextlib.ExitStack() as es:
        gB = sb(es, nc, "fn_gB", [128, D], F32)
        xts = [sb(es, nc, "fn_xt%d" % i, [128, D], F32) for i in range(2)]
        xos = [sb(es, nc, "fn_xo%d" % i, [128, D], F32) for i in range(2)]
        junk = sb(es, nc, "fn_junk", [128, D], BF16)
        smalls = [(sb(es, nc, "fn_ss%d" % i, [128, 1], F32), sb(es, nc, "fn_rs%d" % i, [128, 4], F32)) for i in range(2)]
        load_gain(c, s, gB, c.w['norm_final'])
        for t in range(c.NT):
            xt = xts[t % 2]; xo = xos[t % 2]
            norm_tile(c, s, src[t * 128:(t + 1) * 128, :], xt, gB, xo, smalls[t % 2], junk)
            s.dma('sp', lambda e: e.dma_start(out=dst[t * 128:(t + 1) * 128, :], in_=xo[:]), reads=[xo])
        s.barrier()


def peer_layer(c, s, li, src, dst):
    nc = c.nc
    NH, KK = 8, 16
    with contextlib.ExitStack() as es:
        gB = sb(es, nc, "pr_gB", [128, D], F32)
        wq = sb(es, nc, "pr_wq", [128, 16, D], BF16)
        KT = sb(es, nc, "pr_KT", [128, 16, 128], BF16)
        kst = sb(es, nc, "pr_kst", [128, 128], F32)
        xts = [sb(es, nc, "pr_xt%d" % i, [128, D], F32) for i in range(2)]
        xn = sb(es, nc, "pr_xn", [128, D], BF16)
        junk = sb(es, nc, "pr_junk", [128, D], BF16)
        small = (sb(es, nc, "pr_ss", [128, 1], F32), sb(es, nc, "pr_rs", [128, 4], F32))
        xnT = sb(es, nc, "pr_xnT", [128, 16, 128], BF16)
        qT = sb(es, nc, "pr_qT", [128, 16, 128], BF16)
        NUG = 4
        ug = [sb(es, nc, "pr_ug%d" % i, [128, D], BF16) for i in range(NUG)]
        vg = [sb(es, nc, "pr_vg%d" % i, [128, D], BF16) for i in range(NUG)]
        wexp = sb(es, nc, "pr_wexp", [128, 128 * 128], BF16)
        s16 = sb(es, nc, "pr_s16", [128, 16, 16], F32)
        i16 = sb(es, nc, "pr_i16", [128, 16, 16], U32)
        i16f = sb(es, nc, "pr_i16f", [128, 16, 16], F32)
        srep = sb(es, nc, "pr_srep", [128, 128], F32)
        comb = sb(es, nc, "pr_comb", [128, 8, 256], F32)
        comb2 = sb(es, nc, "pr_comb2", [128, 256], F32)
        ct = sb(es, nc, "pr_ct", [128, 8, 16], F32)
        ci = sb(es, nc, "pr_ci", [128, 8, 16], U32)
        k1 = sb(es, nc, "pr_k1", [128, 128], U32)
        k2 = sb(es, nc, "pr_k2", [128, 128], U32)
        k1f = sb(es, nc, "pr_k1f", [128, 128], F32)
        k2f = sb(es, nc, "pr_k2f", [128, 128], F32)
        oh = sb(es, nc, "pr_oh", [128, 128, 16], F32)
        i1f = sb(es, nc, "pr_i1f", [128, 128], F32)
        i2f = sb(es, nc, "pr_i2f", [128, 128], F32)
        ef = sb(es, nc, "pr_ef", [128, 128], F32)
        eu = sb(es, nc, "pr_eu", [128, 128], I32)
        evTs = [sb(es, nc, "pr_evT%d" % i, [128, 128], I32) for i in range(2)]
        gate = sb(es, nc, "pr_gate", [128, 8, 16], F32)
        gsum = sb(es, nc, "pr_gsum", [128, 8], F32)
        av = sb(es, nc, "pr_av", [128, 128], F32)
        g1 = sb(es, nc, "pr_g1", [128, 128], F32)
        g2 = sb(es, nc, "pr_g2", [128, 128], F32)
        wv = sb(es, nc, "pr_wv", [128, 128], F32)
        wT = sb(es, nc, "pr_wT", [128, 128], BF16)
        iota16 = sb(es, nc, "pr_iota16", [128, 16], F32)
        ps_s = [psb(es, nc, "pr_ps%d" % i, [128, 512], F32) for i in range(2)]
        ps_t = [psb(es, nc, "pr_pt%d" % i, [128, 1024], BF16) for i in range(2)]
        ps_o = [psb(es, nc, "pr_po%d" % i, [128, 512], F32) for i in range(4)]

        load_gain(c, s, gB, c.w['norm_ffn'][li])
        wsrc = c.w['peer_w_query'][li].rearrange("(c p) n -> p c n", p=128)
        for c0 in range(0, 16, 2):
            load_cast(s, wq, wq[:, c0:c0 + 2, :], wsrc[:, c0:c0 + 2, :])
        for hp in range(16):
            s.dma('sp', lambda e: e.dma_start(out=kst[:], in_=c.w['peer_sub_keys'][li, hp // 2, hp % 2]), writes=[kst])
            p = ps_s[hp % 2]
            s.op('pe', lambda e: e.transpose(out=p[:, 0:128], in_=kst[:], identity=c.ident_f[:]),
                 reads=[kst, c.ident_f], writes=[p])
            s.op('dve', lambda e: e.tensor_copy(out=KT[:, hp, :], in_=p[:, 0:128]), reads=[p], writes=[KT])
        s.op('pool', lambda e: e.iota(iota16[:], pattern=[[1, 16]], base=0, channel_multiplier=0,
                                      allow_small_or_imprecise_dtypes=True), writes=[iota16])
        s.op('dve', lambda e: e.memset(wexp[:], 0.0), writes=[wexp])
        utab = c.tab_u
        vtab = c.tab_v

        def stage_A(t):
            xt = xts[t % 2]; evT = evTs[t % 2]
            rows = slice(t * 128, (t + 1) * 128)
            norm_tile(c, s, src[rows, :], xt, gB, xn, small, junk)
            yield
            transpose_tile(c, s, xn, xnT, 0, ps_t)
            yield
            for hp in range(16):
                p = ps_s[hp % 2]
                for kc in range(16):
                    s.op('pe', lambda e: e.matmul(p[:, 0:128], lhsT=wq[:, kc, hp * 128:(hp + 1) * 128], rhs=xnT[:, kc, :],
                                                  start=(kc == 0), stop=(kc == 15)), reads=[wq, xnT], writes=[p])
                if hp % 2 == 0:
                    s.op('act', lambda e: e.copy(out=qT[:, hp, :], in_=p[:, 0:128]), reads=[p], writes=[qT])
                else:
                    s.op('dve', lambda e: e.tensor_copy(out=qT[:, hp, :], in_=p[:, 0:128]), reads=[p], writes=[qT])
                yield
            for hp in range(16):
                p = ps_s[hp % 2]
                s.op('pe', lambda e: e.matmul(p[:, 0:128], lhsT=qT[:, hp, :], rhs=KT[:, hp, :], start=True, stop=True),
                     reads=[qT, KT], writes=[p])
                s.op('dve', lambda e: e.tensor_copy(out=srep[:], in_=p[:, 0:128]), reads=[p], writes=[srep])
                s.op('dve', lambda e: e.max(out=s16[:, hp, 0:8], in_=srep[:]), reads=[srep], writes=[s16])
                s.op('dve', lambda e: e.max_index(out=i16[:, hp, 0:8], in_max=s16[:, hp, 0:8], in_values=srep[:]),
                     reads=[srep, s16], writes=[i16])
                yield
                s.op('dve', lambda e: e.match_replace(out=srep[:], in_to_replace=s16[:, hp, 0:8], in_values=srep[:],
                                                      imm_value=NEG), reads=[s16], writes=[srep])
                s.op('dve', lambda e: e.max(out=s16[:, hp, 8:16], in_=srep[:]), reads=[srep], writes=[s16])
                s.op('dve', lambda e: e.max_index(out=i16[:, hp, 8:16], in_max=s16[:, hp, 8:16], in_values=srep[:]),
                     reads=[srep, s16], writes=[i16])
                yield
            s.op('dve', lambda e: e.tensor_copy(out=i16f[:], in_=i16[:]), reads=[i16], writes=[i16f])
            for h in range(NH):
                s.op('dve', lambda e: e.tensor_tensor(
                    out=comb[:, h, :].rearrange("p (a b) -> p a b", a=16),
                    in0=s16[:, 2 * h, :].unsqueeze(2).to_broadcast([128, 16, 16]),
                    in1=s16[:, 2 * h + 1, :].unsqueeze(1).to_broadcast([128, 16, 16]), op=ALU.add),
                    reads=[s16], writes=[comb])
                s.op('dve', lambda e: e.max(out=ct[:, h, 0:8], in_=comb[:, h, :]), reads=[comb], writes=[ct])
                s.op('dve', lambda e: e.max_index(out=ci[:, h, 0:8], in_max=ct[:, h, 0:8], in_values=comb[:, h, :]),
                     reads=[comb, ct], writes=[ci])
                yield
                s.op('dve', lambda e: e.match_replace(out=comb2[:], in_to_replace=ct[:, h, 0:8], in_values=comb[:, h, :],
                                                      imm_value=NEG), reads=[ct, comb], writes=[comb2])
                s.op('dve', lambda e: e.max(out=ct[:, h, 8:16], in_=comb2[:]), reads=[comb2], writes=[ct])
                s.op('dve', lambda e: e.max_index(out=ci[:, h, 8:16], in_max=ct[:, h, 8:16], in_values=comb2[:]),
                     reads=[comb2, ct], writes=[ci])
                yield
            s.op('dve', lambda e: e.tensor_tensor(out=gate[:], in0=ct[:], in1=ct[:, :, 0:1].to_broadcast([128, 8, 16]),
                                                  op=ALU.subtract), reads=[ct], writes=[gate])
            s.op('act', lambda e: e.activation(out=gate[:], in_=gate[:], func=AF.Exp), reads=[gate], writes=[gate])
            s.op('dve', lambda e: e.tensor_reduce(out=gsum[:], in_=gate[:], axis=AX.X, op=ALU.add), reads=[gate], writes=[gsum])
            s.op('dve', lambda e: e.reciprocal(out=gsum[:], in_=gsum[:]), reads=[gsum], writes=[gsum])
            s.op('dve', lambda e: e.tensor_tensor(out=gate[:], in0=gate[:], in1=gsum[:].unsqueeze(2).to_broadcast([128, 8, 16]),
                                                  op=ALU.mult), reads=[gate, gsum], writes=[gate])
            yield
            cif = ci[:].rearrange("p h k -> p (h k)")
            s.op('dve', lambda e: e.tensor_single_scalar(out=k1[:], in_=cif, scalar=4, op=ALU.logical_shift_right),
                 reads=[ci], writes=[k1])
            s.op('dve', lambda e: e.tensor_single_scalar(out=k2[:], in_=cif, scalar=15, op=ALU.bitwise_and),
                 reads=[ci], writes=[k2])
            s.op('dve', lambda e: e.tensor_copy(out=k1f[:], in_=k1[:]), reads=[k1], writes=[k1f])
            s.op('dve', lambda e: e.tensor_copy(out=k2f[:], in_=k2[:]), reads=[k2], writes=[k2f])
            yield
            for (kf, pi, of) in ((k1f, 0, i1f), (k2f, 1, i2f)):
                s.op('dve', lambda e: e.tensor_tensor(out=oh[:], in0=kf[:].unsqueeze(2).to_broadcast([128, 128, 16]),
                                                      in1=iota16[:].unsqueeze(1).to_broadcast([128, 128, 16]),
                                                      op=ALU.is_equal), reads=[kf, iota16], writes=[oh])
                i16v = i16f[:].rearrange("p (h q) k -> p h q k", q=2)[:, :, pi, :]
                s.op('dve', lambda e: e.tensor_tensor(out=oh[:].rearrange("p (h k) j -> p h k j", h=8),
                                                      in0=oh[:].rearrange("p (h k) j -> p h k j", h=8),
                                                      in1=i16v.unsqueeze(2).to_broadcast([128, 8, 16, 16]),
                                                      op=ALU.mult), reads=[oh, i16f], writes=[oh])
                s.op('dve', lambda e: e.tensor_reduce(out=of[:], in_=oh[:], axis=AX.X, op=ALU.add), reads=[oh], writes=[of])
                yield
            s.op('dve', lambda e: e.scalar_tensor_tensor(out=ef[:], in0=i1f[:], scalar=128.0, in1=i2f[:],
                                                         op0=ALU.mult, op1=ALU.add), reads=[i1f, i2f], writes=[ef])
            if li > 0:
                s.op('dve', lambda e: e.tensor_scalar(out=ef[:], in0=ef[:], scalar1=float(li * 16384), scalar2=None, op0=ALU.add),
                     reads=[ef], writes=[ef])
            s.op('dve', lambda e: e.tensor_copy(out=eu[:], in_=ef[:]), reads=[ef], writes=[eu])
            yield

        def stage_A_tail(t):
            evT = evTs[t % 2]
            p = ps_s[0]
            s.op('pe', lambda e: e.transpose(out=p[:, 0:128], in_=ef[:], identity=c.ident_f[:]), reads=[ef, c.ident_f], writes=[p])
            s.op('dve', lambda e: e.tensor_copy(out=evT[:], in_=p[:, 0:128]), reads=[p], writes=[evT])

        def stage_U(t):
            for k in range(128):
                g = ug[k % NUG]
                s.dma('pool', lambda e: e.indirect_dma_start(out=g[:], out_offset=None, in_=utab,
                                                             in_offset=bass.IndirectOffsetOnAxis(ap=eu[:, k:k + 1], axis=0)),
                      reads=[eu], writes=[g])
                s.op('dve', lambda e: e.scalar_tensor_tensor(out=junk[:], in0=g[:], scalar=1.0, in1=xn[:], op0=ALU.mult,
                                                             op1=ALU.mult, accum_out=av[:, k:k + 1]),
                     reads=[g, xn], writes=[junk, av])
            s.op('dve', lambda e: e.tensor_tensor(out=g1[:], in0=av[:], in1=av[:], op=ALU.mult), reads=[av], writes=[g1])
            s.op('dve', lambda e: e.tensor_scalar(out=g1[:], in0=g1[:], scalar1=0.044715 * 0.7978845608028654,
                                                  scalar2=0.7978845608028654, op0=ALU.mult, op1=ALU.add), reads=[g1], writes=[g1])
            s.op('dve', lambda e: e.tensor_tensor(out=g1[:], in0=g1[:], in1=av[:], op=ALU.mult), reads=[g1, av], writes=[g1])
            s.op('act', lambda e: e.activation(out=g2[:], in_=g1[:], func=AF.Tanh), reads=[g1], writes=[g2])
            s.op('dve', lambda e: e.tensor_scalar(out=g2[:], in0=g2[:], scalar1=1.0, scalar2=0.5, op0=ALU.add, op1=ALU.mult),
                 reads=[g2], writes=[g2])
            s.op('dve', lambda e: e.tensor_tensor(out=g2[:], in0=g2[:], in1=av[:], op=ALU.mult), reads=[g2, av], writes=[g2])
            s.op('dve', lambda e: e.tensor_tensor(out=wv[:], in0=g2[:], in1=gate[:].rearrange("p h k -> p (h k)"), op=ALU.mult),
                 reads=[g2, gate], writes=[wv])
            p = ps_s[1]
            s.op('pe', lambda e: e.transpose(out=p[:, 0:128], in_=wv[:], identity=c.ident_f[:]), reads=[wv, c.ident_f], writes=[p])
            s.op('dve', lambda e: e.tensor_copy(out=wexp[:, 0:128 * 128:129], in_=p[:, 0:128]), reads=[p], writes=[wexp])

        def stage_V(t, gen):
            xt = xts[t % 2]; evT = evTs[t % 2]
            rows = slice(t * 128, (t + 1) * 128)
            for tk in range(128):
                g = vg[tk % NUG]
                s.dma('pool', lambda e: e.indirect_dma_start(out=g[:], out_offset=None, in_=vtab,
                                                             in_offset=bass.IndirectOffsetOnAxis(ap=evT[:, tk:tk + 1], axis=0)),
                      reads=[evT], writes=[g])
                for nb in range(4):
                    s.op('pe', lambda e: e.matmul(ps_o[nb][:], lhsT=wexp[:, tk * 128:(tk + 1) * 128],
                                                  rhs=g[:, nb * 512:(nb + 1) * 512], start=(tk == 0), stop=(tk == 127)),
                         reads=[wexp, g], writes=[ps_o[nb]])
                if gen is not None:
                    next(gen, None)
            if gen is not None:
                for _ in gen:
                    pass
            for nb in range(4):
                s.op('dve', lambda e: e.tensor_tensor(out=xt[:, nb * 512:(nb + 1) * 512], in0=ps_o[nb][:],
                                                      in1=xt[:, nb * 512:(nb + 1) * 512], op=ALU.add),
                     reads=[ps_o[nb], xt], writes=[xt])
            s.dma('sp', lambda e: e.dma_start(out=dst[rows, :], in_=xt[:]), reads=[xt])

        for _ in stage_A(0):
            pass
        stage_A_tail(0)
        stage_U(0)
        for t in range(c.NT):
            stage_V(t, stage_A(t + 1) if t + 1 < c.NT else None)
            if t + 1 < c.NT:
                stage_A_tail(t + 1)
                stage_U(t + 1)
        s.barrier()


def scr(c, name, shape, dt):
    if name not in c.scr:
        kind = "ExternalOutput" if (getattr(c, 'dbg', False) and name in ('hf_scr', 'hn_scr', 'g_scr', 'b_scr', 'ao_scr')) else "Internal"
        c.scr[name] = c.nc.dram_tensor(name, shape, dt, kind=kind).ap()
    return c.scr[name]


def evac(s, i, out_ap, in_ap, reads, writes):
    if i % 2 == 0:
        s.op('act', lambda e: e.copy(out=out_ap, in_=in_ap), reads=reads, writes=writes)
    else:
        s.op('dve', lambda e: e.tensor_copy(out=out_ap, in_=in_ap), reads=reads, writes=writes)


def mla_layer(c, s, li, j, src, dst):
    import os
    nc = c.nc; Sq = c.S; NB = Sq // 512; NT = c.NT
    scale = 192.0 ** -0.5
    qn_scr = scr(c, "qn_scr", [16, 128, Sq], BF16)
    qr_scr = scr(c, "qr_scr", [16, 64, Sq], BF16)
    ao_scr = scr(c, "ao_scr", [16, 128, Sq], BF16)
    ckv_scr = scr(c, "ckv_scr", [2, 128, Sq], BF16)
    kr_scr = scr(c, "kr_scr", [64, Sq], BF16)
    with contextlib.ExitStack() as es:
        gB = sb(es, nc, "m1_gB", [128, D], F32)
        w_in = sb(es, nc, "m1_win", [128, 16, 832], BF16)
        w_krsw = sb(es, nc, "m1_wkrsw", [128, 16, 64], BF16)
        wqu = sb(es, nc, "m1_wqu", [128, 4, 3072], BF16)
        wqsw = sb(es, nc, "m1_wqsw", [128, 4, 16, 64], BF16)
        gq = sb(es, nc, "m1_gq", [128, 4], F32)
        gkv = sb(es, nc, "m1_gkv", [128, 2], F32)
        xt = sb(es, nc, "m1_xt", [128, D], F32)
        xn = sb(es, nc, "m1_xn", [128, D], BF16)
        junk = sb(es, nc, "m1_junk", [128, D], BF16)
        small = (sb(es, nc, "m1_ss", [128, 1], F32), sb(es, nc, "m1_rs", [128, 4], F32))
        xnT = sb(es, nc, "m1_xnT", [128, 16, 512], BF16)
        raw = sb(es, nc, "m1_raw", [128, 6, 512], F32)
        sq = sb(es, nc, "m1_sq", [128, 6, 512], BF16)
        tq = sb(es, nc, "m1_tq", [128, 512], F32)
        tkv = sb(es, nc, "m1_tkv", [128, 512], F32)
        cqn = sb(es, nc, "m1_cqn", [128, 4, 512], BF16)
        ckvn = sb(es, nc, "m1_ckvn", [128, 2, 512], BF16)
        krn = sb(es, nc, "m1_krn", [64, 512], BF16)
        CC = sb(es, nc, "m1_CC", [64, 512], F32); SS = sb(es, nc, "m1_SS", [64, 512], F32)
        CCq = sb(es, nc, "m1_CCq", [64, 512], F32); SSq = sb(es, nc, "m1_SSq", [64, 512], F32)
        t1 = sb(es, nc, "m1_t1", [64, 512], F32); t2 = sb(es, nc, "m1_t2", [64, 512], F32)
        qst = [sb(es, nc, "m1_qst%d" % i, [128, 512], BF16) for i in range(2)]
        qrst = [sb(es, nc, "m1_qrst%d" % i, [64, 512], BF16) for i in range(2)]
        ps_t = [psb(es, nc, "m1_pt%d" % i, [128, 1024], BF16) for i in range(2)]
        ps_m = [psb(es, nc, "m1_pm%d" % i, [128, 512], F32) for i in range(4)]
        ps_r = [psb(es, nc, "m1_pr%d" % i, [128, 512], F32) for i in range(2)]

        load_gain(c, s, gB, c.w['norm_mix'][li])
        wsrc = c.w['mla_w_in'][j].rearrange("(c p) n -> p c n", p=128)
        for c0 in range(0, 16, 4):
            load_cast(s, w_in, w_in[:, c0:c0 + 4, :], wsrc[:, c0:c0 + 4, :])
        load_cast(s, w_krsw, w_krsw[:, :, 0:32], wsrc[:, :, 800:832])
        load_cast(s, w_krsw, w_krsw[:, :, 32:64], wsrc[:, :, 768:800])
        qsrc = c.w['mla_w_q_up'][j].rearrange("(c p) n -> p c n", p=128)
        q5 = c.w['mla_w_q_up'][j].rearrange("(c p) (h f) -> p c h f", p=128, f=192)
        for cc in range(4):
            for h0 in (0, 1536):
                load_cast(s, wqu, wqu[:, cc, h0:h0 + 1536], qsrc[:, cc, h0:h0 + 1536])
            load_cast(s, wqsw, wqsw[:, cc, :, 0:32], q5[:, cc, :, 160:192])
            load_cast(s, wqsw, wqsw[:, cc, :, 32:64], q5[:, cc, :, 128:160])
        s.dma('sp', lambda e: e.dma_start(out=gq[:], in_=c.w['mla_q_norm'][j].rearrange("(c p) -> p c", p=128),
                                          allow_slow_non_contiguous=True), writes=[gq])
        s.dma('sp', lambda e: e.dma_start(out=gkv[:], in_=c.w['mla_kv_norm'][j].rearrange("(c p) -> p c", p=128),
                                          allow_slow_non_contiguous=True), writes=[gkv])
        STG = os.environ.get('MLA_STG', 'Z')
        if STG == 'A':
            s.barrier(); return
        for b in range(NB):
            cols = slice(b * 512, (b + 1) * 512)
            for jt in range(4):
                rows = slice(b * 512 + jt * 128, b * 512 + (jt + 1) * 128)
                norm_tile(c, s, src[rows, :], xt, gB, xn, small, junk)
                transpose_tile(c, s, xn, xnT, jt * 128, ps_t)
            s.dma('sp', lambda e: e.dma_start(out=CC[:], in_=c.w['rope_c'][:, cols]), writes=[CC])
            s.dma('sp', lambda e: e.dma_start(out=SS[:], in_=c.w['rope_s'][:, cols]), writes=[SS])
            s.op('dve', lambda e: e.tensor_scalar(out=CCq[:], in0=CC[:], scalar1=scale, scalar2=None, op0=ALU.mult),
                 reads=[CC], writes=[CCq])
            s.op('dve', lambda e: e.tensor_scalar(out=SSq[:], in0=SS[:], scalar1=scale, scalar2=None, op0=ALU.mult),
                 reads=[SS], writes=[SSq])
            if STG == 'B':
                s.barrier(); return
            for fb in range(6):
                p = ps_m[fb % 4]
                for kc in range(16):
                    s.op('pe', lambda e: e.matmul(p[:], lhsT=w_in[:, kc, fb * 128:(fb + 1) * 128], rhs=xnT[:, kc, :],
                                                  start=(kc == 0), stop=(kc == 15)), reads=[w_in, xnT], writes=[p])
                s.op('dve', lambda e: e.tensor_copy(out=raw[:, fb, :], in_=p[:]), reads=[p], writes=[raw])
                s.op('act', lambda e: e.activation(out=sq[:, fb, :], in_=raw[:, fb, :], func=AF.Square), reads=[raw], writes=[sq])
            if STG == 'C':
                s.barrier(); return
            for (wt, lo, pr) in ((w_in, 768, ps_r[0]), (w_krsw, 0, ps_r[1])):
                for kc in range(16):
                    s.op('pe', lambda e: e.matmul(pr[0:64, :], lhsT=wt[:, kc, lo:lo + 64], rhs=xnT[:, kc, :],
                                                  start=(kc == 0), stop=(kc == 15)), reads=[wt, xnT], writes=[pr])
            s.op('dve', lambda e: e.tensor_tensor(out=t1[:], in0=ps_r[0][0:64, :], in1=CC[:], op=ALU.mult),
                 reads=[ps_r[0], CC], writes=[t1])
            s.op('dve', lambda e: e.tensor_tensor(out=t2[:], in0=ps_r[1][0:64, :], in1=SS[:], op=ALU.mult),
                 reads=[ps_r[1], SS], writes=[t2])
            s.op('dve', lambda e: e.tensor_tensor(out=krn[:], in0=t1[:], in1=t2[:], op=ALU.add), reads=[t1, t2], writes=[krn])
            s.dma('sp', lambda e: e.dma_start(out=kr_scr[:, cols], in_=krn[:]), reads=[krn])
            if STG == 'D':
                s.barrier(); return
            for (f0, nf, tt, n) in ((0, 4, tq, 512.0), (4, 2, tkv, 256.0)):
                p = ps_m[0 if f0 == 0 else 1]
                for i in range(nf):
                    s.op('pe', lambda e: e.matmul(p[:], lhsT=c.ones_bf[:], rhs=sq[:, f0 + i, :], start=(i == 0), stop=(i == nf - 1)),
                         reads=[c.ones_bf, sq], writes=[p])
                s.op('dve', lambda e: e.tensor_scalar(out=tt[:], in0=p[:], scalar1=1.0 / n, scalar2=EPS, op0=ALU.mult, op1=ALU.add),
                     reads=[p], writes=[tt])
                s.op('act', lambda e: e.activation(out=tt[:], in_=tt[:], func=AF.Ln), reads=[tt], writes=[tt])
                s.op('act', lambda e: e.activation(out=tt[:], in_=tt[:], func=AF.Exp, scale=-0.5), reads=[tt], writes=[tt])
            for fb in range(4):
                s.op('dve', lambda e: e.scalar_tensor_tensor(out=cqn[:, fb, :], in0=raw[:, fb, :], scalar=gq[:, fb:fb + 1], in1=tq[:],
                                                             op0=ALU.mult, op1=ALU.mult), reads=[raw, gq, tq], writes=[cqn])
            for fb in range(2):
                s.op('dve', lambda e: e.scalar_tensor_tensor(out=ckvn[:, fb, :], in0=raw[:, 4 + fb, :], scalar=gkv[:, fb:fb + 1],
                                                             in1=tkv[:], op0=ALU.mult, op1=ALU.mult), reads=[raw, gkv, tkv], writes=[ckvn])
            s.dma('sp', lambda e: e.dma_start(out=ckv_scr.rearrange("c p s -> p c s")[:, :, cols], in_=ckvn[:]), reads=[ckvn])
            if STG == 'E':
                s.barrier(); return
            for h in range(16):
                p = ps_m[h % 4]
                for kc in range(4):
                    s.op('pe', lambda e: e.matmul(p[:], lhsT=wqu[:, kc, h * 192:h * 192 + 128], rhs=cqn[:, kc, :],
                                                  start=(kc == 0), stop=(kc == 3)), reads=[wqu, cqn], writes=[p])
                qs = qst[h % 2]
                s.op('act', lambda e: e.mul(out=qs[:], in_=p[:], mul=scale), reads=[p], writes=[qs])
                s.dma('sp', lambda e: e.dma_start(out=qn_scr[h, :, cols], in_=qs[:]), reads=[qs])
                for kc in range(4):
                    s.op('pe', lambda e: e.matmul(ps_r[0][0:64, :], lhsT=wqu[:, kc, h * 192 + 128:h * 192 + 192], rhs=cqn[:, kc, :],
                                                  start=(kc == 0), stop=(kc == 3)), reads=[wqu, cqn], writes=[ps_r[0]])
                for kc in range(4):
                    s.op('pe', lambda e: e.matmul(ps_r[1][0:64, :], lhsT=wqsw[:, kc, h, :], rhs=cqn[:, kc, :],
                                                  start=(kc == 0), stop=(kc == 3)), reads=[wqsw, cqn], writes=[ps_r[1]])
                s.op('dve', lambda e: e.tensor_tensor(out=t1[:], in0=ps_r[0][0:64, :], in1=CCq[:], op=ALU.mult),
                     reads=[ps_r[0], CCq], writes=[t1])
                s.op('dve', lambda e: e.tensor_tensor(out=t2[:], in0=ps_r[1][0:64, :], in1=SSq[:], op=ALU.mult),
                     reads=[ps_r[1], SSq], writes=[t2])
                qr = qrst[h % 2]
                s.op('dve', lambda e: e.tensor_tensor(out=qr[:], in0=t1[:], in1=t2[:], op=ALU.add), reads=[t1, t2], writes=[qr])
                s.dma('sp', lambda e: e.dma_start(out=qr_scr[h, :, cols], in_=qr[:]), reads=[qr])
        s.barrier()
    import os
    if os.environ.get('MLA_STOP') == '1': return
    with contextlib.ExitStack() as es:
        wkv = sb(es, nc, "m2_wkv", [128, 2, 4096], BF16)
        ckvT = sb(es, nc, "m2_ckvT", [128, 2, Sq], BF16)
        krT = sb(es, nc, "m2_krT", [64, Sq], BF16)
        knT = [sb(es, nc, "m2_knT%d" % i, [128, Sq], BF16) for i in range(2)]
        Vh = [sb(es, nc, "m2_Vh%d" % i, [128, NT, 128], BF16) for i in range(2)]
        qn = [sb(es, nc, "m2_qn%d" % i, [128, 512], BF16) for i in range(2)]
        qr = [sb(es, nc, "m2_qr%d" % i, [64, 512], BF16) for i in range(2)]
        pT = [sb(es, nc, "m2_pT%d" % i, [128, 512], BF16) for i in range(3)]
        rden = sb(es, nc, "m2_rden", [128, 512], F32)
        ao = [sb(es, nc, "m2_ao%d" % i, [128, 512], BF16) for i in range(2)]
        ps_s = [psb(es, nc, "m2_ps%d" % i, [128, 512], F32) for i in range(3)]
        ps_o = psb(es, nc, "m2_po", [128, 512], F32)
        ps_d = psb(es, nc, "m2_pd", [128, 512], F32)
        ps_b = [psb(es, nc, "m2_pb%d" % i, [128, 512], F32) for i in range(2)]
        ksrc = c.w['mla_w_kv_up'][j].rearrange("(c p) n -> p c n", p=128)
        for cc in range(2):
            for h0 in (0, 2048):
                load_cast(s, wkv, wkv[:, cc, h0:h0 + 2048], ksrc[:, cc, h0:h0 + 2048])
            s.dma('sp', lambda e: e.dma_start(out=ckvT[:, cc, :], in_=ckv_scr[cc]), writes=[ckvT])
        s.dma('sp', lambda e: e.dma_start(out=krT[:], in_=kr_scr), writes=[krT])
        ev = 0
        for h in range(16):
            kb = knT[h % 2]; vb = Vh[h % 2]
            for b in range(NB):
                cols = slice(b * 512, (b + 1) * 512)
                p = ps_b[b % 2]
                for cc in range(2):
                    s.op('pe', lambda e: e.matmul(p[:], lhsT=wkv[:, cc, h * 256:h * 256 + 128], rhs=ckvT[:, cc, cols],
                                                  start=(cc == 0), stop=(cc == 1)), reads=[wkv, ckvT], writes=[p])
                ev += 1
                evac(s, ev, kb[:, cols], p[:], [p], [kb])
            for g in range(NT // 4):
                p = ps_b[g % 2]
                for i in range(4):
                    tl = g * 4 + i
                    for cc in range(2):
                        s.op('pe', lambda e: e.matmul(p[:, i * 128:(i + 1) * 128], lhsT=ckvT[:, cc, tl * 128:(tl + 1) * 128],
                                                      rhs=wkv[:, cc, h * 256 + 128:h * 256 + 256], start=(cc == 0), stop=(cc == 1)),
                             reads=[wkv, ckvT], writes=[p])
                ev += 1
                evac(s, ev, vb[:, g * 4:(g + 1) * 4, :], p[:].rearrange("p (a b) -> p a b", a=4), [p], [vb])
            for qb in range(NB):
                qcols = slice(qb * 512, (qb + 1) * 512)
                qnb = qn[qb % 2]; qrb = qr[qb % 2]
                s.dma('sp', lambda e: e.dma_start(out=qnb[:], in_=qn_scr[h, :, qcols]), writes=[qnb])
                s.dma('sp', lambda e: e.dma_start(out=qrb[:], in_=qr_scr[h, :, qcols]), writes=[qrb])

                def scores(kt):
                    p = ps_s[kt % 3]
                    s.op('pe', lambda e: e.matmul(p[:], lhsT=kb[:, kt * 128:(kt + 1) * 128], rhs=qnb[:], start=True, stop=False),
                         reads=[kb, qnb], writes=[p])
                    s.op('pe', lambda e: e.matmul(p[:], lhsT=krT[:, kt * 128:(kt + 1) * 128], rhs=qrb[:], start=False, stop=True),
                         reads=[krT, qrb], writes=[p])
                scores(0)
                for kt in range(NT):
                    if kt + 1 < NT:
                        scores(kt + 1)
                    p = ps_s[kt % 3]; pt = pT[kt % 3]
                    s.op('act', lambda e: e.activation(out=pt[:], in_=p[:], func=AF.Exp), reads=[p], writes=[pt])
                    s.op('pe', lambda e: e.matmul(ps_o[:], lhsT=vb[:, kt, :], rhs=pt[:], start=(kt == 0), stop=(kt == NT - 1)),
                         reads=[vb, pt], writes=[ps_o])
                    s.op('pe', lambda e: e.matmul(ps_d[:], lhsT=c.ones_bf[:], rhs=pt[:], start=(kt == 0), stop=(kt == NT - 1)),
                         reads=[c.ones_bf, pt], writes=[ps_d])
                s.op('dve', lambda e: e.reciprocal(out=rden[:], in_=ps_d[:]), reads=[ps_d], writes=[rden])
                aob = ao[qb % 2]
                s.op('dve', lambda e: e.tensor_tensor(out=aob[:], in0=ps_o[:], in1=rden[:], op=ALU.mult),
                     reads=[ps_o, rden], writes=[aob])
                s.dma('sp', lambda e: e.dma_start(out=ao_scr[h, :, qcols], in_=aob[:]), reads=[aob])
        s.barrier()
    if os.environ.get('MLA_STOP') == '2': return
    out_proj(c, s, "m3", ao_scr, c.w['mla_w_out'][j], src, dst)


def out_proj(c, s, pfx, a_scr, w_ap, src, dst):
    nc = c.nc; Sq = c.S; NB = Sq // 512
    with contextlib.ExitStack() as es:
        wo = sb(es, nc, pfx + "_wo", [128, 16, D], BF16)
        aoT = [sb(es, nc, pfx + "_aoT%d" % i, [128, 16, 512], BF16) for i in range(2)]
        xts = [sb(es, nc, pfx + "_xt%d" % i, [128, D], F32) for i in range(2)]
        xos = [sb(es, nc, pfx + "_xo%d" % i, [128, D], F32) for i in range(2)]
        ps = [psb(es, nc, pfx + "_ps%d" % i, [128, 512], F32) for i in range(8)]
        wsrc = w_ap.rearrange("(h p) n -> p h n", p=128)
        for c0 in range(0, 16, 2):
            load_cast(s, wo, wo[:, c0:c0 + 2, :], wsrc[:, c0:c0 + 2, :])
        k = 0
        for b in range(NB):
            cols = slice(b * 512, (b + 1) * 512)
            a = aoT[b % 2]
            s.dma('sp', lambda e: e.dma_start(out=a[:], in_=a_scr.rearrange("h p s -> p h s")[:, :, cols]), writes=[a])
            for jt in range(4):
                rows = slice(b * 512 + jt * 128, b * 512 + (jt + 1) * 128)
                xt = xts[k % 2]; xo = xos[k % 2]
                s.dma('sp', lambda e: e.dma_start(out=xt[:], in_=src[rows, :]), writes=[xt])
                for nb in range(4):
                    p = ps[(k % 2) * 4 + nb]
                    for h in range(16):
                        s.op('pe', lambda e: e.matmul(p[:], lhsT=a[:, h, jt * 128:(jt + 1) * 128], rhs=wo[:, h, nb * 512:(nb + 1) * 512],
                                                      start=(h == 0), stop=(h == 15)), reads=[a, wo], writes=[p])
                    s.op('dve', lambda e: e.tensor_tensor(out=xo[:, nb * 512:(nb + 1) * 512], in0=p[:], in1=xt[:, nb * 512:(nb + 1) * 512],
                                                          op=ALU.add), reads=[p, xt], writes=[xo])
                s.dma('sp', lambda e: e.dma_start(out=dst[rows, :], in_=xo[:]), reads=[xo])
                k += 1
        s.barrier()


def mlstm_layer(c, s, li, j, src, dst):
    nc = c.nc; Sq = c.S; NT = c.NT; NC = NT
    hf_scr = scr(c, "hf_scr", [Sq, D], F32)
    hn_scr = scr(c, "hn_scr", [Sq, D], BF16)
    a_scr = scr(c, "ao_scr", [16, 128, Sq], BF16)
    win = c.w['ml_w_in'][j].rearrange("(c p) n -> p c n", p=128)
    with contextlib.ExitStack() as esl:
        tokv = sb(esl, nc, "l_tokv", [128, NC, 48], F32)
        bc = sb(esl, nc, "l_bc", [128, 2, 8, 2, NC], F32)
        maskF = sb(esl, nc, "l_maskF", [128, 128], F32)
        maskB = sb(esl, nc, "l_maskB", [128, 128], F32)
        SEG = min(Sq, 1024); NSEG = Sq // SEG; CPS = SEG // 128
        g_scr = scr(c, "g_scr", [2, 8, Sq], F32)
        b_scr = scr(c, "b_scr", [2, 8, Sq], F32)
        with contextlib.ExitStack() as es:
            gB = sb(es, nc, "l0_gB", [128, D], F32)
            wg = sb(es, nc, "l0_wg", [128, 16, 32], BF16)
            xt = sb(es, nc, "l0_xt", [128, D], F32); xn = sb(es, nc, "l0_xn", [128, D], BF16)
            junk = sb(es, nc, "l0_junk", [128, D], BF16)
            small = (sb(es, nc, "l0_ss", [128, 1], F32), sb(es, nc, "l0_rs", [128, 4], F32))
            xnT = sb(es, nc, "l0_xnT", [128, 16, 128], BF16)
            bg = sb(es, nc, "l0_bg", [8, 4], F32)
            gp = [sb(es, nc, "l0_gp%d" % i, [8, SEG], F32) for i in range(4)]
            lf = sb(es, nc, "l0_lf", [8, SEG], F32); pf = sb(es, nc, "l0_pf", [8, SEG], F32)
            bb = sb(es, nc, "l0_bb", [8, SEG], F32); gg = sb(es, nc, "l0_gg", [8, SEG], F32)
            tmp = sb(es, nc, "l0_tmp", [8, SEG], F32)
            cmask = sb(es, nc, "l0_cmask", [8, SEG], F32)
            vec = [[sb(es, nc, "l0_vec%d%d" % (d, v), [8, SEG], F32) for v in range(3)] for d in range(2)]
            blA = [sb(es, nc, "l0_bl%d" % d, [8, NC], F32) for d in range(2)]
            gmaxA = [sb(es, nc, "l0_gmax%d" % d, [8, NC], F32) for d in range(2)]
            MtA = [sb(es, nc, "l0_Mt%d" % d, [8, NC], F32) for d in range(2)]
            bmA = [sb(es, nc, "l0_bm%d" % d, [8, NC], F32) for d in range(2)]
            ml_ = sb(es, nc, "l0_ml", [8, NC], F32); mst = sb(es, nc, "l0_mst", [8, NC + 1], F32)
            t8 = sb(es, nc, "l0_t8", [8, NC], F32)
            dca = [[sb(es, nc, "l0_dca%d%d" % (d, v), [8, NC], F32) for v in range(2)] for d in range(2)]
            sel = sb(es, nc, "l0_sel", [8, 8, 128], F32)
            io = sb(es, nc, "l0_io", [128, 128], F32); ip = sb(es, nc, "l0_ip", [128, 128], F32)
            ps_t = [psb(es, nc, "l0_pt%d" % i, [128, 1024], BF16) for i in range(2)]
            ps_g = [psb(es, nc, "l0_pg%d" % i, [128, 512], F32) for i in range(4)]
            load_gain(c, s, gB, c.w['norm_mix'][li])
            load_cast(s, wg, wg[:], win[:, :, 6144:6176])
            s.dma('sp', lambda e: e.dma_start(out=bg[:], in_=c.w['ml_b_gates'][j].rearrange("t h -> h t"),
                                              allow_slow_non_contiguous=True), writes=[bg])
            s.op('dve', lambda e: e.tensor_scalar(out=bg[:], in0=bg[:], scalar1=1.0 / 15.0, scalar2=None, op0=ALU.mult),
                 reads=[bg], writes=[bg])
            s.op('pool', lambda e: e.iota(io[:], pattern=[[1, 128]], base=0, channel_multiplier=0,
                                          allow_small_or_imprecise_dtypes=True), writes=[io])
            s.op('pool', lambda e: e.iota(ip[:], pattern=[[0, 128]], base=0, channel_multiplier=1,
                                          allow_small_or_imprecise_dtypes=True), writes=[ip])
            s.op('dve', lambda e: e.tensor_tensor(out=maskF[:], in0=io[:], in1=ip[:], op=ALU.is_ge), reads=[io, ip], writes=[maskF])
            s.op('dve', lambda e: e.tensor_tensor(out=maskB[:], in0=io[:], in1=ip[:], op=ALU.is_le), reads=[io, ip], writes=[maskB])
            s.op('dve', lambda e: e.tensor_copy(out=sel[:], in_=c.ident_f[0:8, 0:8].unsqueeze(2).to_broadcast([8, 8, 128])),
                 reads=[c.ident_f], writes=[sel])
            s.op('dve', lambda e: e.memset(cmask[:], 1.0), writes=[cmask])
            s.op('dve', lambda e: e.memset(cmask[:, 0:SEG:128], 0.0), writes=[cmask])
            v3 = lambda tl: tl[:].rearrange("p (a b) -> p a b", b=128)
            bcast = lambda ap: ap.unsqueeze(2).to_broadcast([8, CPS, 128])
            for sg in range(NSEG):
                ch = slice(sg * CPS, (sg + 1) * CPS)
                scols = slice(sg * SEG, (sg + 1) * SEG)
                for tl in range(CPS):
                    t = sg * CPS + tl
                    rows = slice(t * 128, (t + 1) * 128)
                    lrows = slice(tl * 128, (tl + 1) * 128)
                    norm_tile(c, s, src[rows, :], xt, gB, xn, small, junk)
                    transpose_tile(c, s, xn, xnT, 0, ps_t)
                    for ty in range(4):
                        p = ps_g[ty]
                        for kc in range(16):
                            s.op('pe', lambda e: e.matmul(p[0:8, 0:128], lhsT=wg[:, kc, ty * 8:(ty + 1) * 8], rhs=xnT[:, kc, :],
                                                          start=(kc == 0), stop=(kc == 15)), reads=[wg, xnT], writes=[p])
                        s.op('act', lambda e: e.activation(out=gp[ty][:, lrows], in_=p[0:8, 0:128], func=AF.Tanh,
                                                           bias=bg[:, ty:ty + 1], scale=1.0 / 15.0), reads=[p, bg], writes=[gp[ty]])
                for d in range(2):
                    lig = gp[2 * d]; fpg = gp[2 * d + 1]; bl = blA[d]; gmax = gmaxA[d]
                    s.op('dve', lambda e: e.tensor_scalar(out=lig[:], in0=lig[:], scalar1=15.0, scalar2=None, op0=ALU.mult),
                         reads=[lig], writes=[lig])
                    s.op('act', lambda e: e.activation(out=lf[:], in_=fpg[:], func=AF.Sigmoid, scale=15.0), reads=[fpg], writes=[lf])
                    s.op('act', lambda e: e.activation(out=lf[:], in_=lf[:], func=AF.Ln), reads=[lf], writes=[lf])
                    s.op('dve', lambda e: e.tensor_tensor_scan(out=pf[:], data0=cmask[:], data1=lf[:], initial=0.0,
                                                               op0=ALU.mult, op1=ALU.add), reads=[cmask, lf], writes=[pf])
                    s.op('dve', lambda e: e.tensor_copy(out=bl[:, ch], in_=pf[:, 127:SEG:128]), reads=[pf], writes=[bl])
                    if d == 0:
                        s.op('dve', lambda e: e.tensor_copy(out=bb[:], in_=pf[:]), reads=[pf], writes=[bb])
                    else:
                        s.op('dve', lambda e: e.tensor_tensor(out=bb[:], in0=lf[:], in1=pf[:], op=ALU.subtract), reads=[lf, pf], writes=[bb])
                        s.op('dve', lambda e: e.tensor_tensor(out=v3(bb), in0=v3(bb), in1=bcast(bl[:, ch]), op=ALU.add),
                             reads=[bb, bl], writes=[bb])
                    s.op('dve', lambda e: e.tensor_tensor(out=gg[:], in0=lig[:], in1=bb[:], op=ALU.subtract), reads=[lig, bb], writes=[gg])
                    s.op('dve', lambda e: e.tensor_reduce(out=gmax[:, ch], in_=v3(gg), axis=AX.X, op=ALU.max), reads=[gg], writes=[gmax])
                    s.dma('sp', lambda e: e.dma_start(out=g_scr[d, :, scols], in_=gg[:]), reads=[gg])
                    s.dma('sp', lambda e: e.dma_start(out=b_scr[d, :, scols], in_=bb[:]), reads=[bb])
            for d in range(2):
                bl = blA[d]; gmax = gmaxA[d]; Mt = MtA[d]
                s.op('dve', lambda e: e.tensor_tensor(out=ml_[:], in0=bl[:], in1=gmax[:], op=ALU.add), reads=[bl, gmax], writes=[ml_])
                s.op('dve', lambda e: e.memset(mst[:], 0.0), writes=[mst])
                order = range(NC) if d == 0 else range(NC - 1, -1, -1)
                for jc in order:
                    pi, ni = (jc, jc + 1) if d == 0 else (jc + 1, jc)
                    s.op('dve', lambda e: e.scalar_tensor_tensor(out=mst[:, ni:ni + 1], in0=bl[:, jc:jc + 1], scalar=mst[:, pi:pi + 1],
                                                                 in1=ml_[:, jc:jc + 1], op0=ALU.add, op1=ALU.max),
                         reads=[bl, mst, ml_], writes=[mst])
                mp = mst[:, 0:NC] if d == 0 else mst[:, 1:NC + 1]
                mn = mst[:, 1:NC + 1] if d == 0 else mst[:, 0:NC]
                s.op('dve', lambda e: e.tensor_tensor(out=Mt[:], in0=mp, in1=gmax[:], op=ALU.max), reads=[mst, gmax], writes=[Mt])
                s.op('dve', lambda e: e.tensor_tensor(out=bmA[d][:], in0=bl[:], in1=mn, op=ALU.subtract), reads=[bl, mst], writes=[bmA[d]])
                s.op('dve', lambda e: e.tensor_tensor(out=t8[:], in0=bmA[d][:], in1=mp, op=ALU.add), reads=[bmA[d], mst], writes=[t8])
                s.op('act', lambda e: e.activation(out=dca[d][0][:], in_=t8[:], func=AF.Exp), reads=[t8], writes=[dca[d][0]])
                s.op('dve', lambda e: e.tensor_tensor(out=t8[:], in0=mp, in1=Mt[:], op=ALU.subtract), reads=[Mt, mst], writes=[t8])
                s.op('act', lambda e: e.activation(out=dca[d][1][:], in_=t8[:], func=AF.Exp), reads=[t8], writes=[dca[d][1]])
                for h in range(8):
                    for v in range(2):
                        p = ps_g[(h * 2 + v) % 4]
                        s.op('pe', lambda e: e.matmul(p[:, 0:NC], lhsT=sel[:, h, :], rhs=dca[d][v][:], start=True, stop=True),
                             reads=[sel, dca[d][v]], writes=[p])
                        s.op('dve', lambda e: e.tensor_copy(out=bc[:, d, h, v, :], in_=p[:, 0:NC]), reads=[p], writes=[bc])
            for sg in range(NSEG):
                ch = slice(sg * CPS, (sg + 1) * CPS)
                scols = slice(sg * SEG, (sg + 1) * SEG)
                for d in range(2):
                    Mt = MtA[d]
                    s.dma('sp', lambda e: e.dma_start(out=gg[:], in_=g_scr[d, :, scols]), writes=[gg])
                    s.dma('sp', lambda e: e.dma_start(out=bb[:], in_=b_scr[d, :, scols]), writes=[bb])
                    s.op('dve', lambda e: e.tensor_tensor(out=v3(tmp), in0=v3(gg), in1=bcast(Mt[:, ch]), op=ALU.subtract),
                         reads=[gg, Mt], writes=[tmp])
                    s.op('act', lambda e: e.activation(out=vec[d][0][:], in_=tmp[:], func=AF.Exp), reads=[tmp], writes=[vec[d][0]])
                    s.op('dve', lambda e: e.tensor_tensor(out=v3(tmp), in0=v3(gg), in1=bcast(bmA[d][:, ch]), op=ALU.add),
                         reads=[gg, bmA[d]], writes=[tmp])
                    s.op('act', lambda e: e.activation(out=vec[d][1][:], in_=tmp[:], func=AF.Exp), reads=[tmp], writes=[vec[d][1]])
                    s.op('dve', lambda e: e.tensor_tensor(out=v3(tmp), in0=v3(bb), in1=bcast(Mt[:, ch]), op=ALU.add),
                         reads=[bb, Mt], writes=[tmp])
                    s.op('act', lambda e: e.activation(out=vec[d][2][:], in_=tmp[:], func=AF.Exp, scale=-1.0), reads=[tmp], writes=[vec[d][2]])
                for tl in range(CPS):
                    jc = sg * CPS + tl
                    p = ps_g[jc % 4]
                    for d in range(2):
                        for v in range(3):
                            i = d * 3 + v
                            s.op('pe', lambda e: e.transpose(out=p[:, i * 8:(i + 1) * 8], in_=vec[d][v][:, tl * 128:(tl + 1) * 128],
                                                             identity=c.ident_f[0:8, 0:8]), reads=[vec[d][v], c.ident_f], writes=[p])
                    s.op('dve', lambda e: e.tensor_copy(out=tokv[:, jc, :], in_=p[:, 0:48]), reads=[p], writes=[tokv])
            if getattr(c, 'dbg', False):
                d1 = nc.dram_tensor("dbg_tokv", [128, NC, 48], F32, kind="ExternalOutput").ap()
                d2 = nc.dram_tensor("dbg_bc", [128, 2, 8, 2, NC], F32, kind="ExternalOutput").ap()
                s.dma('sp', lambda e: e.dma_start(out=d1, in_=tokv[:]), reads=[tokv])
                s.dma('sp', lambda e: e.dma_start(out=d2, in_=bc[:]), reads=[bc])
            s.barrier()
        for d in range(2):
            for hg in range(2):
                with contextlib.ExitStack() as es:
                    gB = sb(es, nc, "l1_gB", [128, D], F32)
                    wq = sb(es, nc, "l1_wq", [128, 16, 512], BF16)
                    wk = sb(es, nc, "l1_wk", [128, 16, 512], BF16)
                    wv = sb(es, nc, "l1_wv", [128, 16, 1024], BF16)
                    xt = sb(es, nc, "l1_xt", [128, D], F32); xn = sb(es, nc, "l1_xn", [128, D], BF16)
                    junk = sb(es, nc, "l1_junk", [128, D], BF16)
                    small = (sb(es, nc, "l1_ss", [128, 1], F32), sb(es, nc, "l1_rs", [128, 4], F32))
                    xnT = sb(es, nc, "l1_xnT", [128, 16, 128], BF16)
                    qT = sb(es, nc, "l1_qT", [128, 4, 128], BF16); kT = sb(es, nc, "l1_kT", [128, 4, 128], BF16)
                    kw = sb(es, nc, "l1_kw", [128, 4, 128], BF16)
                    vaug = sb(es, nc, "l1_vaug", [128, 4, 258], BF16)
                    Cst = sb(es, nc, "l1_C", [128, 4, 258], F32)
                    Ct = sb(es, nc, "l1_Ct", [128, 4, 258], BF16)
                    hacc = sb(es, nc, "l1_hacc", [128, 1024], F32)
                    hft = sb(es, nc, "l1_hft", [128, 1024], F32)
                    hn = sb(es, nc, "l1_hn", [128, 1024], BF16)
                    gH = sb(es, nc, "l1_gH", [128, 1024], F32)
                    PT = [sb(es, nc, "l1_PT%d" % i, [128, 128], BF16) for i in range(2)]
                    dd = sb(es, nc, "l1_dd", [128, 4], F32)
                    hs2 = (sb(es, nc, "l1_hss", [128, 1], F32), sb(es, nc, "l1_hrs", [128, 4], F32))
                    ps_t = [psb(es, nc, "l1_pt%d" % i, [128, 1024], BF16) for i in range(2)]
                    ps_a = [psb(es, nc, "l1_pa%d" % i, [128, 512], F32) for i in range(2)]
                    ps_s = psb(es, nc, "l1_pss", [128, 512], F32)
                    ps_n = [psb(es, nc, "l1_pn%d" % i, [128, 512], F32) for i in range(2)]
                    ps_c = psb(es, nc, "l1_pc", [128, 512], F32)
                    load_gain(c, s, gB, c.w['norm_mix'][li])
                    s.dma('sp', lambda e: e.dma_start(out=gH[:], in_=c.w['ml_head_norm'][j].rearrange("h d -> (h d)")[hg * 1024:(hg + 1) * 1024]
                                                      .partition_broadcast(128)), writes=[gH])
                    for c0 in range(0, 16, 4):
                        load_cast(s, wq, wq[:, c0:c0 + 4, :], win[:, c0:c0 + 4, hg * 512:(hg + 1) * 512])
                        load_cast(s, wk, wk[:, c0:c0 + 4, :], win[:, c0:c0 + 4, 1024 + hg * 512:1024 + (hg + 1) * 512])
                        load_cast(s, wv, wv[:, c0:c0 + 4, :], win[:, c0:c0 + 4, 2048 + hg * 1024:2048 + (hg + 1) * 1024])
                    s.op('dve', lambda e: e.memset(vaug[:], 1.0), writes=[vaug])
                    s.op('dve', lambda e: e.memset(Cst[:], 0.0), writes=[Cst])
                    s.op('dve', lambda e: e.memset(Ct[:], 0.0), writes=[Ct])
                    mask = maskF if d == 0 else maskB
                    order = list(range(NC)) if d == 0 else list(range(NC - 1, -1, -1))
                    for oi, jc in enumerate(order):
                        rows = slice(jc * 128, (jc + 1) * 128)
                        hcols = slice(hg * 1024, (hg + 1) * 1024)
                        norm_tile(c, s, src[rows, :], xt, gB, xn, small, junk)
                        transpose_tile(c, s, xn, xnT, 0, ps_t)
                        if d == 1:
                            s.dma('sp', lambda e: e.dma_start(out=hft[:], in_=hf_scr[rows, hcols]), writes=[hft])
                        for (wt, outt, sc, p) in ((wq, qT, 128.0 ** -0.5, ps_a[0]), (wk, kT, 1.0, ps_a[1])):
                            for h in range(4):
                                for kc in range(16):
                                    s.op('pe', lambda e: e.matmul(p[:, h * 128:(h + 1) * 128], lhsT=wt[:, kc, h * 128:(h + 1) * 128],
                                                                  rhs=xnT[:, kc, :], start=(kc == 0), stop=(kc == 15)),
                                         reads=[wt, xnT], writes=[p])
                            s.op('act', lambda e: e.mul(out=outt[:].rearrange("p a b -> p (a b)"), in_=p[:], mul=sc), reads=[p], writes=[outt])
                        p = ps_a[0]
                        for kc in range(16):
                            s.op('pe', lambda e: e.matmul(p[:], lhsT=xnT[:, kc, :], rhs=wk[:, kc, :], start=(kc == 0), stop=(kc == 15)),
                                 reads=[wk, xnT], writes=[p])
                        for h in range(4):
                            col = (d * 3 + 1) * 8 + hg * 4 + h
                            s.op('dve', lambda e: e.tensor_scalar(out=kw[:, h, :], in0=p[:, h * 128:(h + 1) * 128],
                                                                  scalar1=tokv[:, jc, col:col + 1], scalar2=None, op0=ALU.mult),
                                 reads=[p, tokv], writes=[kw])
                        for nb in range(2):
                            p = ps_a[1] if nb == 0 else ps_a[0]
                            for kc in range(16):
                                s.op('pe', lambda e: e.matmul(p[:], lhsT=xnT[:, kc, :], rhs=wv[:, kc, nb * 512:(nb + 1) * 512],
                                                              start=(kc == 0), stop=(kc == 15)), reads=[wv, xnT], writes=[p])
                            s.op('act', lambda e: e.copy(out=vaug[:, 2 * nb:2 * nb + 2, 0:256], in_=p[:].rearrange("p (a b) -> p a b", a=2)),
                                 reads=[p], writes=[vaug])
                        for h in range(4):
                            hh = hg * 4 + h
                            cew = (d * 3 + 0) * 8 + hh; ccl = (d * 3 + 2) * 8 + hh
                            s.op('pe', lambda e: e.matmul(ps_s[:, 0:128], lhsT=kT[:, h, :], rhs=qT[:, h, :], start=True, stop=True),
                                 reads=[kT, qT], writes=[ps_s])
                            pt = PT[h % 2]
                            s.op('dve', lambda e: e.scalar_tensor_tensor(out=pt[:], in0=ps_s[:, 0:128], scalar=tokv[:, jc, cew:cew + 1],
                                                                         in1=mask[:], op0=ALU.mult, op1=ALU.mult),
                                 reads=[ps_s, tokv, mask], writes=[pt])
                            pn = ps_n[h % 2]
                            s.op('pe', lambda e: e.matmul(pn[:, 0:258], lhsT=pt[:], rhs=vaug[:, h, :], start=True, stop=False),
                                 reads=[pt, vaug], writes=[pn])
                            s.op('pe', lambda e: e.matmul(pn[:, 0:258], lhsT=qT[:, h, :], rhs=Ct[:, h, :], start=False, stop=True),
                                 reads=[qT, Ct], writes=[pn])
                            s.op('dve', lambda e: e.tensor_scalar(out=dd[:, 0:1], in0=pn[:, 256:257], scalar1=-1.0, scalar2=None, op0=ALU.mult),
                                 reads=[pn], writes=[dd])
                            s.op('dve', lambda e: e.tensor_tensor(out=dd[:, 3:4], in0=dd[:, 0:1], in1=pn[:, 256:257], op=ALU.max),
                                 reads=[dd, pn], writes=[dd])
                            s.op('dve', lambda e: e.tensor_tensor(out=dd[:, 1:2], in0=dd[:, 3:4], in1=tokv[:, jc, ccl:ccl + 1], op=ALU.max),
                                 reads=[dd, tokv], writes=[dd])
                            s.op('dve', lambda e: e.reciprocal(out=dd[:, 2:3], in_=dd[:, 1:2]), reads=[dd], writes=[dd])
                            if d == 0:
                                s.op('dve', lambda e: e.tensor_scalar(out=hacc[:, h * 256:(h + 1) * 256], in0=pn[:, 0:256], scalar1=dd[:, 2:3],
                                                                      scalar2=None, op0=ALU.mult), reads=[pn, dd], writes=[hacc])
                            else:
                                s.op('dve', lambda e: e.scalar_tensor_tensor(out=hacc[:, h * 256:(h + 1) * 256], in0=pn[:, 0:256], scalar=dd[:, 2:3],
                                                                             in1=hft[:, h * 256:(h + 1) * 256], op0=ALU.mult, op1=ALU.add),
                                     reads=[pn, dd, hft], writes=[hacc])
                            s.op('pe', lambda e: e.matmul(ps_c[:, 0:258], lhsT=kw[:, h, :], rhs=vaug[:, h, :], start=True, stop=True),
                                 reads=[kw, vaug], writes=[ps_c])
                            s.op('dve', lambda e: e.scalar_tensor_tensor(out=Cst[:, h, :], in0=Cst[:, h, :], scalar=bc[:, d, hh, 0, jc:jc + 1],
                                                                         in1=ps_c[:, 0:258], op0=ALU.mult, op1=ALU.add),
                                 reads=[Cst, bc, ps_c], writes=[Cst])
                            if oi + 1 < NC:
                                jn = order[oi + 1]
                                s.op('dve', lambda e: e.tensor_scalar(out=Ct[:, h, :], in0=Cst[:, h, :], scalar1=bc[:, d, hh, 1, jn:jn + 1],
                                                                      scalar2=None, op0=ALU.mult), reads=[Cst, bc], writes=[Ct])
                        if d == 0:
                            s.dma('sp', lambda e: e.dma_start(out=hf_scr[rows, hcols], in_=hacc[:]), reads=[hacc])
                        else:
                            for h in range(4):
                                hsl = slice(h * 256, (h + 1) * 256)
                                ss, rs = hs2
                                s.op('act', lambda e: e.activation(out=junk[:, 0:256], in_=hacc[:, hsl], func=AF.Square, accum_out=ss[:, 0:1]),
                                     reads=[hacc], writes=[junk, ss])
                                s.op('dve', lambda e: e.tensor_scalar(out=rs[:, 0:1], in0=ss[:, 0:1], scalar1=1.0 / 256, scalar2=EPS,
                                                                      op0=ALU.mult, op1=ALU.add), reads=[ss], writes=[rs])
                                s.op('act', lambda e: e.activation(out=rs[:, 1:2], in_=rs[:, 0:1], func=AF.Sqrt), reads=[rs], writes=[rs])
                                s.op('dve', lambda e: e.reciprocal(out=rs[:, 2:3], in_=rs[:, 1:2]), reads=[rs], writes=[rs])
                                s.op('dve', lambda e: e.scalar_tensor_tensor(out=hn[:, hsl], in0=hacc[:, hsl], scalar=rs[:, 2:3], in1=gH[:, hsl],
                                                                             op0=ALU.mult, op1=ALU.mult), reads=[hacc, rs, gH], writes=[hn])
                            s.dma('sp', lambda e: e.dma_start(out=hn_scr[rows, hcols], in_=hn[:]), reads=[hn])
                    s.barrier()
        with contextlib.ExitStack() as es:
            gB = sb(es, nc, "l2_gB", [128, D], F32)
            wo = sb(es, nc, "l2_wo", [128, 16, D], BF16)
            xt = sb(es, nc, "l2_xt", [128, D], F32); xn = sb(es, nc, "l2_xn", [128, D], BF16)
            junk = sb(es, nc, "l2_junk", [128, D], BF16)
            small = (sb(es, nc, "l2_ss", [128, 1], F32), sb(es, nc, "l2_rs", [128, 4], F32))
            xnT = sb(es, nc, "l2_xnT", [128, 16, 128], BF16)
            og = sb(es, nc, "l2_og", [128, D], F32)
            hn = sb(es, nc, "l2_hn", [128, D], BF16)
            yv = sb(es, nc, "l2_yv", [128, D], BF16)
            yT = sb(es, nc, "l2_yT", [128, 16, 128], BF16)
            ps_t = [psb(es, nc, "l2_pt%d" % i, [128, 1024], BF16) for i in range(2)]
            ps_a = [psb(es, nc, "l2_pa%d" % i, [128, 512], F32) for i in range(4)]
            load_gain(c, s, gB, c.w['norm_mix'][li])
            for c0 in range(0, 16, 2):
                load_cast(s, wo, wo[:, c0:c0 + 2, :], win[:, c0:c0 + 2, 4096:6144])
            for t in range(NT):
                rows = slice(t * 128, (t + 1) * 128)
                norm_tile(c, s, src[rows, :], xt, gB, xn, small, junk)
                transpose_tile(c, s, xn, xnT, 0, ps_t)
                s.dma('sp', lambda e: e.dma_start(out=hn[:], in_=hn_scr[rows, :]), writes=[hn])
                for nb in range(4):
                    p = ps_a[nb]
                    for kc in range(16):
                        s.op('pe', lambda e: e.matmul(p[:], lhsT=xnT[:, kc, :], rhs=wo[:, kc, nb * 512:(nb + 1) * 512],
                                                      start=(kc == 0), stop=(kc == 15)), reads=[wo, xnT], writes=[p])
                    s.op('dve', lambda e: e.tensor_copy(out=og[:, nb * 512:(nb + 1) * 512], in_=p[:]), reads=[p], writes=[og])
                s.op('act', lambda e: e.activation(out=og[:], in_=og[:], func=AF.Sigmoid), reads=[og], writes=[og])
                s.op('dve', lambda e: e.tensor_tensor(out=yv[:], in0=og[:], in1=hn[:], op=ALU.mult), reads=[og, hn], writes=[yv])
                transpose_tile(c, s, yv, yT, 0, ps_t)
                s.dma('sp', lambda e: e.dma_start(out=a_scr.rearrange("h p s -> p h s")[:, :, rows], in_=yT[:]), reads=[yT])
            s.barrier()
    out_proj(c, s, "l3", a_scr, c.w['ml_w_out'][j], src, dst)


def build(Sq, layers, final=True, test=False, dbg=False):
    nc = bass.Bass("TRN2", target_bir_lowering=False)
    c = Ctx(); c.nc = nc; c.S = Sq; c.NT = Sq // 128; c.dbg = dbg
    shapes = dict(
        norm_mix=[4, D], norm_ffn=[4, D], norm_final=[D],
        mla_w_in=[2, D, 832], mla_q_norm=[2, 512], mla_w_q_up=[2, 512, 3072], mla_kv_norm=[2, 256],
        mla_w_kv_up=[2, 256, 4096], mla_w_out=[2, D, D],
        ml_w_in=[2, D, 6176], ml_b_gates=[2, 4, 8], ml_head_norm=[2, 8, 256], ml_w_out=[2, D, D],
        peer_w_query=[4, D, D], peer_sub_keys=[4, 8, 2, 128, 128], peer_u=[4, 16384, D], peer_v=[4, 16384, D],
        rope_c=[64, Sq], rope_s=[64, Sq])
    kinds = set(k for (k, _, _) in layers)
    if test:
        need = {'norm_final'}
        if 'peer' in kinds: need |= {'norm_ffn', 'peer_w_query', 'peer_sub_keys', 'peer_u', 'peer_v'}
        if 'mla' in kinds: need |= {'norm_mix', 'rope_c', 'rope_s'} | {k for k in shapes if k.startswith('mla_')}
        if 'mlstm' in kinds: need |= {'norm_mix'} | {k for k in shapes if k.startswith('ml_')}
        shapes = {k: ([1] + v[1:] if (k not in ('norm_final', 'rope_c', 'rope_s')) else v) for k, v in shapes.items() if k in need}
    c.w = {k: nc.dram_tensor(k, v, F32, kind="ExternalInput").ap() for k, v in shapes.items()}
    x = nc.dram_tensor("x", [Sq, D], F32, kind="ExternalInput").ap()
    y = nc.dram_tensor("y", [Sq, D], F32, kind="ExternalOutput").ap()
    res = nc.dram_tensor("res", [Sq, D], F32, kind="Internal").ap()
    c.x = x; c.y = y; c.res = res; c.scr = {}
    with contextlib.ExitStack() as es:
        s = S(nc)
        c.ident_bf = sb(es, nc, "ident_bf", [128, 128], BF16)
        c.ident_f = sb(es, nc, "ident_f", [128, 128], F32)
        c.ones_bf = sb(es, nc, "ones_bf", [128, 128], BF16)
        with contextlib.ExitStack() as es2:
            io = sb(es2, nc, "io_a", [128, 128], F32)
            ip = sb(es2, nc, "io_b", [128, 128], F32)
            s.op('pool', lambda e: e.iota(io[:], pattern=[[1, 128]], base=0, channel_multiplier=0,
                                          allow_small_or_imprecise_dtypes=True), writes=[io])
            s.op('pool', lambda e: e.iota(ip[:], pattern=[[0, 128]], base=0, channel_multiplier=1,
                                          allow_small_or_imprecise_dtypes=True), writes=[ip])
            s.op('dve', lambda e: e.tensor_tensor(out=c.ident_f[:], in0=io[:], in1=ip[:], op=ALU.is_equal),
                 reads=[io, ip], writes=[c.ident_f])
            s.op('dve', lambda e: e.tensor_copy(out=c.ident_bf[:], in_=c.ident_f[:]), reads=[c.ident_f], writes=[c.ident_bf])
            s.op('dve', lambda e: e.memset(c.ones_bf[:], 1.0), writes=[c.ones_bf])
            s.barrier()
        if 'peer' in kinds:
            nl = c.w['peer_u'].shape[0]
            c.tab_u = nc.dram_tensor("tab_u_bf", [nl * 16384, D], BF16, kind="Internal").ap()
            c.tab_v = nc.dram_tensor("tab_v_bf", [nl * 16384, D], BF16, kind="Internal").ap()
            for (srcn, dstt) in (('peer_u', c.tab_u), ('peer_v', c.tab_v)):
                fl = c.w[srcn].rearrange("l e d -> (l e) d")
                for r0 in range(0, nl * 16384, 8192):
                    s.dma('pool', lambda e: e.dma_start(out=dstt[r0:r0 + 8192, :], in_=fl[r0:r0 + 8192, :]))
            s.barrier()
        cur = x
        for (kind, li, j) in layers:
            if kind == 'peer':
                peer_layer(c, s, li, cur, res)
            elif kind == 'mla':
                mla_layer(c, s, li, j, cur, res)
            elif kind == 'mlstm':
                mlstm_layer(c, s, li, j, cur, res)
            cur = res
        if final:
            final_norm(c, s, cur, y)
        s.barrier()
        c.ninst = s.ninst
    return nc, c


def rope_tables(Sq):
    inv = 10000.0 ** (-np.arange(0, 64, 2, dtype=np.float32) / 64.0)
    ang = np.arange(Sq, dtype=np.float32)[:, None] * inv[None, :]
    cos = np.cos(ang).astype(np.float32).T
    sin = np.sin(ang).astype(np.float32).T
    return (np.ascontiguousarray(np.concatenate([cos, cos], 0)),
            np.ascontiguousarray(np.concatenate([-sin, sin], 0)))


FULL_LAYERS = [('mla', 0, 0), ('peer', 0, 0), ('mlstm', 1, 0), ('peer', 1, 0),
               ('mla', 2, 1), ('peer', 2, 0), ('mlstm', 3, 1), ('peer', 3, 0)]


def kernel(**inputs):
    Sq = 8192
    nc, c = build(Sq, FULL_LAYERS)
    rc, rs = rope_tables(Sq)
    wts = {k: np.ascontiguousarray(np.asarray(v, dtype=np.float32)) for k, v in inputs.items()
           if k not in ('x_prompt', 'x_sample')}
    wts['rope_c'] = rc; wts['rope_s'] = rs
    xs = [np.asarray(inputs['x_prompt'][0]), np.asarray(inputs['x_prompt'][1]), np.asarray(inputs['x_sample'][0])]
    in_maps = []
    for i in range(3):
        m = dict(wts); m['x'] = np.ascontiguousarray(xs[i], dtype=np.float32)
        in_maps.append(m)
    r = run_bass_kernel_spmd(nc, in_maps, core_ids=[0, 1, 2])
    ys = [np.asarray(r.results[i]['y'], dtype=np.float32) for i in range(3)]
    return (np.stack([ys[0], ys[1]], 0), ys[2][None])
```

```python
import contextlib
import numpy as np
import concourse.bass as bass
import concourse.mybir as mybir
from concourse.bass_utils import run_bass_kernel_spmd

F32 = mybir.dt.float32; BF16 = mybir.dt.bfloat16; I32 = mybir.dt.int32; U32 = mybir.dt.uint32
AF = mybir.ActivationFunctionType
ALU = mybir.AluOpType
AX = mybir.AxisListType
D = 2048
EPS = 1e-6
NEG = -1.0e30


class T:
    def __init__(self, h, name=""):
        self.h = h; self.name = name; self.w = None; self.r = []
    def __getitem__(self, idx):
        return self.h[idx]


class S:
    NDMA = 16
    def __init__(self, nc):
        self.nc = nc
        self.eng = {'pe': nc.tensor, 'act': nc.scalar, 'dve': nc.vector, 'pool': nc.gpsimd, 'sp': nc.sync}
        self.sem = {k: nc.alloc_semaphore("s_" + k) for k in self.eng}
        self.cnt = {k: 0 for k in self.eng}
        self.seen = {k: {} for k in self.eng}
        self.dsem = [nc.alloc_semaphore("d%d" % i) for i in range(self.NDMA)]
        self.dval = [0] * self.NDMA
        self.dnext = 0
        self.ninst = 0
    def _wait(self, eng, deps):
        need = {}
        for tok in deps:
            if tok is None: continue
            k, v = tok
            if v > need.get(k, 0): need[k] = v
        for k, v in need.items():
            if self.seen[eng].get(k, 0) >= v: continue
            sem = self.sem[k] if isinstance(k, str) else self.dsem[k]
            self.eng[eng].wait_ge(sem, v)
            self.seen[eng][k] = v
    def _deps(self, reads, writes):
        deps = []
        for t in reads: deps.append(t.w)
        for t in writes:
            deps.append(t.w); deps.extend(t.r)
        return deps
    def _commit(self, tok, reads, writes):
        for t in reads:
            t.r.append(tok)
            if len(t.r) > 64: t.r = t.r[-48:]
        for t in writes:
            t.w = tok; t.r = []
    def op(self, eng, fn, reads=(), writes=(), extra=()):
        self._wait(eng, self._deps(reads, writes) + list(extra))
        inst = fn(self.eng[eng])
        self.cnt[eng] += 1; self.ninst += 1
        inst.then_inc(self.sem[eng], 1)
        tok = (eng, self.cnt[eng])
        self._commit(tok, reads, writes)
        return tok
    def dma(self, q, fn, reads=(), writes=(), extra=()):
        j = self.dnext; self.dnext = (self.dnext + 1) % self.NDMA
        deps = self._deps(reads, writes) + list(extra)
        if self.dval[j] > 0: deps.append((j, self.dval[j]))
        self._wait(q, deps)
        inst = fn(self.eng[q])
        self.dval[j] += 16; self.ninst += 1
        inst.then_inc(self.dsem[j], 16)
        tok = (j, self.dval[j])
        self._commit(tok, reads, writes)
        return tok
    def barrier(self):
        allt = [(k, self.cnt[k]) for k in self.eng if self.cnt[k] > 0]
        allt += [(j, self.dval[j]) for j in range(self.NDMA) if self.dval[j] > 0]
        for e in self.eng:
            self._wait(e, allt)


class Ctx:
    pass


_UNIQ = [0]


def sb(es, nc, name, shape, dt):
    _UNIQ[0] += 1
    name = "%s_%d" % (name, _UNIQ[0])
    return T(es.enter_context(nc.sbuf_tensor(name, shape, dt)), name)


def psb(es, nc, name, shape, dt):
    _UNIQ[0] += 1
    name = "%s_%d" % (name, _UNIQ[0])
    return T(es.enter_context(nc.psum_tensor(name, shape, dt)), name)


def load_cast(s, dst, dst_ap, src_ap, q='pool'):
    return s.dma(q, lambda e: e.dma_start(out=dst_ap, in_=src_ap), writes=[dst])


def norm_tile(c, s, src_ap, xt, gB, xn, small, junk):
    ss, rs = small
    s.dma('sp', lambda e: e.dma_start(out=xt[:], in_=src_ap), writes=[xt])
    s.op('act', lambda e: e.activation(out=junk[:], in_=xt[:], func=AF.Square, accum_out=ss[:, 0:1]),
         reads=[xt], writes=[junk, ss])
    s.op('dve', lambda e: e.tensor_scalar(out=rs[:, 0:1], in0=ss[:, 0:1], scalar1=1.0 / D, scalar2=EPS,
                                          op0=ALU.mult, op1=ALU.add), reads=[ss], writes=[rs])
    s.op('act', lambda e: e.activation(out=rs[:, 1:2], in_=rs[:, 0:1], func=AF.Sqrt), reads=[rs], writes=[rs])
    s.op('dve', lambda e: e.reciprocal(out=rs[:, 2:3], in_=rs[:, 1:2]), reads=[rs], writes=[rs])
    s.op('dve', lambda e: e.scalar_tensor_tensor(out=xn[:], in0=xt[:], scalar=rs[:, 2:3], in1=gB[:],
                                                 op0=ALU.mult, op1=ALU.mult), reads=[xt, rs, gB], writes=[xn])


def transpose_tile(c, s, xn, xnT, col0, pst, nchunk=16):
    for g in range(nchunk // 4):
        p = pst[g % len(pst)]
        for i in range(4):
            ch = g * 4 + i
            s.op('pe', lambda e: e.transpose(out=p[:, i * 128:(i + 1) * 128], in_=xn[:, ch * 128:(ch + 1) * 128],
                                             identity=c.ident_bf[:]), reads=[xn, c.ident_bf], writes=[p])
        eng = 'act' if g % 2 == 0 else 'dve'
        if eng == 'act':
            s.op('act', lambda e: e.copy(out=xnT[:, g * 4:g * 4 + 4, col0:col0 + 128],
                                         in_=p[:, 0:512].rearrange("p (a b) -> p a b", a=4)), reads=[p], writes=[xnT])
        else:
            s.op('dve', lambda e: e.tensor_copy(out=xnT[:, g * 4:g * 4 + 4, col0:col0 + 128],
                                                in_=p[:, 0:512].rearrange("p (a b) -> p a b", a=4)), reads=[p], writes=[xnT])


def load_gain(c, s, gB, vec_ap):
    s.dma('sp', lambda e: e.dma_start(out=gB[:], in_=vec_ap.partition_broadcast(128)), writes=[gB])


def final_norm(c, s, src, dst):
    nc = c.nc
    with contextlib.ExitStack() as es:
        gB = sb(es, nc, "fn_gB", [128, D], F32)
        xts = [sb(es, nc, "fn_xt%d" % i, [128, D], F32) for i in range(2)]
        xos = [sb(es, nc, "fn_xo%d" % i, [128, D], F32) for i in range(2)]
        junk = sb(es, nc, "fn_junk", [128, D], BF16)
        smalls = [(sb(es, nc, "fn_ss%d" % i, [128, 1], F32), sb(es, nc, "fn_rs%d" % i, [128, 4], F32)) for i in range(2)]
        load_gain(c, s, gB, c.w['norm_final'])
        for t in range(c.NT):
            xt = xts[t % 2]; xo = xos[t % 2]
            norm_tile(c, s, src[t * 128:(t + 1) * 128, :], xt, gB, xo, smalls[t % 2], junk)
            s.dma('sp', lambda e: e.dma_start(out=dst[t * 128:(t + 1) * 128, :], in_=xo[:]), reads=[xo])
        s.barrier()


def peer_layer(c, s, li, src, dst):
    nc = c.nc
    NH, KK = 8, 16
    with contextlib.ExitStack() as es:
        gB = sb(es, nc, "pr_gB", [128, D], F32)
        wq = sb(es, nc, "pr_wq", [128, 16, D], BF16)
        KT = sb(es, nc, "pr_KT", [128, 16, 128], BF16)
        kst = sb(es, nc, "pr_kst", [128, 128], F32)
        xts = [sb(es, nc, "pr_xt%d" % i, [128, D], F32) for i in range(2)]
        xn = sb(es, nc, "pr_xn", [128, D], BF16)
        junk = sb(es, nc, "pr_junk", [128, D], BF16)
        small = (sb(es, nc, "pr_ss", [128, 1], F32), sb(es, nc, "pr_rs", [128, 4], F32))
        xnT = sb(es, nc, "pr_xnT", [128, 16, 128], BF16)
        qT = sb(es, nc, "pr_qT", [128, 16, 128], BF16)
        NUG = 4
        ug = [sb(es, nc, "pr_ug%d" % i, [128, D], BF16) for i in range(NUG)]
        vg = [sb(es, nc, "pr_vg%d" % i, [128, D], BF16) for i in range(NUG)]
        wexp = sb(es, nc, "pr_wexp", [128, 128 * 128], BF16)
        s16 = sb(es, nc, "pr_s16", [128, 16, 16], F32)
        i16 = sb(es, nc, "pr_i16", [128, 16, 16], U32)
        i16f = sb(es, nc, "pr_i16f", [128, 16, 16], F32)
        srep = sb(es, nc, "pr_srep", [128, 128], F32)
        comb = sb(es, nc, "pr_comb", [128, 8, 256], F32)
        comb2 = sb(es, nc, "pr_comb2", [128, 256], F32)
        ct = sb(es, nc, "pr_ct", [128, 8, 16], F32)
        ci = sb(es, nc, "pr_ci", [128, 8, 16], U32)
        k1 = sb(es, nc, "pr_k1", [128, 128], U32)
        k2 = sb(es, nc, "pr_k2", [128, 128], U32)
        k1f = sb(es, nc, "pr_k1f", [128, 128], F32)
        k2f = sb(es, nc, "pr_k2f", [128, 128], F32)
        oh = sb(es, nc, "pr_oh", [128, 128, 16], F32)
        i1f = sb(es, nc, "pr_i1f", [128, 128], F32)
        i2f = sb(es, nc, "pr_i2f", [128, 128], F32)
        ef = sb(es, nc, "pr_ef", [128, 128], F32)
        eu = sb(es, nc, "pr_eu", [128, 128], I32)
        evTs = [sb(es, nc, "pr_evT%d" % i, [128, 128], I32) for i in range(2)]
        gate = sb(es, nc, "pr_gate", [128, 8, 16], F32)
        gsum = sb(es, nc, "pr_gsum", [128, 8], F32)
        av = sb(es, nc, "pr_av", [128, 128], F32)
        g1 = sb(es, nc, "pr_g1", [128, 128], F32)
        g2 = sb(es, nc, "pr_g2", [128, 128], F32)
        wv = sb(es, nc, "pr_wv", [128, 128], F32)
        wT = sb(es, nc, "pr_wT", [128, 128], BF16)
        iota16 = sb(es, nc, "pr_iota16", [128, 16], F32)
        ps_s = [psb(es, nc, "pr_ps%d" % i, [128, 512], F32) for i in range(2)]
        ps_t = [psb(es, nc, "pr_pt%d" % i, [128, 1024], BF16) for i in range(2)]
        ps_o = [psb(es, nc, "pr_po%d" % i, [128, 512], F32) for i in range(4)]

        load_gain(c, s, gB, c.w['norm_ffn'][li])
        wsrc = c.w['peer_w_query'][li].rearrange("(c p) n -> p c n", p=128)
        for c0 in range(0, 16, 2):
            load_cast(s, wq, wq[:, c0:c0 + 2, :], wsrc[:, c0:c0 + 2, :])
        for hp in range(16):
            s.dma('sp', lambda e: e.dma_start(out=kst[:], in_=c.w['peer_sub_keys'][li, hp // 2, hp % 2]), writes=[kst])
            p = ps_s[hp % 2]
            s.op('pe', lambda e: e.transpose(out=p[:, 0:128], in_=kst[:], identity=c.ident_f[:]),
                 reads=[kst, c.ident_f], writes=[p])
            s.op('dve', lambda e: e.tensor_copy(out=KT[:, hp, :], in_=p[:, 0:128]), reads=[p], writes=[KT])
        s.op('pool', lambda e: e.iota(iota16[:], pattern=[[1, 16]], base=0, channel_multiplier=0,
                                      allow_small_or_imprecise_dtypes=True), writes=[iota16])
        s.op('dve', lambda e: e.memset(wexp[:], 0.0), writes=[wexp])
        utab = c.tab_u
        vtab = c.tab_v

        def stage_A(t):
            xt = xts[t % 2]; evT = evTs[t % 2]
            rows = slice(t * 128, (t + 1) * 128)
            norm_tile(c, s, src[rows, :], xt, gB, xn, small, junk)
            yield
            transpose_tile(c, s, xn, xnT, 0, ps_t)
            yield
            for nb in range(4):
                p = ps_s[nb % 2]
                for kc in range(16):
                    s.op('pe', lambda e: e.matmul(p[:], lhsT=xnT[:, kc, :], rhs=wq[:, kc, nb * 512:(nb + 1) * 512],
                                                  start=(kc == 0), stop=(kc == 15)), reads=[wq, xnT], writes=[p])
                if nb % 2 == 0:
                    s.op('act', lambda e: e.copy(out=junk[:, nb * 512:(nb + 1) * 512], in_=p[:]), reads=[p], writes=[junk])
                else:
                    s.op('dve', lambda e: e.tensor_copy(out=junk[:, nb * 512:(nb + 1) * 512], in_=p[:]), reads=[p], writes=[junk])
                yield
            transpose_tile(c, s, junk, qT, 0, ps_t)
            yield
            for hp in range(16):
                p = ps_s[hp % 2]
                s.op('pe', lambda e: e.matmul(p[:, 0:128], lhsT=qT[:, hp, :], rhs=KT[:, hp, :], start=True, stop=True),
                     reads=[qT, KT], writes=[p])
                s.op('dve', lambda e: e.tensor_copy(out=srep[:], in_=p[:, 0:128]), reads=[p], writes=[srep])
                s.op('dve', lambda e: e.max(out=s16[:, hp, 0:8], in_=srep[:]), reads=[srep], writes=[s16])
                s.op('dve', lambda e: e.max_index(out=i16[:, hp, 0:8], in_max=s16[:, hp, 0:8], in_values=srep[:]),
                     reads=[srep, s16], writes=[i16])
                yield
                s.op('dve', lambda e: e.match_replace(out=srep[:], in_to_replace=s16[:, hp, 0:8], in_values=srep[:],
                                                      imm_value=NEG), reads=[s16], writes=[srep])
                s.op('dve', lambda e: e.max(out=s16[:, hp, 8:16], in_=srep[:]), reads=[srep], writes=[s16])
                s.op('dve', lambda e: e.max_index(out=i16[:, hp, 8:16], in_max=s16[:, hp, 8:16], in_values=srep[:]),
                     reads=[srep, s16], writes=[i16])
                yield
            s.op('dve', lambda e: e.tensor_copy(out=i16f[:], in_=i16[:]), reads=[i16], writes=[i16f])
            for h in range(NH):
                s.op('dve', lambda e: e.tensor_tensor(
                    out=comb[:, h, :].rearrange("p (a b) -> p a b", a=16),
                    in0=s16[:, 2 * h, :].unsqueeze(2).to_broadcast([128, 16, 16]),
                    in1=s16[:, 2 * h + 1, :].unsqueeze(1).to_broadcast([128, 16, 16]), op=ALU.add),
                    reads=[s16], writes=[comb])
                s.op('dve', lambda e: e.max(out=ct[:, h, 0:8], in_=comb[:, h, :]), reads=[comb], writes=[ct])
                s.op('dve', lambda e: e.max_index(out=ci[:, h, 0:8], in_max=ct[:, h, 0:8], in_values=comb[:, h, :]),
                     reads=[comb, ct], writes=[ci])
                yield
                s.op('dve', lambda e: e.match_replace(out=comb2[:], in_to_replace=ct[:, h, 0:8], in_values=comb[:, h, :],
                                                      imm_value=NEG), reads=[ct, comb], writes=[comb2])
                s.op('dve', lambda e: e.max(out=ct[:, h, 8:16], in_=comb2[:]), reads=[comb2], writes=[ct])
                s.op('dve', lambda e: e.max_index(out=ci[:, h, 8:16], in_max=ct[:, h, 8:16], in_values=comb2[:]),
                     reads=[comb2, ct], writes=[ci])
                yield
            s.op('dve', lambda e: e.tensor_tensor(out=gate[:], in0=ct[:], in1=ct[:, :, 0:1].to_broadcast([128, 8, 16]),
                                                  op=ALU.subtract), reads=[ct], writes=[gate])
            s.op('act', lambda e: e.activation(out=gate[:], in_=gate[:], func=AF.Exp), reads=[gate], writes=[gate])
            s.op('dve', lambda e: e.tensor_reduce(out=gsum[:], in_=gate[:], axis=AX.X, op=ALU.add), reads=[gate], writes=[gsum])
            s.op('dve', lambda e: e.reciprocal(out=gsum[:], in_=gsum[:]), reads=[gsum], writes=[gsum])
            s.op('dve', lambda e: e.tensor_tensor(out=gate[:], in0=gate[:], in1=gsum[:].unsqueeze(2).to_broadcast([128, 8, 16]),
                                                  op=ALU.mult), reads=[gate, gsum], writes=[gate])
            yield
            cif = ci[:].rearrange("p h k -> p (h k)")
            s.op('dve', lambda e: e.tensor_single_scalar(out=k1[:], in_=cif, scalar=4, op=ALU.logical_shift_right),
                 reads=[ci], writes=[k1])
            s.op('dve', lambda e: e.tensor_single_scalar(out=k2[:], in_=cif, scalar=15, op=ALU.bitwise_and),
                 reads=[ci], writes=[k2])
            s.op('dve', lambda e: e.tensor_copy(out=k1f[:], in_=k1[:]), reads=[k1], writes=[k1f])
            s.op('dve', lambda e: e.tensor_copy(out=k2f[:], in_=k2[:]), reads=[k2], writes=[k2f])
            yield
            for (kf, pi, of) in ((k1f, 0, i1f), (k2f, 1, i2f)):
                s.op('dve', lambda e: e.tensor_tensor(out=oh[:], in0=kf[:].unsqueeze(2).to_broadcast([128, 128, 16]),
                                                      in1=iota16[:].unsqueeze(1).to_broadcast([128, 128, 16]),
                                                      op=ALU.is_equal), reads=[kf, iota16], writes=[oh])
                i16v = i16f[:].rearrange("p (h q) k -> p h q k", q=2)[:, :, pi, :]
                s.op('dve', lambda e: e.tensor_tensor(out=oh[:].rearrange("p (h k) j -> p h k j", h=8),
                                                      in0=oh[:].rearrange("p (h k) j -> p h k j", h=8),
                                                      in1=i16v.unsqueeze(2).to_broadcast([128, 8, 16, 16]),
                                                      op=ALU.mult), reads=[oh, i16f], writes=[oh])
                s.op('dve', lambda e: e.tensor_reduce(out=of[:], in_=oh[:], axis=AX.X, op=ALU.add), reads=[oh], writes=[of])
                yield
            s.op('dve', lambda e: e.scalar_tensor_tensor(out=ef[:], in0=i1f[:], scalar=128.0, in1=i2f[:],
                                                         op0=ALU.mult, op1=ALU.add), reads=[i1f, i2f], writes=[ef])
            if li > 0:
                s.op('dve', lambda e: e.tensor_scalar(out=ef[:], in0=ef[:], scalar1=float(li * 16384), scalar2=None, op0=ALU.add),
                     reads=[ef], writes=[ef])
            s.op('dve', lambda e: e.tensor_copy(out=eu[:], in_=ef[:]), reads=[ef], writes=[eu])
            yield

        def stage_A_tail(t):
            evT = evTs[t % 2]
            p = ps_s[0]
            s.op('pe', lambda e: e.transpose(out=p[:, 0:128], in_=ef[:], identity=c.ident_f[:]), reads=[ef, c.ident_f], writes=[p])
            s.op('dve', lambda e: e.tensor_copy(out=evT[:], in_=p[:, 0:128]), reads=[p], writes=[evT])

        def stage_U(t):
            for k in range(128):
                g = ug[k % NUG]
                s.dma('pool', lambda e: e.indirect_dma_start(out=g[:], out_offset=None, in_=utab,
                                                             in_offset=bass.IndirectOffsetOnAxis(ap=eu[:, k:k + 1], axis=0)),
                      reads=[eu], writes=[g])
                s.op('dve', lambda e: e.scalar_tensor_tensor(out=junk[:], in0=g[:], scalar=1.0, in1=xn[:], op0=ALU.mult,
                                                             op1=ALU.mult, accum_out=av[:, k:k + 1]),
                     reads=[g, xn], writes=[junk, av])
            s.op('dve', lambda e: e.tensor_tensor(out=g1[:], in0=av[:], in1=av[:], op=ALU.mult), reads=[av], writes=[g1])
            s.op('dve', lambda e: e.tensor_scalar(out=g1[:], in0=g1[:], scalar1=0.044715 * 0.7978845608028654,
                                                  scalar2=0.7978845608028654, op0=ALU.mult, op1=ALU.add), reads=[g1], writes=[g1])
            s.op('dve', lambda e: e.tensor_tensor(out=g1[:], in0=g1[:], in1=av[:], op=ALU.mult), reads=[g1, av], writes=[g1])
            s.op('act', lambda e: e.activation(out=g2[:], in_=g1[:], func=AF.Tanh), reads=[g1], writes=[g2])
            s.op('dve', lambda e: e.tensor_scalar(out=g2[:], in0=g2[:], scalar1=1.0, scalar2=0.5, op0=ALU.add, op1=ALU.mult),
                 reads=[g2], writes=[g2])
            s.op('dve', lambda e: e.tensor_tensor(out=g2[:], in0=g2[:], in1=av[:], op=ALU.mult), reads=[g2, av], writes=[g2])
            s.op('dve', lambda e: e.tensor_tensor(out=wv[:], in0=g2[:], in1=gate[:].rearrange("p h k -> p (h k)"), op=ALU.mult),
                 reads=[g2, gate], writes=[wv])
            p = ps_s[1]
            s.op('pe', lambda e: e.transpose(out=p[:, 0:128], in_=wv[:], identity=c.ident_f[:]), reads=[wv, c.ident_f], writes=[p])
            s.op('dve', lambda e: e.tensor_copy(out=wexp[:, 0:128 * 128:129], in_=p[:, 0:128]), reads=[p], writes=[wexp])

        def stage_V(t, gen):
            xt = xts[t % 2]; evT = evTs[t % 2]
            rows = slice(t * 128, (t + 1) * 128)
            for tk in range(128):
                g = vg[tk % NUG]
                s.dma('pool', lambda e: e.indirect_dma_start(out=g[:], out_offset=None, in_=vtab,
                                                             in_offset=bass.IndirectOffsetOnAxis(ap=evT[:, tk:tk + 1], axis=0)),
                      reads=[evT], writes=[g])
                for nb in range(4):
                    s.op('pe', lambda e: e.matmul(ps_o[nb][:], lhsT=wexp[:, tk * 128:(tk + 1) * 128],
                                                  rhs=g[:, nb * 512:(nb + 1) * 512], start=(tk == 0), stop=(tk == 127)),
                         reads=[wexp, g], writes=[ps_o[nb]])
                if gen is not None:
                    next(gen, None)
            if gen is not None:
                for _ in gen:
                    pass
            for nb in range(4):
                s.op('dve', lambda e: e.tensor_tensor(out=xt[:, nb * 512:(nb + 1) * 512], in0=ps_o[nb][:],
                                                      in1=xt[:, nb * 512:(nb + 1) * 512], op=ALU.add),
                     reads=[ps_o[nb], xt], writes=[xt])
            s.dma('sp', lambda e: e.dma_start(out=dst[rows, :], in_=xt[:]), reads=[xt])

        for _ in stage_A(0):
            pass
        stage_A_tail(0)
        stage_U(0)
        for t in range(c.NT):
            stage_V(t, stage_A(t + 1) if t + 1 < c.NT else None)
            if t + 1 < c.NT:
                stage_A_tail(t + 1)
                stage_U(t + 1)
        s.barrier()


def scr(c, name, shape, dt):
    if name not in c.scr:
        kind = "ExternalOutput" if (getattr(c, 'dbg', False) and name in ('hf_scr', 'hn_scr', 'g_scr', 'b_scr', 'ao_scr')) else "Internal"
        c.scr[name] = c.nc.dram_tensor(name, shape, dt, kind=kind).ap()
    return c.scr[name]


def evac(s, i, out_ap, in_ap, reads, writes):
    if i % 2 == 0:
        s.op('act', lambda e: e.copy(out=out_ap, in_=in_ap), reads=reads, writes=writes)
    else:
        s.op('dve', lambda e: e.tensor_copy(out=out_ap, in_=in_ap), reads=reads, writes=writes)


def mla_layer(c, s, li, j, src, dst):
    import os
    nc = c.nc; Sq = c.S; NB = Sq // 512; NT = c.NT
    scale = 192.0 ** -0.5
    qn_scr = scr(c, "qn_scr", [16, 128, Sq], BF16)
    qr_scr = scr(c, "qr_scr", [16, 64, Sq], BF16)
    ao_scr = scr(c, "ao_scr", [16, 128, Sq], BF16)
    ckv_scr = scr(c, "ckv_scr", [2, 128, Sq], BF16)
    kr_scr = scr(c, "kr_scr", [64, Sq], BF16)
    with contextlib.ExitStack() as es:
        gB = sb(es, nc, "m1_gB", [128, D], F32)
        w_in = sb(es, nc, "m1_win", [128, 16, 832], BF16)
        w_krsw = sb(es, nc, "m1_wkrsw", [128, 16, 64], BF16)
        wqu = sb(es, nc, "m1_wqu", [128, 4, 3072], BF16)
        wqsw = sb(es, nc, "m1_wqsw", [128, 4, 16, 64], BF16)
        gq = sb(es, nc, "m1_gq", [128, 4], F32)
        gkv = sb(es, nc, "m1_gkv", [128, 2], F32)
        xt = sb(es, nc, "m1_xt", [128, D], F32)
        xn = sb(es, nc, "m1_xn", [128, D], BF16)
        junk = sb(es, nc, "m1_junk", [128, D], BF16)
        small = (sb(es, nc, "m1_ss", [128, 1], F32), sb(es, nc, "m1_rs", [128, 4], F32))
        xnT = sb(es, nc, "m1_xnT", [128, 16, 512], BF16)
        raw = sb(es, nc, "m1_raw", [128, 6, 512], F32)
        sq = sb(es, nc, "m1_sq", [128, 6, 512], BF16)
        tq = sb(es, nc, "m1_tq", [128, 512], F32)
        tkv = sb(es, nc, "m1_tkv", [128, 512], F32)
        cqn = sb(es, nc, "m1_cqn", [128, 4, 512], BF16)
        ckvn = sb(es, nc, "m1_ckvn", [128, 2, 512], BF16)
        krn = sb(es, nc, "m1_krn", [64, 512], BF16)
        CC = sb(es, nc, "m1_CC", [64, 512], F32); SS = sb(es, nc, "m1_SS", [64, 512], F32)
        CCq = sb(es, nc, "m1_CCq", [64, 512], F32); SSq = sb(es, nc, "m1_SSq", [64, 512], F32)
        t1 = sb(es, nc, "m1_t1", [64, 512], F32); t2 = sb(es, nc, "m1_t2", [64, 512], F32)
        qst = [sb(es, nc, "m1_qst%d" % i, [128, 512], BF16) for i in range(2)]
        qrst = [sb(es, nc, "m1_qrst%d" % i, [64, 512], BF16) for i in range(2)]
        ps_t = [psb(es, nc, "m1_pt%d" % i, [128, 1024], BF16) for i in range(2)]
        ps_m = [psb(es, nc, "m1_pm%d" % i, [128, 512], F32) for i in range(4)]
        ps_r = [psb(es, nc, "m1_pr%d" % i, [128, 512], F32) for i in range(2)]

        load_gain(c, s, gB, c.w['norm_mix'][li])
        wsrc = c.w['mla_w_in'][j].rearrange("(c p) n -> p c n", p=128)
        for c0 in range(0, 16, 4):
            load_cast(s, w_in, w_in[:, c0:c0 + 4, :], wsrc[:, c0:c0 + 4, :])
        load_cast(s, w_krsw, w_krsw[:, :, 0:32], wsrc[:, :, 800:832])
        load_cast(s, w_krsw, w_krsw[:, :, 32:64], wsrc[:, :, 768:800])
        qsrc = c.w['mla_w_q_up'][j].rearrange("(c p) n -> p c n", p=128)
        q5 = c.w['mla_w_q_up'][j].rearrange("(c p) (h f) -> p c h f", p=128, f=192)
        for cc in range(4):
            for h0 in (0, 1536):
                load_cast(s, wqu, wqu[:, cc, h0:h0 + 1536], qsrc[:, cc, h0:h0 + 1536])
            load_cast(s, wqsw, wqsw[:, cc, :, 0:32], q5[:, cc, :, 160:192])
            load_cast(s, wqsw, wqsw[:, cc, :, 32:64], q5[:, cc, :, 128:160])
        s.dma('sp', lambda e: e.dma_start(out=gq[:], in_=c.w['mla_q_norm'][j].rearrange("(c p) -> p c", p=128),
                                          allow_slow_non_contiguous=True), writes=[gq])
        s.dma('sp', lambda e: e.dma_start(out=gkv[:], in_=c.w['mla_kv_norm'][j].rearrange("(c p) -> p c", p=128),
                                          allow_slow_non_contiguous=True), writes=[gkv])
        STG = os.environ.get('MLA_STG', 'Z')
        if STG == 'A':
            s.barrier(); return
        for b in range(NB):
            cols = slice(b * 512, (b + 1) * 512)
            for jt in range(4):
                rows = slice(b * 512 + jt * 128, b * 512 + (jt + 1) * 128)
                norm_tile(c, s, src[rows, :], xt, gB, xn, small, junk)
                transpose_tile(c, s, xn, xnT, jt * 128, ps_t)
            s.dma('sp', lambda e: e.dma_start(out=CC[:], in_=c.w['rope_c'][:, cols]), writes=[CC])
            s.dma('sp', lambda e: e.dma_start(out=SS[:], in_=c.w['rope_s'][:, cols]), writes=[SS])
            s.op('dve', lambda e: e.tensor_scalar(out=CCq[:], in0=CC[:], scalar1=scale, scalar2=None, op0=ALU.mult),
                 reads=[CC], writes=[CCq])
            s.op('dve', lambda e: e.tensor_scalar(out=SSq[:], in0=SS[:], scalar1=scale, scalar2=None, op0=ALU.mult),
                 reads=[SS], writes=[SSq])
            if STG == 'B':
                s.barrier(); return
            for fb in range(6):
                p = ps_m[fb % 4]
                for kc in range(16):
                    s.op('pe', lambda e: e.matmul(p[:], lhsT=w_in[:, kc, fb * 128:(fb + 1) * 128], rhs=xnT[:, kc, :],
                                                  start=(kc == 0), stop=(kc == 15)), reads=[w_in, xnT], writes=[p])
                s.op('dve', lambda e: e.tensor_copy(out=raw[:, fb, :], in_=p[:]), reads=[p], writes=[raw])
                s.op('act', lambda e: e.activation(out=sq[:, fb, :], in_=raw[:, fb, :], func=AF.Square), reads=[raw], writes=[sq])
            if STG == 'C':
                s.barrier(); return
            for (wt, lo, pr) in ((w_in, 768, ps_r[0]), (w_krsw, 0, ps_r[1])):
                for kc in range(16):
                    s.op('pe', lambda e: e.matmul(pr[0:64, :], lhsT=wt[:, kc, lo:lo + 64], rhs=xnT[:, kc, :],
                                                  start=(kc == 0), stop=(kc == 15)), reads=[wt, xnT], writes=[pr])
            s.op('dve', lambda e: e.tensor_tensor(out=t1[:], in0=ps_r[0][0:64, :], in1=CC[:], op=ALU.mult),
                 reads=[ps_r[0], CC], writes=[t1])
            s.op('dve', lambda e: e.tensor_tensor(out=t2[:], in0=ps_r[1][0:64, :], in1=SS[:], op=ALU.mult),
                 reads=[ps_r[1], SS], writes=[t2])
            s.op('dve', lambda e: e.tensor_tensor(out=krn[:], in0=t1[:], in1=t2[:], op=ALU.add), reads=[t1, t2], writes=[krn])
            s.dma('sp', lambda e: e.dma_start(out=kr_scr[:, cols], in_=krn[:]), reads=[krn])
            if STG == 'D':
                s.barrier(); return
            for (f0, nf, tt, n) in ((0, 4, tq, 512.0), (4, 2, tkv, 256.0)):
                p = ps_m[0 if f0 == 0 else 1]
                for i in range(nf):
                    s.op('pe', lambda e: e.matmul(p[:], lhsT=c.ones_bf[:], rhs=sq[:, f0 + i, :], start=(i == 0), stop=(i == nf - 1)),
                         reads=[c.ones_bf, sq], writes=[p])
                s.op('dve', lambda e: e.tensor_scalar(out=tt[:], in0=p[:], scalar1=1.0 / n, scalar2=EPS, op0=ALU.mult, op1=ALU.add),
                     reads=[p], writes=[tt])
                s.op('act', lambda e: e.activation(out=tt[:], in_=tt[:], func=AF.Ln), reads=[tt], writes=[tt])
                s.op('act', lambda e: e.activation(out=tt[:], in_=tt[:], func=AF.Exp, scale=-0.5), reads=[tt], writes=[tt])
            for fb in range(4):
                s.op('dve', lambda e: e.scalar_tensor_tensor(out=cqn[:, fb, :], in0=raw[:, fb, :], scalar=gq[:, fb:fb + 1], in1=tq[:],
                                                             op0=ALU.mult, op1=ALU.mult), reads=[raw, gq, tq], writes=[cqn])
            for fb in range(2):
                s.op('dve', lambda e: e.scalar_tensor_tensor(out=ckvn[:, fb, :], in0=raw[:, 4 + fb, :], scalar=gkv[:, fb:fb + 1],
                                                             in1=tkv[:], op0=ALU.mult, op1=ALU.mult), reads=[raw, gkv, tkv], writes=[ckvn])
            s.dma('sp', lambda e: e.dma_start(out=ckv_scr.rearrange("c p s -> p c s")[:, :, cols], in_=ckvn[:]), reads=[ckvn])
            if STG == 'E':
                s.barrier(); return
            for h in range(16):
                p = ps_m[h % 4]
                for kc in range(4):
                    s.op('pe', lambda e: e.matmul(p[:], lhsT=wqu[:, kc, h * 192:h * 192 + 128], rhs=cqn[:, kc, :],
                                                  start=(kc == 0), stop=(kc == 3)), reads=[wqu, cqn], writes=[p])
                qs = qst[h % 2]
                s.op('act', lambda e: e.mul(out=qs[:], in_=p[:], mul=scale), reads=[p], writes=[qs])
                s.dma('sp', lambda e: e.dma_start(out=qn_scr[h, :, cols], in_=qs[:]), reads=[qs])
                for kc in range(4):
                    s.op('pe', lambda e: e.matmul(ps_r[0][0:64, :], lhsT=wqu[:, kc, h * 192 + 128:h * 192 + 192], rhs=cqn[:, kc, :],
                                                  start=(kc == 0), stop=(kc == 3)), reads=[wqu, cqn], writes=[ps_r[0]])
                for kc in range(4):
                    s.op('pe', lambda e: e.matmul(ps_r[1][0:64, :], lhsT=wqsw[:, kc, h, :], rhs=cqn[:, kc, :],
                                                  start=(kc == 0), stop=(kc == 3)), reads=[wqsw, cqn], writes=[ps_r[1]])
                s.op('dve', lambda e: e.tensor_tensor(out=t1[:], in0=ps_r[0][0:64, :], in1=CCq[:], op=ALU.mult),
                     reads=[ps_r[0], CCq], writes=[t1])
                s.op('dve', lambda e: e.tensor_tensor(out=t2[:], in0=ps_r[1][0:64, :], in1=SSq[:], op=ALU.mult),
                     reads=[ps_r[1], SSq], writes=[t2])
                qr = qrst[h % 2]
                s.op('dve', lambda e: e.tensor_tensor(out=qr[:], in0=t1[:], in1=t2[:], op=ALU.add), reads=[t1, t2], writes=[qr])
                s.dma('sp', lambda e: e.dma_start(out=qr_scr[h, :, cols], in_=qr[:]), reads=[qr])
        s.barrier()
    import os
    if os.environ.get('MLA_STOP') == '1': return
    with contextlib.ExitStack() as es:
        wkv = sb(es, nc, "m2_wkv", [128, 2, 4096], BF16)
        ckvT = sb(es, nc, "m2_ckvT", [128, 2, Sq], BF16)
        krT = sb(es, nc, "m2_krT", [64, Sq], BF16)
        knT = [sb(es, nc, "m2_knT%d" % i, [128, Sq], BF16) for i in range(2)]
        Vh = [sb(es, nc, "m2_Vh%d" % i, [128, NT, 128], BF16) for i in range(2)]
        qn = [sb(es, nc, "m2_qn%d" % i, [128, 512], BF16) for i in range(2)]
        qr = [sb(es, nc, "m2_qr%d" % i, [64, 512], BF16) for i in range(2)]
        pT = [sb(es, nc, "m2_pT%d" % i, [128, 512], BF16) for i in range(3)]
        rden = sb(es, nc, "m2_rden", [128, 512], F32)
        ao = [sb(es, nc, "m2_ao%d" % i, [128, 512], BF16) for i in range(2)]
        ps_s = [psb(es, nc, "m2_ps%d" % i, [128, 512], F32) for i in range(3)]
        ps_o = psb(es, nc, "m2_po", [128, 512], F32)
        ps_d = psb(es, nc, "m2_pd", [128, 512], F32)
        ps_b = [psb(es, nc, "m2_pb%d" % i, [128, 512], F32) for i in range(2)]
        ksrc = c.w['mla_w_kv_up'][j].rearrange("(c p) n -> p c n", p=128)
        for cc in range(2):
            for h0 in (0, 2048):
                load_cast(s, wkv, wkv[:, cc, h0:h0 + 2048], ksrc[:, cc, h0:h0 + 2048])
            s.dma('sp', lambda e: e.dma_start(out=ckvT[:, cc, :], in_=ckv_scr[cc]), writes=[ckvT])
        s.dma('sp', lambda e: e.dma_start(out=krT[:], in_=kr_scr), writes=[krT])
        ev = 0
        for h in range(16):
            kb = knT[h % 2]; vb = Vh[h % 2]
            for b in range(NB):
                cols = slice(b * 512, (b + 1) * 512)
                p = ps_b[b % 2]
                for cc in range(2):
                    s.op('pe', lambda e: e.matmul(p[:], lhsT=wkv[:, cc, h * 256:h * 256 + 128], rhs=ckvT[:, cc, cols],
                                                  start=(cc == 0), stop=(cc == 1)), reads=[wkv, ckvT], writes=[p])
                ev += 1
                evac(s, ev, kb[:, cols], p[:], [p], [kb])
            for g in range(NT // 4):
                p = ps_b[g % 2]
                for i in range(4):
                    tl = g * 4 + i
                    for cc in range(2):
                        s.op('pe', lambda e: e.matmul(p[:, i * 128:(i + 1) * 128], lhsT=ckvT[:, cc, tl * 128:(tl + 1) * 128],
                                                      rhs=wkv[:, cc, h * 256 + 128:h * 256 + 256], start=(cc == 0), stop=(cc == 1)),
                             reads=[wkv, ckvT], writes=[p])
                ev += 1
                evac(s, ev, vb[:, g * 4:(g + 1) * 4, :], p[:].rearrange("p (a b) -> p a b", a=4), [p], [vb])
            for qb in range(NB):
                qcols = slice(qb * 512, (qb + 1) * 512)
                qnb = qn[qb % 2]; qrb = qr[qb % 2]
                s.dma('sp', lambda e: e.dma_start(out=qnb[:], in_=qn_scr[h, :, qcols]), writes=[qnb])
                s.dma('sp', lambda e: e.dma_start(out=qrb[:], in_=qr_scr[h, :, qcols]), writes=[qrb])

                def scores(kt):
                    p = ps_s[kt % 3]
                    s.op('pe', lambda e: e.matmul(p[:], lhsT=kb[:, kt * 128:(kt + 1) * 128], rhs=qnb[:], start=True, stop=False),
                         reads=[kb, qnb], writes=[p])
                    s.op('pe', lambda e: e.matmul(p[:], lhsT=krT[:, kt * 128:(kt + 1) * 128], rhs=qrb[:], start=False, stop=True),
                         reads=[krT, qrb], writes=[p])
                scores(0)
                for kt in range(NT):
                    if kt + 1 < NT:
                        scores(kt + 1)
                    p = ps_s[kt % 3]; pt = pT[kt % 3]
                    s.op('act', lambda e: e.activation(out=pt[:], in_=p[:], func=AF.Exp), reads=[p], writes=[pt])
                    s.op('pe', lambda e: e.matmul(ps_o[:], lhsT=vb[:, kt, :], rhs=pt[:], start=(kt == 0), stop=(kt == NT - 1)),
                         reads=[vb, pt], writes=[ps_o])
                    s.op('pe', lambda e: e.matmul(ps_d[:], lhsT=c.ones_bf[:], rhs=pt[:], start=(kt == 0), stop=(kt == NT - 1)),
                         reads=[c.ones_bf, pt], writes=[ps_d])
                s.op('dve', lambda e: e.reciprocal(out=rden[:], in_=ps_d[:]), reads=[ps_d], writes=[rden])
                aob = ao[qb % 2]
                s.op('dve', lambda e: e.tensor_tensor(out=aob[:], in0=ps_o[:], in1=rden[:], op=ALU.mult),
                     reads=[ps_o, rden], writes=[aob])
                s.dma('sp', lambda e: e.dma_start(out=ao_scr[h, :, qcols], in_=aob[:]), reads=[aob])
        s.barrier()
    if os.environ.get('MLA_STOP') == '2': return
    out_proj(c, s, "m3", ao_scr, c.w['mla_w_out'][j], src, dst)


def out_proj(c, s, pfx, a_scr, w_ap, src, dst):
    nc = c.nc; Sq = c.S; NB = Sq // 512
    with contextlib.ExitStack() as es:
        wo = sb(es, nc, pfx + "_wo", [128, 16, D], BF16)
        aoT = [sb(es, nc, pfx + "_aoT%d" % i, [128, 16, 512], BF16) for i in range(2)]
        xts = [sb(es, nc, pfx + "_xt%d" % i, [128, D], F32) for i in range(2)]
        xos = [sb(es, nc, pfx + "_xo%d" % i, [128, D], F32) for i in range(2)]
        ps = [psb(es, nc, pfx + "_ps%d" % i, [128, 512], F32) for i in range(8)]
        wsrc = w_ap.rearrange("(h p) n -> p h n", p=128)
        for c0 in range(0, 16, 2):
            load_cast(s, wo, wo[:, c0:c0 + 2, :], wsrc[:, c0:c0 + 2, :])
        k = 0
        for b in range(NB):
            cols = slice(b * 512, (b + 1) * 512)
            a = aoT[b % 2]
            s.dma('sp', lambda e: e.dma_start(out=a[:], in_=a_scr.rearrange("h p s -> p h s")[:, :, cols]), writes=[a])
            for jt in range(4):
                rows = slice(b * 512 + jt * 128, b * 512 + (jt + 1) * 128)
                xt = xts[k % 2]; xo = xos[k % 2]
                s.dma('sp', lambda e: e.dma_start(out=xt[:], in_=src[rows, :]), writes=[xt])
                for nb in range(4):
                    p = ps[(k % 2) * 4 + nb]
                    for h in range(16):
                        s.op('pe', lambda e: e.matmul(p[:], lhsT=a[:, h, jt * 128:(jt + 1) * 128], rhs=wo[:, h, nb * 512:(nb + 1) * 512],
                                                      start=(h == 0), stop=(h == 15)), reads=[a, wo], writes=[p])
                    s.op('dve', lambda e: e.tensor_tensor(out=xo[:, nb * 512:(nb + 1) * 512], in0=p[:], in1=xt[:, nb * 512:(nb + 1) * 512],
                                                          op=ALU.add), reads=[p, xt], writes=[xo])
                s.dma('sp', lambda e: e.dma_start(out=dst[rows, :], in_=xo[:]), reads=[xo])
                k += 1
        s.barrier()


def mlstm_layer(c, s, li, j, src, dst):
    nc = c.nc; Sq = c.S; NT = c.NT; NC = NT
    hf_scr = scr(c, "hf_scr", [Sq, D], F32)
    hn_scr = scr(c, "hn_scr", [Sq, D], BF16)
    a_scr = scr(c, "ao_scr", [16, 128, Sq], BF16)
    win = c.w['ml_w_in'][j].rearrange("(c p) n -> p c n", p=128)
    with contextlib.ExitStack() as esl:
        tokv = sb(esl, nc, "l_tokv", [128, NC, 48], F32)
        bc = sb(esl, nc, "l_bc", [128, 2, 8, 2, NC], F32)
        maskF = sb(esl, nc, "l_maskF", [128, 128], F32)
        maskB = sb(esl, nc, "l_maskB", [128, 128], F32)
        SEG = min(Sq, 1024); NSEG = Sq // SEG; CPS = SEG // 128
        g_scr = scr(c, "g_scr", [2, 8, Sq], F32)
        b_scr = scr(c, "b_scr", [2, 8, Sq], F32)
        with contextlib.ExitStack() as es:
            gB = sb(es, nc, "l0_gB", [128, D], F32)
            wg = sb(es, nc, "l0_wg", [128, 16, 32], BF16)
            xt = sb(es, nc, "l0_xt", [128, D], F32); xn = sb(es, nc, "l0_xn", [128, D], BF16)
            junk = sb(es, nc, "l0_junk", [128, D], BF16)
            small = (sb(es, nc, "l0_ss", [128, 1], F32), sb(es, nc, "l0_rs", [128, 4], F32))
            xnT = sb(es, nc, "l0_xnT", [128, 16, 128], BF16)
            bg = sb(es, nc, "l0_bg", [8, 4], F32)
            gp = [sb(es, nc, "l0_gp%d" % i, [8, SEG], F32) for i in range(4)]
            lf = sb(es, nc, "l0_lf", [8, SEG], F32); pf = sb(es, nc, "l0_pf", [8, SEG], F32)
            bb = sb(es, nc, "l0_bb", [8, SEG], F32); gg = sb(es, nc, "l0_gg", [8, SEG], F32)
            tmp = sb(es, nc, "l0_tmp", [8, SEG], F32)
            cmask = sb(es, nc, "l0_cmask", [8, SEG], F32)
            vec = [[sb(es, nc, "l0_vec%d%d" % (d, v), [8, SEG], F32) for v in range(3)] for d in range(2)]
            blA = [sb(es, nc, "l0_bl%d" % d, [8, NC], F32) for d in range(2)]
            gmaxA = [sb(es, nc, "l0_gmax%d" % d, [8, NC], F32) for d in range(2)]
            MtA = [sb(es, nc, "l0_Mt%d" % d, [8, NC], F32) for d in range(2)]
            bmA = [sb(es, nc, "l0_bm%d" % d, [8, NC], F32) for d in range(2)]
            ml_ = sb(es, nc, "l0_ml", [8, NC], F32); mst = sb(es, nc, "l0_mst", [8, NC + 1], F32)
            t8 = sb(es, nc, "l0_t8", [8, NC], F32)
            dca = [[sb(es, nc, "l0_dca%d%d" % (d, v), [8, NC], F32) for v in range(2)] for d in range(2)]
            sel = sb(es, nc, "l0_sel", [8, 8, 128], F32)
            io = sb(es, nc, "l0_io", [128, 128], F32); ip = sb(es, nc, "l0_ip", [128, 128], F32)
            ps_t = [psb(es, nc, "l0_pt%d" % i, [128, 1024], BF16) for i in range(2)]
            ps_g = [psb(es, nc, "l0_pg%d" % i, [128, 512], F32) for i in range(4)]
            load_gain(c, s, gB, c.w['norm_mix'][li])
            load_cast(s, wg, wg[:], win[:, :, 6144:6176])
            s.dma('sp', lambda e: e.dma_start(out=bg[:], in_=c.w['ml_b_gates'][j].rearrange("t h -> h t"),
                                              allow_slow_non_contiguous=True), writes=[bg])
            s.op('dve', lambda e: e.tensor_scalar(out=bg[:], in0=bg[:], scalar1=1.0 / 15.0, scalar2=None, op0=ALU.mult),
                 reads=[bg], writes=[bg])
            s.op('pool', lambda e: e.iota(io[:], pattern=[[1, 128]], base=0, channel_multiplier=0,
                                          allow_small_or_imprecise_dtypes=True), writes=[io])
            s.op('pool', lambda e: e.iota(ip[:], pattern=[[0, 128]], base=0, channel_multiplier=1,
                                          allow_small_or_imprecise_dtypes=True), writes=[ip])
            s.op('dve', lambda e: e.tensor_tensor(out=maskF[:], in0=io[:], in1=ip[:], op=ALU.is_ge), reads=[io, ip], writes=[maskF])
            s.op('dve', lambda e: e.tensor_tensor(out=maskB[:], in0=io[:], in1=ip[:], op=ALU.is_le), reads=[io, ip], writes=[maskB])
            s.op('dve', lambda e: e.tensor_copy(out=sel[:], in_=c.ident_f[0:8, 0:8].unsqueeze(2).to_broadcast([8, 8, 128])),
                 reads=[c.ident_f], writes=[sel])
            s.op('dve', lambda e: e.memset(cmask[:], 1.0), writes=[cmask])
            s.op('dve', lambda e: e.memset(cmask[:, 0:SEG:128], 0.0), writes=[cmask])
            v3 = lambda tl: tl[:].rearrange("p (a b) -> p a b", b=128)
            bcast = lambda ap: ap.unsqueeze(2).to_broadcast([8, CPS, 128])
            for sg in range(NSEG):
                ch = slice(sg * CPS, (sg + 1) * CPS)
                scols = slice(sg * SEG, (sg + 1) * SEG)
                for tl in range(CPS):
                    t = sg * CPS + tl
                    rows = slice(t * 128, (t + 1) * 128)
                    lrows = slice(tl * 128, (tl + 1) * 128)
                    norm_tile(c, s, src[rows, :], xt, gB, xn, small, junk)
                    transpose_tile(c, s, xn, xnT, 0, ps_t)
                    for ty in range(4):
                        p = ps_g[ty]
                        for kc in range(16):
                            s.op('pe', lambda e: e.matmul(p[0:8, 0:128], lhsT=wg[:, kc, ty * 8:(ty + 1) * 8], rhs=xnT[:, kc, :],
                                                          start=(kc == 0), stop=(kc == 15)), reads=[wg, xnT], writes=[p])
                        s.op('act', lambda e: e.activation(out=gp[ty][:, lrows], in_=p[0:8, 0:128], func=AF.Tanh,
                                                           bias=bg[:, ty:ty + 1], scale=1.0 / 15.0), reads=[p, bg], writes=[gp[ty]])
                for d in range(2):
                    lig = gp[2 * d]; fpg = gp[2 * d + 1]; bl = blA[d]; gmax = gmaxA[d]
                    s.op('dve', lambda e: e.tensor_scalar(out=lig[:], in0=lig[:], scalar1=15.0, scalar2=None, op0=ALU.mult),
                         reads=[lig], writes=[lig])
                    s.op('act', lambda e: e.activation(out=lf[:], in_=fpg[:], func=AF.Sigmoid, scale=15.0), reads=[fpg], writes=[lf])
                    s.op('act', lambda e: e.activation(out=lf[:], in_=lf[:], func=AF.Ln), reads=[lf], writes=[lf])
                    s.op('dve', lambda e: e.tensor_tensor_scan(out=pf[:], data0=cmask[:], data1=lf[:], initial=0.0,
                                                               op0=ALU.mult, op1=ALU.add), reads=[cmask, lf], writes=[pf])
                    s.op('dve', lambda e: e.tensor_copy(out=bl[:, ch], in_=pf[:, 127:SEG:128]), reads=[pf], writes=[bl])
                    if d == 0:
                        s.op('dve', lambda e: e.tensor_copy(out=bb[:], in_=pf[:]), reads=[pf], writes=[bb])
                    else:
                        s.op('dve', lambda e: e.tensor_tensor(out=bb[:], in0=lf[:], in1=pf[:], op=ALU.subtract), reads=[lf, pf], writes=[bb])
                        s.op('dve', lambda e: e.tensor_tensor(out=v3(bb), in0=v3(bb), in1=bcast(bl[:, ch]), op=ALU.add),
                             reads=[bb, bl], writes=[bb])
                    s.op('dve', lambda e: e.tensor_tensor(out=gg[:], in0=lig[:], in1=bb[:], op=ALU.subtract), reads=[lig, bb], writes=[gg])
                    s.op('dve', lambda e: e.tensor_reduce(out=gmax[:, ch], in_=v3(gg), axis=AX.X, op=ALU.max), reads=[gg], writes=[gmax])
                    s.dma('sp', lambda e: e.dma_start(out=g_scr[d, :, scols], in_=gg[:]), reads=[gg])
                    s.dma('sp', lambda e: e.dma_start(out=b_scr[d, :, scols], in_=bb[:]), reads=[bb])
            for d in range(2):
                bl = blA[d]; gmax = gmaxA[d]; Mt = MtA[d]
                s.op('dve', lambda e: e.tensor_tensor(out=ml_[:], in0=bl[:], in1=gmax[:], op=ALU.add), reads=[bl, gmax], writes=[ml_])
                s.op('dve', lambda e: e.memset(mst[:], 0.0), writes=[mst])
                order = range(NC) if d == 0 else range(NC - 1, -1, -1)
                for jc in order:
                    pi, ni = (jc, jc + 1) if d == 0 else (jc + 1, jc)
                    s.op('dve', lambda e: e.scalar_tensor_tensor(out=mst[:, ni:ni + 1], in0=bl[:, jc:jc + 1], scalar=mst[:, pi:pi + 1],
                                                                 in1=ml_[:, jc:jc + 1], op0=ALU.add, op1=ALU.max),
                         reads=[bl, mst, ml_], writes=[mst])
                mp = mst[:, 0:NC] if d == 0 else mst[:, 1:NC + 1]
                mn = mst[:, 1:NC + 1] if d == 0 else mst[:, 0:NC]
                s.op('dve', lambda e: e.tensor_tensor(out=Mt[:], in0=mp, in1=gmax[:], op=ALU.max), reads=[mst, gmax], writes=[Mt])
                s.op('dve', lambda e: e.tensor_tensor(out=bmA[d][:], in0=bl[:], in1=mn, op=ALU.subtract), reads=[bl, mst], writes=[bmA[d]])
                s.op('dve', lambda e: e.tensor_tensor(out=t8[:], in0=bmA[d][:], in1=mp, op=ALU.add), reads=[bmA[d], mst], writes=[t8])
                s.op('act', lambda e: e.activation(out=dca[d][0][:], in_=t8[:], func=AF.Exp), reads=[t8], writes=[dca[d][0]])
                s.op('dve', lambda e: e.tensor_tensor(out=t8[:], in0=mp, in1=Mt[:], op=ALU.subtract), reads=[Mt, mst], writes=[t8])
                s.op('act', lambda e: e.activation(out=dca[d][1][:], in_=t8[:], func=AF.Exp), reads=[t8], writes=[dca[d][1]])
                for h in range(8):
                    for v in range(2):
                        p = ps_g[(h * 2 + v) % 4]
                        s.op('pe', lambda e: e.matmul(p[:, 0:NC], lhsT=sel[:, h, :], rhs=dca[d][v][:], start=True, stop=True),
                             reads=[sel, dca[d][v]], writes=[p])
                        s.op('dve', lambda e: e.tensor_copy(out=bc[:, d, h, v, :], in_=p[:, 0:NC]), reads=[p], writes=[bc])
            for sg in range(NSEG):
                ch = slice(sg * CPS, (sg + 1) * CPS)
                scols = slice(sg * SEG, (sg + 1) * SEG)
                for d in range(2):
                    Mt = MtA[d]
                    s.dma('sp', lambda e: e.dma_start(out=gg[:], in_=g_scr[d, :, scols]), writes=[gg])
                    s.dma('sp', lambda e: e.dma_start(out=bb[:], in_=b_scr[d, :, scols]), writes=[bb])
                    s.op('dve', lambda e: e.tensor_tensor(out=v3(tmp), in0=v3(gg), in1=bcast(Mt[:, ch]), op=ALU.subtract),
                         reads=[gg, Mt], writes=[tmp])
                    s.op('act', lambda e: e.activation(out=vec[d][0][:], in_=tmp[:], func=AF.Exp), reads=[tmp], writes=[vec[d][0]])
                    s.op('dve', lambda e: e.tensor_tensor(out=v3(tmp), in0=v3(gg), in1=bcast(bmA[d][:, ch]), op=ALU.add),
                         reads=[gg, bmA[d]], writes=[tmp])
                    s.op('act', lambda e: e.activation(out=vec[d][1][:], in_=tmp[:], func=AF.Exp), reads=[tmp], writes=[vec[d][1]])
                    s.op('dve', lambda e: e.tensor_tensor(out=v3(tmp), in0=v3(bb), in1=bcast(Mt[:, ch]), op=ALU.add),
                         reads=[bb, Mt], writes=[tmp])
                    s.op('act', lambda e: e.activation(out=vec[d][2][:], in_=tmp[:], func=AF.Exp, scale=-1.0), reads=[tmp], writes=[vec[d][2]])
                for tl in range(CPS):
                    jc = sg * CPS + tl
                    p = ps_g[jc % 4]
                    for d in range(2):
                        for v in range(3):
                            i = d * 3 + v
                            s.op('pe', lambda e: e.transpose(out=p[:, i * 8:(i + 1) * 8], in_=vec[d][v][:, tl * 128:(tl + 1) * 128],
                                                             identity=c.ident_f[0:8, 0:8]), reads=[vec[d][v], c.ident_f], writes=[p])
                    s.op('dve', lambda e: e.tensor_copy(out=tokv[:, jc, :], in_=p[:, 0:48]), reads=[p], writes=[tokv])
            if getattr(c, 'dbg', False):
                d1 = nc.dram_tensor("dbg_tokv", [128, NC, 48], F32, kind="ExternalOutput").ap()
                d2 = nc.dram_tensor("dbg_bc", [128, 2, 8, 2, NC], F32, kind="ExternalOutput").ap()
                s.dma('sp', lambda e: e.dma_start(out=d1, in_=tokv[:]), reads=[tokv])
                s.dma('sp', lambda e: e.dma_start(out=d2, in_=bc[:]), reads=[bc])
            s.barrier()
        for d in range(2):
            for hg in range(2):
                with contextlib.ExitStack() as es:
                    gB = sb(es, nc, "l1_gB", [128, D], F32)
                    wq = sb(es, nc, "l1_wq", [128, 16, 512], BF16)
                    wk = sb(es, nc, "l1_wk", [128, 16, 512], BF16)
                    wv = sb(es, nc, "l1_wv", [128, 16, 1024], BF16)
                    xt = sb(es, nc, "l1_xt", [128, D], F32); xn = sb(es, nc, "l1_xn", [128, D], BF16)
                    junk = sb(es, nc, "l1_junk", [128, D], BF16)
                    small = (sb(es, nc, "l1_ss", [128, 1], F32), sb(es, nc, "l1_rs", [128, 4], F32))
                    xnT = sb(es, nc, "l1_xnT", [128, 16, 128], BF16)
                    qT = sb(es, nc, "l1_qT", [128, 4, 128], BF16); kT = sb(es, nc, "l1_kT", [128, 4, 128], BF16)
                    kw = sb(es, nc, "l1_kw", [128, 4, 128], BF16)
                    vaug = sb(es, nc, "l1_vaug", [128, 4, 258], BF16)
                    Cst = sb(es, nc, "l1_C", [128, 4, 258], F32)
                    Ct = sb(es, nc, "l1_Ct", [128, 4, 258], BF16)
                    hacc = sb(es, nc, "l1_hacc", [128, 1024], F32)
                    hft = sb(es, nc, "l1_hft", [128, 1024], F32)
                    hn = sb(es, nc, "l1_hn", [128, 1024], BF16)
                    gH = sb(es, nc, "l1_gH", [128, 1024], F32)
                    PT = [sb(es, nc, "l1_PT%d" % i, [128, 128], BF16) for i in range(2)]
                    dd = sb(es, nc, "l1_dd", [128, 4], F32)
                    hs2 = (sb(es, nc, "l1_hss", [128, 1], F32), sb(es, nc, "l1_hrs", [128, 4], F32))
                    ps_t = [psb(es, nc, "l1_pt%d" % i, [128, 1024], BF16) for i in range(2)]
                    ps_a = [psb(es, nc, "l1_pa%d" % i, [128, 512], F32) for i in range(2)]
                    ps_s = psb(es, nc, "l1_pss", [128, 512], F32)
                    ps_n = [psb(es, nc, "l1_pn%d" % i, [128, 512], F32) for i in range(2)]
                    ps_c = psb(es, nc, "l1_pc", [128, 512], F32)
                    load_gain(c, s, gB, c.w['norm_mix'][li])
                    s.dma('sp', lambda e: e.dma_start(out=gH[:], in_=c.w['ml_head_norm'][j].rearrange("h d -> (h d)")[hg * 1024:(hg + 1) * 1024]
                                                      .partition_broadcast(128)), writes=[gH])
                    for c0 in range(0, 16, 4):
                        load_cast(s, wq, wq[:, c0:c0 + 4, :], win[:, c0:c0 + 4, hg * 512:(hg + 1) * 512])
                        load_cast(s, wk, wk[:, c0:c0 + 4, :], win[:, c0:c0 + 4, 1024 + hg * 512:1024 + (hg + 1) * 512])
                        load_cast(s, wv, wv[:, c0:c0 + 4, :], win[:, c0:c0 + 4, 2048 + hg * 1024:2048 + (hg + 1) * 1024])
                    s.op('dve', lambda e: e.memset(vaug[:], 1.0), writes=[vaug])
                    s.op('dve', lambda e: e.memset(Cst[:], 0.0), writes=[Cst])
                    s.op('dve', lambda e: e.memset(Ct[:], 0.0), writes=[Ct])
                    mask = maskF if d == 0 else maskB
                    order = list(range(NC)) if d == 0 else list(range(NC - 1, -1, -1))
                    for oi, jc in enumerate(order):
                        rows = slice(jc * 128, (jc + 1) * 128)
                        hcols = slice(hg * 1024, (hg + 1) * 1024)
                        norm_tile(c, s, src[rows, :], xt, gB, xn, small, junk)
                        transpose_tile(c, s, xn, xnT, 0, ps_t)
                        if d == 1:
                            s.dma('sp', lambda e: e.dma_start(out=hft[:], in_=hf_scr[rows, hcols]), writes=[hft])
                        for (wt, outt, sc, p) in ((wq, qT, 128.0 ** -0.5, ps_a[0]), (wk, kT, 1.0, ps_a[1])):
                            for h in range(4):
                                for kc in range(16):
                                    s.op('pe', lambda e: e.matmul(p[:, h * 128:(h + 1) * 128], lhsT=wt[:, kc, h * 128:(h + 1) * 128],
                                                                  rhs=xnT[:, kc, :], start=(kc == 0), stop=(kc == 15)),
                                         reads=[wt, xnT], writes=[p])
                            s.op('act', lambda e: e.mul(out=outt[:].rearrange("p a b -> p (a b)"), in_=p[:], mul=sc), reads=[p], writes=[outt])
                        p = ps_a[0]
                        for kc in range(16):
                            s.op('pe', lambda e: e.matmul(p[:], lhsT=xnT[:, kc, :], rhs=wk[:, kc, :], start=(kc == 0), stop=(kc == 15)),
                                 reads=[wk, xnT], writes=[p])
                        for h in range(4):
                            col = (d * 3 + 1) * 8 + hg * 4 + h
                            s.op('dve', lambda e: e.tensor_scalar(out=kw[:, h, :], in0=p[:, h * 128:(h + 1) * 128],
                                                                  scalar1=tokv[:, jc, col:col + 1], scalar2=None, op0=ALU.mult),
                                 reads=[p, tokv], writes=[kw])
                        for nb in range(2):
                            p = ps_a[1] if nb == 0 else ps_a[0]
                            for kc in range(16):
                                s.op('pe', lambda e: e.matmul(p[:], lhsT=xnT[:, kc, :], rhs=wv[:, kc, nb * 512:(nb + 1) * 512],
                                                              start=(kc == 0), stop=(kc == 15)), reads=[wv, xnT], writes=[p])
                            s.op('act', lambda e: e.copy(out=vaug[:, 2 * nb:2 * nb + 2, 0:256], in_=p[:].rearrange("p (a b) -> p a b", a=2)),
                                 reads=[p], writes=[vaug])
                        for h in range(4):
                            hh = hg * 4 + h
                            cew = (d * 3 + 0) * 8 + hh; ccl = (d * 3 + 2) * 8 + hh
                            s.op('pe', lambda e: e.matmul(ps_s[:, 0:128], lhsT=kT[:, h, :], rhs=qT[:, h, :], start=True, stop=True),
                                 reads=[kT, qT], writes=[ps_s])
                            pt = PT[h % 2]
                            s.op('dve', lambda e: e.scalar_tensor_tensor(out=pt[:], in0=ps_s[:, 0:128], scalar=tokv[:, jc, cew:cew + 1],
                                                                         in1=mask[:], op0=ALU.mult, op1=ALU.mult),
                                 reads=[ps_s, tokv, mask], writes=[pt])
                            pn = ps_n[h % 2]
                            s.op('pe', lambda e: e.matmul(pn[:, 0:258], lhsT=pt[:], rhs=vaug[:, h, :], start=True, stop=False),
                                 reads=[pt, vaug], writes=[pn])
                            s.op('pe', lambda e: e.matmul(pn[:, 0:258], lhsT=qT[:, h, :], rhs=Ct[:, h, :], start=False, stop=True),
                                 reads=[qT, Ct], writes=[pn])
                            s.op('dve', lambda e: e.tensor_scalar(out=dd[:, 0:1], in0=pn[:, 256:257], scalar1=-1.0, scalar2=None, op0=ALU.mult),
                                 reads=[pn], writes=[dd])
                            s.op('dve', lambda e: e.tensor_tensor(out=dd[:, 3:4], in0=dd[:, 0:1], in1=pn[:, 256:257], op=ALU.max),
                                 reads=[dd, pn], writes=[dd])
                            s.op('dve', lambda e: e.tensor_tensor(out=dd[:, 1:2], in0=dd[:, 3:4], in1=tokv[:, jc, ccl:ccl + 1], op=ALU.max),
                                 reads=[dd, tokv], writes=[dd])
                            s.op('dve', lambda e: e.reciprocal(out=dd[:, 2:3], in_=dd[:, 1:2]), reads=[dd], writes=[dd])
                            if d == 0:
                                s.op('dve', lambda e: e.tensor_scalar(out=hacc[:, h * 256:(h + 1) * 256], in0=pn[:, 0:256], scalar1=dd[:, 2:3],
                                                                      scalar2=None, op0=ALU.mult), reads=[pn, dd], writes=[hacc])
                            else:
                                s.op('dve', lambda e: e.scalar_tensor_tensor(out=hacc[:, h * 256:(h + 1) * 256], in0=pn[:, 0:256], scalar=dd[:, 2:3],
                                                                             in1=hft[:, h * 256:(h + 1) * 256], op0=ALU.mult, op1=ALU.add),
                                     reads=[pn, dd, hft], writes=[hacc])
                            s.op('pe', lambda e: e.matmul(ps_c[:, 0:258], lhsT=kw[:, h, :], rhs=vaug[:, h, :], start=True, stop=True),
                                 reads=[kw, vaug], writes=[ps_c])
                            s.op('dve', lambda e: e.scalar_tensor_tensor(out=Cst[:, h, :], in0=Cst[:, h, :], scalar=bc[:, d, hh, 0, jc:jc + 1],
                                                                         in1=ps_c[:, 0:258], op0=ALU.mult, op1=ALU.add),
                                 reads=[Cst, bc, ps_c], writes=[Cst])
                            if oi + 1 < NC:
                                jn = order[oi + 1]
                                s.op('dve', lambda e: e.tensor_scalar(out=Ct[:, h, :], in0=Cst[:, h, :], scalar1=bc[:, d, hh, 1, jn:jn + 1],
                                                                      scalar2=None, op0=ALU.mult), reads=[Cst, bc], writes=[Ct])
                        if d == 0:
                            s.dma('sp', lambda e: e.dma_start(out=hf_scr[rows, hcols], in_=hacc[:]), reads=[hacc])
                        else:
                            for h in range(4):
                                hsl = slice(h * 256, (h + 1) * 256)
                                ss, rs = hs2
                                s.op('act', lambda e: e.activation(out=junk[:, 0:256], in_=hacc[:, hsl], func=AF.Square, accum_out=ss[:, 0:1]),
                                     reads=[hacc], writes=[junk, ss])
                                s.op('dve', lambda e: e.tensor_scalar(out=rs[:, 0:1], in0=ss[:, 0:1], scalar1=1.0 / 256, scalar2=EPS,
                                                                      op0=ALU.mult, op1=ALU.add), reads=[ss], writes=[rs])
                                s.op('act', lambda e: e.activation(out=rs[:, 1:2], in_=rs[:, 0:1], func=AF.Sqrt), reads=[rs], writes=[rs])
                                s.op('dve', lambda e: e.reciprocal(out=rs[:, 2:3], in_=rs[:, 1:2]), reads=[rs], writes=[rs])
                                s.op('dve', lambda e: e.scalar_tensor_tensor(out=hn[:, hsl], in0=hacc[:, hsl], scalar=rs[:, 2:3], in1=gH[:, hsl],
                                                                             op0=ALU.mult, op1=ALU.mult), reads=[hacc, rs, gH], writes=[hn])
                            s.dma('sp', lambda e: e.dma_start(out=hn_scr[rows, hcols], in_=hn[:]), reads=[hn])
                    s.barrier()
        with contextlib.ExitStack() as es:
            gB = sb(es, nc, "l2_gB", [128, D], F32)
            wo = sb(es, nc, "l2_wo", [128, 16, D], BF16)
            xt = sb(es, nc, "l2_xt", [128, D], F32); xn = sb(es, nc, "l2_xn", [128, D], BF16)
            junk = sb(es, nc, "l2_junk", [128, D], BF16)
            small = (sb(es, nc, "l2_ss", [128, 1], F32), sb(es, nc, "l2_rs", [128, 4], F32))
            xnT = sb(es, nc, "l2_xnT", [128, 16, 128], BF16)
            og = sb(es, nc, "l2_og", [128, D], F32)
            hn = sb(es, nc, "l2_hn", [128, D], BF16)
            yv = sb(es, nc, "l2_yv", [128, D], BF16)
            yT = sb(es, nc, "l2_yT", [128, 16, 128], BF16)
            ps_t = [psb(es, nc, "l2_pt%d" % i, [128, 1024], BF16) for i in range(2)]
            ps_a = [psb(es, nc, "l2_pa%d" % i, [128, 512], F32) for i in range(4)]
            load_gain(c, s, gB, c.w['norm_mix'][li])
            for c0 in range(0, 16, 2):
                load_cast(s, wo, wo[:, c0:c0 + 2, :], win[:, c0:c0 + 2, 4096:6144])
            for t in range(NT):
                rows = slice(t * 128, (t + 1) * 128)
                norm_tile(c, s, src[rows, :], xt, gB, xn, small, junk)
                transpose_tile(c, s, xn, xnT, 0, ps_t)
                s.dma('sp', lambda e: e.dma_start(out=hn[:], in_=hn_scr[rows, :]), writes=[hn])
                for nb in range(4):
                    p = ps_a[nb]
                    for kc in range(16):
                        s.op('pe', lambda e: e.matmul(p[:], lhsT=xnT[:, kc, :], rhs=wo[:, kc, nb * 512:(nb + 1) * 512],
                                                      start=(kc == 0), stop=(kc == 15)), reads=[wo, xnT], writes=[p])
                    s.op('dve', lambda e: e.tensor_copy(out=og[:, nb * 512:(nb + 1) * 512], in_=p[:]), reads=[p], writes=[og])
                s.op('act', lambda e: e.activation(out=og[:], in_=og[:], func=AF.Sigmoid), reads=[og], writes=[og])
                s.op('dve', lambda e: e.tensor_tensor(out=yv[:], in0=og[:], in1=hn[:], op=ALU.mult), reads=[og, hn], writes=[yv])
                transpose_tile(c, s, yv, yT, 0, ps_t)
                s.dma('sp', lambda e: e.dma_start(out=a_scr.rearrange("h p s -> p h s")[:, :, rows], in_=yT[:]), reads=[yT])
            s.barrier()
    out_proj(c, s, "l3", a_scr, c.w['ml_w_out'][j], src, dst)


def build(Sq, layers, final=True, test=False, dbg=False):
    nc = bass.Bass("TRN2", target_bir_lowering=False)
    c = Ctx(); c.nc = nc; c.S = Sq; c.NT = Sq // 128; c.dbg = dbg
    shapes = dict(
        norm_mix=[4, D], norm_ffn=[4, D], norm_final=[D],
        mla_w_in=[2, D, 832], mla_q_norm=[2, 512], mla_w_q_up=[2, 512, 3072], mla_kv_norm=[2, 256],
        mla_w_kv_up=[2, 256, 4096], mla_w_out=[2, D, D],
        ml_w_in=[2, D, 6176], ml_b_gates=[2, 4, 8], ml_head_norm=[2, 8, 256], ml_w_out=[2, D, D],
        peer_w_query=[4, D, D], peer_sub_keys=[4, 8, 2, 128, 128], peer_u=[4, 16384, D], peer_v=[4, 16384, D],
        rope_c=[64, Sq], rope_s=[64, Sq])
    kinds = set(k for (k, _, _) in layers)
    if test:
        need = {'norm_final'}
        if 'peer' in kinds: need |= {'norm_ffn', 'peer_w_query', 'peer_sub_keys', 'peer_u', 'peer_v'}
        if 'mla' in kinds: need |= {'norm_mix', 'rope_c', 'rope_s'} | {k for k in shapes if k.startswith('mla_')}
        if 'mlstm' in kinds: need |= {'norm_mix'} | {k for k in shapes if k.startswith('ml_')}
        shapes = {k: ([1] + v[1:] if (k not in ('norm_final', 'rope_c', 'rope_s')) else v) for k, v in shapes.items() if k in need}
    c.w = {k: nc.dram_tensor(k, v, F32, kind="ExternalInput").ap() for k, v in shapes.items()}
    x = nc.dram_tensor("x", [Sq, D], F32, kind="ExternalInput").ap()
    y = nc.dram_tensor("y", [Sq, D], F32, kind="ExternalOutput").ap()
    res = nc.dram_tensor("res", [Sq, D], F32, kind="Internal").ap()
    c.x = x; c.y = y; c.res = res; c.scr = {}
    with contextlib.ExitStack() as es:
        s = S(nc)
        c.ident_bf = sb(es, nc, "ident_bf", [128, 128], BF16)
        c.ident_f = sb(es, nc, "ident_f", [128, 128], F32)
        c.ones_bf = sb(es, nc, "ones_bf", [128, 128], BF16)
        with contextlib.ExitStack() as es2:
            io = sb(es2, nc, "io_a", [128, 128], F32)
            ip = sb(es2, nc, "io_b", [128, 128], F32)
            s.op('pool', lambda e: e.iota(io[:], pattern=[[1, 128]], base=0, channel_multiplier=0,
                                          allow_small_or_imprecise_dtypes=True), writes=[io])
            s.op('pool', lambda e: e.iota(ip[:], pattern=[[0, 128]], base=0, channel_multiplier=1,
                                          allow_small_or_imprecise_dtypes=True), writes=[ip])
            s.op('dve', lambda e: e.tensor_tensor(out=c.ident_f[:], in0=io[:], in1=ip[:], op=ALU.is_equal),
                 reads=[io, ip], writes=[c.ident_f])
            s.op('dve', lambda e: e.tensor_copy(out=c.ident_bf[:], in_=c.ident_f[:]), reads=[c.ident_f], writes=[c.ident_bf])
            s.op('dve', lambda e: e.memset(c.ones_bf[:], 1.0), writes=[c.ones_bf])
            s.barrier()
        if 'peer' in kinds:
            nl = c.w['peer_u'].shape[0]
            c.tab_u = nc.dram_tensor("tab_u_bf", [nl * 16384, D], BF16, kind="Internal").ap()
            c.tab_v = nc.dram_tensor("tab_v_bf", [nl * 16384, D], BF16, kind="Internal").ap()
            for (srcn, dstt) in (('peer_u', c.tab_u), ('peer_v', c.tab_v)):
                fl = c.w[srcn].rearrange("l e d -> (l e) d")
                for r0 in range(0, nl * 16384, 8192):
                    s.dma('pool', lambda e: e.dma_start(out=dstt[r0:r0 + 8192, :], in_=fl[r0:r0 + 8192, :]))
            s.barrier()
        cur = x
        for (kind, li, j) in layers:
            if kind == 'peer':
                peer_layer(c, s, li, cur, res)
            elif kind == 'mla':
                mla_layer(c, s, li, j, cur, res)
            elif kind == 'mlstm':
                mlstm_layer(c, s, li, j, cur, res)
            cur = res
        if final:
            final_norm(c, s, cur, y)
        s.barrier()
        c.ninst = s.ninst
    return nc, c


def rope_tables(Sq):
    inv = 10000.0 ** (-np.arange(0, 64, 2, dtype=np.float32) / 64.0)
    ang = np.arange(Sq, dtype=np.float32)[:, None] * inv[None, :]
    cos = np.cos(ang).astype(np.float32).T
    sin = np.sin(ang).astype(np.float32).T
    return (np.ascontiguousarray(np.concatenate([cos, cos], 0)),
            np.ascontiguousarray(np.concatenate([-sin, sin], 0)))


FULL_LAYERS = [('mla', 0, 0), ('peer', 0, 0), ('mlstm', 1, 0), ('peer', 1, 0),
               ('mla', 2, 1), ('peer', 2, 0), ('mlstm', 3, 1), ('peer', 3, 0)]


def kernel(**inputs):
    Sq = 8192
    nc, c = build(Sq, FULL_LAYERS)
    rc, rs = rope_tables(Sq)
    wts = {k: np.ascontiguousarray(np.asarray(v, dtype=np.float32)) for k, v in inputs.items()
           if k not in ('x_prompt', 'x_sample')}
    wts['rope_c'] = rc; wts['rope_s'] = rs
    xs = [np.asarray(inputs['x_prompt'][0]), np.asarray(inputs['x_prompt'][1]), np.asarray(inputs['x_sample'][0])]
    in_maps = []
    for i in range(3):
        m = dict(wts); m['x'] = np.ascontiguousarray(xs[i], dtype=np.float32)
        in_maps.append(m)
    r = run_bass_kernel_spmd(nc, in_maps, core_ids=[0, 1, 2])
    ys = [np.asarray(r.results[i]['y'], dtype=np.float32) for i in range(3)]
    return (np.stack([ys[0], ys[1]], 0), ys[2][None])
```
